# Optimizing a Trainium2 kernel written in Bass

```python
import math
import jax
import jax.numpy as jnp
from jax import lax
import numpy as np

D_MODEL = 2048
BATCH = 1
SEQ = 16384
DEPTH = 4

HEAD_DIM = 128
GROUP_WIDTH = D_MODEL // 4
N_GROUP_HEADS = GROUP_WIDTH // HEAD_DIM
MIX_WIDTH = 4 * GROUP_WIDTH

SSM_CH = GROUP_WIDTH
SSM_GROUP = 16
SSM_NG = SSM_CH // SSM_GROUP
SSM_STATE = 64

MOBA_BLOCK = 256
MOBA_TOPK = 3

NSA_CMP_LEN = 32
NSA_CMP_STRIDE = 16
NSA_CMP_HIDDEN = 256
NSA_SEL_BLOCK = 64
NSA_TOPK = 16
NSA_WINDOW = 512

N_MEM = 256

ROPE_THETA = 500000.0
ROPE_DIM = HEAD_DIM // 4

D_FF = 5632
N_EXPERTS = 8
TOP_K = 2
MOE_BLOCK = 256

Q_BLOCK = 128
LN_EPS = 1e-5
NEG_INF = -1e30
FORCE_SCORE = 1e9
ALPHA = (2.0 * DEPTH) ** 0.25
BETA = (8.0 * DEPTH) ** -0.25

OFF_SSM = 0
OFF_MOBA = OFF_SSM + SSM_CH
OFF_NSA_Q = OFF_MOBA + 3 * GROUP_WIDTH
OFF_NSA_KV = OFF_NSA_Q + GROUP_WIDTH
OFF_NSA_G = OFF_NSA_KV + 6 * HEAD_DIM
OFF_X_Q = OFF_NSA_G + 3 * N_GROUP_HEADS
IN_WIDTH = OFF_X_Q + GROUP_WIDTH

kernel_name = 'hybrid_s5_moba_nsa_xattn_deepnorm_moe'


def layer_norm(x, g, b):
    xf = x.astype(jnp.float32)
    mu = jnp.mean(xf, axis=-1, keepdims=True)
    var = jnp.mean(jnp.square(xf - mu), axis=-1, keepdims=True)
    y = (xf - mu) * lax.rsqrt(var + LN_EPS)
    return (y * g.astype(jnp.float32) + b.astype(jnp.float32)).astype(x.dtype)


def masked_softmax(s, mask):
    s = jnp.where(mask, s.astype(jnp.float32), NEG_INF)
    m = jnp.max(s, axis=-1, keepdims=True)
    e = jnp.where(mask, jnp.exp(s - m), 0.0)
    return e / jnp.maximum(jnp.sum(e, axis=-1, keepdims=True), 1e-30)


def rope_tables(seq_len):
    inv_freq = 1.0 / (ROPE_THETA ** (jnp.arange(0, ROPE_DIM, 2, dtype=jnp.float32) / ROPE_DIM))
    ang = jnp.arange(seq_len, dtype=jnp.float32)[:, None] * inv_freq[None, :]
    return jnp.cos(ang), jnp.sin(ang)


def partial_rope(t, cos, sin):
    half = ROPE_DIM // 2
    bshape = (t.shape[0],) + (1,) * (t.ndim - 2) + (half,)
    c = cos.reshape(bshape).astype(t.dtype)
    s = sin.reshape(bshape).astype(t.dtype)
    t1, t2, rest = t[..., :half], t[..., half:ROPE_DIM], t[..., ROPE_DIM:]
    return jnp.concatenate([t1 * c - t2 * s, t1 * s + t2 * c, rest], axis=-1)


def s5_mixer(u, a_re, a_im, log_dt, b_re, b_im, c_re, c_im, d_skip, w_glu):
    L = u.shape[0]
    f32 = jnp.float32
    uf = u.astype(f32).reshape(L, SSM_NG, SSM_GROUP)
    dt = jnp.exp(log_dt.astype(f32))[:, None]
    ar, ai = a_re.astype(f32), a_im.astype(f32)
    mag = jnp.exp(ar * dt)
    lam_re, lam_im = mag * jnp.cos(ai * dt), mag * jnp.sin(ai * dt)
    den = ar * ar + ai * ai
    nr, ni = lam_re - 1.0, lam_im
    coef_re = (nr * ar + ni * ai) / den
    coef_im = (ni * ar - nr * ai) / den
    br, bi = b_re.astype(f32), b_im.astype(f32)
    bbar_re = coef_re[..., None] * br - coef_im[..., None] * bi
    bbar_im = coef_re[..., None] * bi + coef_im[..., None] * br
    bu_re = jnp.einsum('lgc,gnc->lgn', uf, bbar_re)
    bu_im = jnp.einsum('lgc,gnc->lgn', uf, bbar_im)
    a_seq_re = jnp.broadcast_to(lam_re, bu_re.shape)
    a_seq_im = jnp.broadcast_to(lam_im, bu_im.shape)

    def combine(e1, e2):
        a1r, a1i, s1r, s1i = e1
        a2r, a2i, s2r, s2i = e2
        return (a2r * a1r - a2i * a1i, a2r * a1i + a2i * a1r,
                a2r * s1r - a2i * s1i + s2r, a2r * s1i + a2i * s1r + s2i)

    _, _, h_re, h_im = lax.associative_scan(combine, (a_seq_re, a_seq_im, bu_re, bu_im), axis=0)
    y = (jnp.einsum('lgn,gcn->lgc', h_re, c_re.astype(f32))
         - jnp.einsum('lgn,gcn->lgc', h_im, c_im.astype(f32))
         + d_skip.astype(f32).reshape(SSM_NG, SSM_GROUP) * uf).reshape(L, SSM_CH)
    z = jax.nn.gelu(y)
    out = z * jax.nn.sigmoid(z @ w_glu.astype(f32))
    return out.astype(u.dtype)


def moba_mixer(q, k, v, cos, sin):
    L, H, Dh = q.shape
    q = partial_rope(q, cos, sin)
    k = partial_rope(k, cos, sin)
    Lp = -(-L // MOBA_BLOCK) * MOBA_BLOCK
    k = jnp.pad(k, ((0, Lp - L), (0, 0), (0, 0)))
    v = jnp.pad(v, ((0, Lp - L), (0, 0), (0, 0)))
    nb = Lp // MOBA_BLOCK
    kb = k.reshape(nb, MOBA_BLOCK, H, Dh).transpose(2, 0, 1, 3)
    vb = v.reshape(nb, MOBA_BLOCK, H, Dh).transpose(2, 0, 1, 3)
    kmean = jnp.mean(kb.astype(jnp.float32), axis=2).astype(k.dtype)
    topk = min(MOBA_TOPK, nb)
    scale = HEAD_DIM ** -0.5
    h_idx = jnp.arange(H)[None, :, None]
    blk_ids = jnp.arange(nb)
    own_off = jnp.arange(MOBA_BLOCK)

    def one_block(c):
        start = c * Q_BLOCK
        qc = lax.dynamic_slice_in_dim(q, start, Q_BLOCK, 0)
        pos = start + jnp.arange(Q_BLOCK)
        own = start // MOBA_BLOCK
        gs = jnp.einsum('qhd,hbd->qhb', qc, kmean)
        gs = jnp.where(blk_ids < own, gs.astype(jnp.float32), NEG_INF)
        gval, idx = lax.top_k(gs, topk)
        sel_ok = gval > 0.5 * NEG_INF
        kg = kb[h_idx, idx]
        vg = vb[h_idx, idx]
        s_sel = jnp.einsum('qhd,qhkjd->qhkj', qc, kg).reshape(Q_BLOCK, H, topk * MOBA_BLOCK)
        m_sel = jnp.broadcast_to(sel_ok[..., None], (Q_BLOCK, H, topk, MOBA_BLOCK)).reshape(Q_BLOCK, H, topk * MOBA_BLOCK)
        k_own = lax.dynamic_slice_in_dim(k, own * MOBA_BLOCK, MOBA_BLOCK, 0)
        v_own = lax.dynamic_slice_in_dim(v, own * MOBA_BLOCK, MOBA_BLOCK, 0)
        s_own = jnp.einsum('qhd,jhd->qhj', qc, k_own)
        m_own = jnp.broadcast_to(((own * MOBA_BLOCK + own_off)[None, :] <= pos[:, None])[:, None, :], s_own.shape)
        p = masked_softmax(jnp.concatenate([s_sel, s_own], axis=-1) * scale,
                           jnp.concatenate([m_sel, m_own], axis=-1)).astype(v.dtype)
        p_sel = p[..., :topk * MOBA_BLOCK].reshape(Q_BLOCK, H, topk, MOBA_BLOCK)
        p_own = p[..., topk * MOBA_BLOCK:]
        return jnp.einsum('qhkj,qhkjd->qhd', p_sel, vg) + jnp.einsum('qhj,jhd->qhd', p_own, v_own)

    out = lax.map(one_block, jnp.arange(L // Q_BLOCK))
    return out.reshape(L, H, Dh)


def nsa_mixer(q, kv, gates, pos_k, pos_v, ck1, ck2, cv1, cv2, cos, sin):
    L, H, Dh = q.shape
    k_cmp, v_cmp, k_sel, v_sel, k_win, v_win = [kv[:, j] for j in range(6)]
    q_rope = partial_rope(q, cos, sin)
    k_sel = partial_rope(k_sel, cos, sin)
    k_win = partial_rope(k_win, cos, sin)
    scale = HEAD_DIM ** -0.5

    nc = (L - NSA_CMP_LEN) // NSA_CMP_STRIDE + 1
    cidx = jnp.arange(nc)[:, None] * NSA_CMP_STRIDE + jnp.arange(NSA_CMP_LEN)[None, :]

    def compress(t, pos_emb, w1, w2):
        blk = (t[cidx] + pos_emb).reshape(nc, NSA_CMP_LEN * HEAD_DIM)
        return jax.nn.gelu(blk @ w1) @ w2

    kc = compress(k_cmp, pos_k, ck1, ck2)
    vc = compress(v_cmp, pos_v, cv1, cv2)
    cmp_end = jnp.arange(nc) * NSA_CMP_STRIDE + NSA_CMP_LEN - 1

    ns = L // NSA_SEL_BLOCK
    kS = k_sel.reshape(ns, NSA_SEL_BLOCK, Dh)
    vS = v_sel.reshape(ns, NSA_SEL_BLOCK, Dh)
    topk = min(NSA_TOPK, ns)
    ratio = NSA_SEL_BLOCK // NSA_CMP_STRIDE
    lead = NSA_CMP_LEN // NSA_CMP_STRIDE - 1
    n_terms = ratio + lead
    right = max(ratio * ns - nc, 0)
    span = ratio * (ns - 1) + 1
    blk_start = jnp.arange(ns) * NSA_SEL_BLOCK
    blk_ids = jnp.arange(ns)
    sel_off = jnp.arange(NSA_SEL_BLOCK)

    k_wp = jnp.pad(k_win, ((NSA_WINDOW, 0), (0, 0)))
    v_wp = jnp.pad(v_win, ((NSA_WINDOW, 0), (0, 0)))
    win_off = jnp.arange(Q_BLOCK + NSA_WINDOW) - NSA_WINDOW

    def one_block(c):
        start = c * Q_BLOCK
        pos = start + jnp.arange(Q_BLOCK)
        qc = lax.dynamic_slice_in_dim(q, start, Q_BLOCK, 0)
        qr = lax.dynamic_slice_in_dim(q_rope, start, Q_BLOCK, 0)
        g = lax.dynamic_slice_in_dim(gates, start, Q_BLOCK, 0)
        m_c = (cmp_end[None, :] <= pos[:, None])[:, None, :]
        p_c = masked_softmax(jnp.einsum('qhd,cd->qhc', qc, kc) * scale, m_c)
        o_c = jnp.einsum('qhc,cd->qhd', p_c.astype(vc.dtype), vc)
        imp = jnp.pad(jnp.sum(p_c, axis=1), ((0, 0), (lead, right)))
        imp_sel = sum(imp[:, r:r + span:ratio] for r in range(n_terms))
        avail = blk_start[None, :] <= pos[:, None]
        own = (pos // NSA_SEL_BLOCK)[:, None] == blk_ids[None, :]
        imp_sel = jnp.where(own, FORCE_SCORE, jnp.where(avail, imp_sel, NEG_INF))
        sval, sidx = lax.top_k(imp_sel, topk)
        s_ok = sval > 0.5 * NEG_INF
        kg = kS[sidx]
        vg = vS[sidx].reshape(Q_BLOCK, topk * NSA_SEL_BLOCK, Dh)
        kpos = sidx[..., None] * NSA_SEL_BLOCK + sel_off
        m_s = (s_ok[..., None] & (kpos <= pos[:, None, None])).reshape(Q_BLOCK, 1, topk * NSA_SEL_BLOCK)
        s_s = jnp.einsum('qhd,qkjd->qhkj', qr, kg).reshape(Q_BLOCK, H, topk * NSA_SEL_BLOCK) * scale
        p_s = masked_softmax(s_s, m_s).astype(vg.dtype)
        o_s = jnp.einsum('qhn,qnd->qhd', p_s, vg)
        kw = lax.dynamic_slice_in_dim(k_wp, start, Q_BLOCK + NSA_WINDOW, 0)
        vw = lax.dynamic_slice_in_dim(v_wp, start, Q_BLOCK + NSA_WINDOW, 0)
        wpos = start + win_off
        m_w = ((wpos[None, :] <= pos[:, None]) & (wpos[None, :] > pos[:, None] - NSA_WINDOW)
               & (wpos[None, :] >= 0))[:, None, :]
        p_w = masked_softmax(jnp.einsum('qhd,jd->qhj', qr, kw) * scale, m_w).astype(vw.dtype)
        o_w = jnp.einsum('qhj,jd->qhd', p_w, vw)
        return g[..., 0:1] * o_c + g[..., 1:2] * o_s + g[..., 2:3] * o_w

    out = lax.map(one_block, jnp.arange(L // Q_BLOCK))
    return out.reshape(L, H, Dh)


def mixing_sublayer(x, mem, cos, sin, w_in, a_re, a_im, log_dt, b_re, b_im, c_re, c_im,
                    d_skip, w_glu, pos_k, pos_v, ck1, ck2, cv1, cv2, mem_wk, mem_wv, w_o):
    B, L, _ = x.shape
    H, Dh, GW = N_GROUP_HEADS, HEAD_DIM, GROUP_WIDTH
    h = x @ w_in
    u = h[..., OFF_SSM:OFF_SSM + SSM_CH]
    mq = h[..., OFF_MOBA:OFF_MOBA + GW].reshape(B, L, H, Dh)
    mk = h[..., OFF_MOBA + GW:OFF_MOBA + 2 * GW].reshape(B, L, H, Dh)
    mv = h[..., OFF_MOBA + 2 * GW:OFF_MOBA + 3 * GW].reshape(B, L, H, Dh)
    nq = h[..., OFF_NSA_Q:OFF_NSA_Q + GW].reshape(B, L, H, Dh)
    nkv = h[..., OFF_NSA_KV:OFF_NSA_KV + 6 * Dh].reshape(B, L, 6, Dh)
    ng = jax.nn.sigmoid(h[..., OFF_NSA_G:OFF_NSA_G + 3 * H]).reshape(B, L, H, 3)
    xq = h[..., OFF_X_Q:OFF_X_Q + GW].reshape(B, L, H, Dh)

    y_ssm = jax.vmap(lambda ub: s5_mixer(ub, a_re, a_im, log_dt, b_re, b_im, c_re, c_im, d_skip, w_glu))(u)
    y_moba = jax.vmap(lambda qb, kb, vb: moba_mixer(qb, kb, vb, cos, sin))(mq, mk, mv)
    y_nsa = jax.vmap(lambda qb, kvb, gb: nsa_mixer(qb, kvb, gb, pos_k, pos_v, ck1, ck2, cv1, cv2, cos, sin))(nq, nkv, ng)

    mem_k = (mem @ mem_wk).reshape(B, N_MEM, H, Dh)
    mem_v = (mem @ mem_wv).reshape(B, N_MEM, H, Dh)
    s_x = jnp.einsum('blhd,bmhd->blhm', xq, mem_k) * (HEAD_DIM ** -0.5)
    p_x = jax.nn.softmax(s_x.astype(jnp.float32), axis=-1).astype(mem_v.dtype)
    y_x = jnp.einsum('blhm,bmhd->blhd', p_x, mem_v)

    y = jnp.concatenate([y_ssm, y_moba.reshape(B, L, GW), y_nsa.reshape(B, L, GW),
                         y_x.reshape(B, L, GW)], axis=-1)
    return y @ w_o


def swiglu(x, wg, wu, wd):
    return (jax.nn.silu(x @ wg) * (x @ wu)) @ wd


def moe_swiglu(x, router, wg, wu, wd):
    B, L, D = x.shape
    T = B * L
    xt = x.reshape(T, D)
    logits = (xt @ router).astype(jnp.float32)
    top_val, top_idx = lax.top_k(logits, TOP_K)
    gate = jax.nn.softmax(top_val, axis=-1)
    e_flat = top_idx.reshape(-1)
    tok_flat = jnp.repeat(jnp.arange(T, dtype=jnp.int32), TOP_K)
    w_flat = gate.reshape(-1)
    order = jnp.argsort(e_flat)
    e_s, tok_s, w_s = e_flat[order], tok_flat[order], w_flat[order]
    counts = jnp.zeros((N_EXPERTS,), jnp.int32).at[e_flat].add(1)
    padded = (counts + MOE_BLOCK - 1) // MOE_BLOCK * MOE_BLOCK
    start = jnp.cumsum(counts) - counts
    pend = jnp.cumsum(padded)
    pstart = pend - padded
    dest = pstart[e_s] + (jnp.arange(T * TOP_K, dtype=jnp.int32) - start[e_s])
    n_blocks = -(-(T * TOP_K + N_EXPERTS * (MOE_BLOCK - 1)) // MOE_BLOCK)
    P = n_blocks * MOE_BLOCK
    buf_tok = jnp.zeros((P,), jnp.int32).at[dest].set(tok_s)
    buf_w = jnp.zeros((P,), jnp.float32).at[dest].set(w_s)
    blk_exp = jnp.minimum(jnp.searchsorted(pend, jnp.arange(n_blocks, dtype=jnp.int32) * MOE_BLOCK, side='right'),
                          N_EXPERTS - 1)

    def one_block(b):
        toks = lax.dynamic_slice_in_dim(buf_tok, b * MOE_BLOCK, MOE_BLOCK)
        e = blk_exp[b]
        xb = xt[toks]
        return (jax.nn.silu(xb @ wg[e]) * (xb @ wu[e])) @ wd[e]

    yb = lax.map(one_block, jnp.arange(n_blocks)).reshape(P, D)
    y = jnp.zeros_like(xt).at[buf_tok].add((yb * buf_w[:, None]).astype(xt.dtype))
    return y.reshape(B, L, D)


def setup_inputs(seed: int = 0) -> dict:
    key = jax.random.key(seed)
    ks = jax.random.split(key, 40)
    f32 = jnp.float32
    n_dense = (DEPTH + 1) // 2
    n_moe = DEPTH // 2

    def nrm(i, shape, scale):
        return jax.random.normal(ks[i], shape, f32) * scale

    a_im = math.pi * jnp.broadcast_to(jnp.arange(SSM_STATE, dtype=f32), (DEPTH, SSM_NG, SSM_STATE))
    return {
        'x': nrm(0, (BATCH, SEQ, D_MODEL), 1.0),
        'mem': nrm(1, (BATCH, N_MEM, D_MODEL), 1.0),
        'ln_in_g': 1.0 + nrm(2, (D_MODEL,), 0.02),
        'ln_in_b': nrm(3, (D_MODEL,), 0.02),
        'w_in': nrm(4, (DEPTH, D_MODEL, IN_WIDTH), D_MODEL ** -0.5),
        'ssm_a_re': -0.5 + nrm(5, (DEPTH, SSM_NG, SSM_STATE), 0.01),
        'ssm_a_im': a_im + nrm(6, (DEPTH, SSM_NG, SSM_STATE), 0.01),
        'ssm_log_dt': jax.random.uniform(ks[7], (DEPTH, SSM_NG), f32, math.log(1e-3), math.log(1e-1)),
        'ssm_b_re': nrm(8, (DEPTH, SSM_NG, SSM_STATE, SSM_GROUP), (2.0 * SSM_GROUP) ** -0.5),
        'ssm_b_im': nrm(9, (DEPTH, SSM_NG, SSM_STATE, SSM_GROUP), (2.0 * SSM_GROUP) ** -0.5),
        'ssm_c_re': nrm(10, (DEPTH, SSM_NG, SSM_GROUP, SSM_STATE), (2.0 * SSM_STATE) ** -0.5),
        'ssm_c_im': nrm(11, (DEPTH, SSM_NG, SSM_GROUP, SSM_STATE), (2.0 * SSM_STATE) ** -0.5),
        'ssm_d': nrm(12, (DEPTH, SSM_CH), 1.0),
        'ssm_w_glu': nrm(13, (DEPTH, SSM_CH, SSM_CH), SSM_CH ** -0.5),
        'nsa_pos_k': nrm(14, (DEPTH, NSA_CMP_LEN, HEAD_DIM), 0.02),
        'nsa_pos_v': nrm(15, (DEPTH, NSA_CMP_LEN, HEAD_DIM), 0.02),
        'nsa_ck1': nrm(16, (DEPTH, NSA_CMP_LEN * HEAD_DIM, NSA_CMP_HIDDEN), (NSA_CMP_LEN * HEAD_DIM) ** -0.5),
        'nsa_ck2': nrm(17, (DEPTH, NSA_CMP_HIDDEN, HEAD_DIM), NSA_CMP_HIDDEN ** -0.5),
        'nsa_cv1': nrm(18, (DEPTH, NSA_CMP_LEN * HEAD_DIM, NSA_CMP_HIDDEN), (NSA_CMP_LEN * HEAD_DIM) ** -0.5),
        'nsa_cv2': nrm(19, (DEPTH, NSA_CMP_HIDDEN, HEAD_DIM), NSA_CMP_HIDDEN ** -0.5),
        'mem_wk': nrm(20, (DEPTH, D_MODEL, GROUP_WIDTH), D_MODEL ** -0.5),
        'mem_wv': nrm(21, (DEPTH, D_MODEL, GROUP_WIDTH), D_MODEL ** -0.5),
        'w_o': nrm(22, (DEPTH, MIX_WIDTH, D_MODEL), BETA * MIX_WIDTH ** -0.5),
        'ln1_g': 1.0 + nrm(23, (DEPTH, D_MODEL), 0.02),
        'ln1_b': nrm(24, (DEPTH, D_MODEL), 0.02),
        'ln2_g': 1.0 + nrm(25, (DEPTH, D_MODEL), 0.02),
        'ln2_b': nrm(26, (DEPTH, D_MODEL), 0.02),
        'ffn_w_gate': nrm(27, (n_dense, D_MODEL, D_FF), D_MODEL ** -0.5),
        'ffn_w_up': nrm(28, (n_dense, D_MODEL, D_FF), D_MODEL ** -0.5),
        'ffn_w_down': nrm(29, (n_dense, D_FF, D_MODEL), BETA * D_FF ** -0.5),
        'moe_router': nrm(30, (n_moe, D_MODEL, N_EXPERTS), D_MODEL ** -0.5),
        'moe_w_gate': nrm(31, (n_moe, N_EXPERTS, D_MODEL, D_FF), D_MODEL ** -0.5),
        'moe_w_up': nrm(32, (n_moe, N_EXPERTS, D_MODEL, D_FF), D_MODEL ** -0.5),
        'moe_w_down': nrm(33, (n_moe, N_EXPERTS, D_FF, D_MODEL), BETA * D_FF ** -0.5),
    }


def reference(x, mem, ln_in_g, ln_in_b, w_in, ssm_a_re, ssm_a_im, ssm_log_dt, ssm_b_re, ssm_b_im,
              ssm_c_re, ssm_c_im, ssm_d, ssm_w_glu, nsa_pos_k, nsa_pos_v, nsa_ck1, nsa_ck2, nsa_cv1,
              nsa_cv2, mem_wk, mem_wv, w_o, ln1_g, ln1_b, ln2_g, ln2_b, ffn_w_gate, ffn_w_up,
              ffn_w_down, moe_router, moe_w_gate, moe_w_up, moe_w_down):
    x = layer_norm(x, ln_in_g, ln_in_b)
    cos, sin = rope_tables(x.shape[1])
    for i in range(DEPTH):
        y = mixing_sublayer(x, mem, cos, sin, w_in[i], ssm_a_re[i], ssm_a_im[i], ssm_log_dt[i],
                            ssm_b_re[i], ssm_b_im[i], ssm_c_re[i], ssm_c_im[i], ssm_d[i], ssm_w_glu[i],
                            nsa_pos_k[i], nsa_pos_v[i], nsa_ck1[i], nsa_ck2[i], nsa_cv1[i], nsa_cv2[i],
                            mem_wk[i], mem_wv[i], w_o[i])
        x = layer_norm(ALPHA * x + y, ln1_g[i], ln1_b[i])
        if i % 2 == 0:
            f = swiglu(x, ffn_w_gate[i // 2], ffn_w_up[i // 2], ffn_w_down[i // 2])
        else:
            f = moe_swiglu(x, moe_router[i // 2], moe_w_gate[i // 2], moe_w_up[i // 2], moe_w_down[i // 2])
        x = layer_norm(ALPHA * x + f, ln2_g[i], ln2_b[i])
    return x
```

```python
from contextlib import ExitStack
import numpy as np
import concourse.bass as bass
import concourse.mybir as mybir
from concourse.bass_utils import run_bass_kernel_spmd

F32 = mybir.dt.float32
BF16 = mybir.dt.bfloat16
I32 = mybir.dt.int32
U32 = mybir.dt.uint32
ALU = mybir.AluOpType
AF = mybir.ActivationFunctionType
AX = mybir.AxisListType

N_DMA_SEMS = 48


class Res:
    __slots__ = ("name", "w", "r")

    def __init__(self, name):
        self.name = name
        self.w = None
        self.r = {}


class Eng:
    def __init__(self, name, be, sem):
        self.name = name
        self.be = be
        self.sem = sem
        self.count = 0
        self.waited = {}


class Prog:
    def __init__(self):
        self.nc = bass.Bass("TRN2", target_bir_lowering=False)
        self.es = ExitStack()
        self.cur = self.es
        nc = self.nc
        self.sems = {}
        self.engs = {}
        for name, be in (("pe", nc.tensor), ("dve", nc.vector), ("act", nc.scalar),
                         ("pool", nc.gpsimd), ("sp", nc.sync)):
            sem = self.es.enter_context(nc.semaphore("s_" + name))
            self.sems["s_" + name] = sem
            self.engs[name] = Eng(name, be, sem)
        self.dma_sems = []
        self.dma_q = {}
        for qn, cnt in (("sp", 24), ("pool", 12), ("act", 8)):
            lst = []
            for i in range(cnt):
                k = "d%s%d" % (qn, i)
                self.sems[k] = self.es.enter_context(nc.semaphore(k))
                slot = [k, 0]
                self.dma_sems.append(slot)
                lst.append(slot)
            self.dma_q[qn] = [lst, 0]
        self.n_inst = 0
        self.n_wait = 0
        self._uid = 0

    def sb(self, shape, dt, name=None):
        self._uid += 1
        name = "sb_%s_%d" % (name or "t", self._uid)
        t = self.cur.enter_context(self.nc.sbuf_tensor(name, list(shape), dt))
        return t, Res(name)

    def ps(self, shape, dt, name=None):
        self._uid += 1
        name = "ps_%s_%d" % (name or "p", self._uid)
        t = self.cur.enter_context(self.nc.psum_tensor(name, list(shape), dt))
        return t, Res(name)

    def barrier(self):
        evs = [("s_" + n, en.count) for n, en in self.engs.items() if en.count]
        evs += [(k, v) for k, v in self.dma_sems if v]
        for e in self.engs.values():
            for ev in evs:
                self._wait(e, ev)

    def scope(self):
        prog = self

        class _S:
            def __enter__(s):
                s.prev = prog.cur
                prog.cur = ExitStack()
                return s

            def __exit__(s, *a):
                prog.barrier()
                prog.cur.close()
                prog.cur = s.prev
                return False
        return _S()

    def dram(self, name, shape, dt, kind="Internal"):
        t = self.nc.dram_tensor(name, list(shape), dt, kind=kind)
        return t, Res(name)

    def _wait(self, e, ev):
        if ev is None:
            return
        k, v = ev
        if e.waited.get(k, 0) >= v:
            return
        e.be.wait_ge(self.sems[k], v)
        e.waited[k] = v
        self.n_wait += 1

    def _deps(self, e, reads, writes):
        for r in reads:
            self._wait(e, r.w)
        for r in writes:
            self._wait(e, r.w)
            for k, v in r.r.items():
                self._wait(e, (k, v))

    def _mark(self, ev, reads, writes):
        k, v = ev
        for r in reads:
            if r.r.get(k, 0) < v:
                r.r[k] = v
        for r in writes:
            r.w = ev
            r.r = {}

    def op(self, eng, fn, reads=(), writes=()):
        e = self.engs[eng]
        self._deps(e, reads, writes)
        ins = fn(e.be)
        e.count += 1
        ins.then_inc(e.sem, 1)
        ev = ("s_" + eng, e.count)
        self._mark(ev, reads, writes)
        self.n_inst += 1
        return ev

    def op_nosync(self, eng, fn):
        e = self.engs[eng]
        ins = fn(e.be)
        self.n_inst += 1
        return ins

    def mm_group(self, out_res, mms, reads, eng="pe"):
        n = len(mms)
        for i, fn in enumerate(mms):
            f = (lambda e, fn=fn, i=i: fn(e, i == 0, i == n - 1))
            if i == 0:
                ev = self.op(eng, f, reads=reads, writes=[out_res])
            elif i == n - 1:
                ev = self.op(eng, f, reads=reads, writes=[])
                out_res.w = ev
            else:
                self.op_nosync(eng, f)
        return ev

    def dma(self, eng, out, in_, reads=(), writes=(), **kw):
        e = self.engs[eng]
        self._deps(e, reads, writes)
        q = self.dma_q[eng]
        slot = q[0][q[1] % len(q[0])]
        q[1] += 1
        k, v = slot
        if v:
            self._wait(e, (k, v))
        ins = e.be.dma_start(out=out, in_=in_, **kw)
        slot[1] = v + 16
        ins.then_inc(self.sems[k], 16)
        ev = (k, v + 16)
        self._mark(ev, reads, writes)
        self.n_inst += 1
        return ev

    def finish(self, final_res=()):
        e = self.engs["sp"]
        for k, v in self.dma_sems:
            if v:
                self._wait(e, (k, v))
        for r in final_res:
            self._wait(e, r.w)
        for name, en in self.engs.items():
            if en.count:
                self._wait(e, ("s_" + name, en.count))
        self.es.close()
        return self.nc


import numpy as np, ml_dtypes
BF = ml_dtypes.bfloat16
BIG = 30000.0
L = 16384; TOK = 2048; INW = 3852

def consts():
    c = {}
    c["ident"] = np.eye(128, dtype=np.float32).astype(BF)
    x = np.arange(8192)
    c["eall"] = (x[None, :] // 64 == np.arange(128)[:, None]).astype(np.float32).astype(BF)
    j = np.arange(4)[:, None, None]; p = np.arange(128)[None, :, None]; q = np.arange(512)[None, None, :]
    key = 128 * j + p
    c["dmoba"] = np.where((key // 256 == q // 256) & (key <= q), 0.0, -BIG).astype(np.float32).astype(BF)
    return c

def core_consts(c):
    d = {}
    pos = 2048 * c + np.arange(2048)
    own = pos // 256
    d["gmask"] = np.where(np.arange(64)[None, :] < own[:, None], 0.0, -1e30).astype(np.float32)
    return d

def prep_B_moba_cross(hq, c):
    sl = slice(2048 * c, 2048 * (c + 1))
    d = {}
    mq = hq[sl, 512:1024]; mk = hq[:, 1024:1536]; mv = hq[:, 1536:2048]
    d["mqT"] = np.ascontiguousarray(mq.reshape(2048, 4, 128).transpose(1, 2, 0))
    d["mkT"] = np.ascontiguousarray(mk.reshape(L, 4, 128).transpose(1, 2, 0))
    d["mv"] = np.ascontiguousarray(mv.reshape(128, 128, 4, 128).transpose(2, 1, 0, 3))
    d["mkTl"] = np.ascontiguousarray(d["mkT"][:, :, sl])
    d["mvl"] = np.ascontiguousarray(d["mv"][:, :, 16 * c:16 * (c + 1), :])
    d["xqT"] = np.ascontiguousarray(hq[sl, 3340:3852].reshape(2048, 4, 128).transpose(1, 2, 0))
    return d


def consts_nsa():
    c = {}
    p = np.arange(128)[:, None, None]; ct = np.arange(8)[None, :, None]; s = np.arange(256)[None, None, :]
    cc = 128 * ct + p
    c["amat"] = ((cc >= 4 * s - 1) & (cc <= 4 * s + 3) & (cc <= 1022)).astype(np.float32).astype(BF)
    p = np.arange(128)[:, None]; q = np.tile(np.arange(128), 4)[None, :]
    c["v16"] = (16.0 * p - q).astype(np.float32)
    c["dsel"] = np.where((p // 64 == q // 64) & (p <= q), 0.0, -BIG).astype(np.float32).astype(BF)
    return c

def core_consts_nsa(c):
    d = {}
    pos = 2048 * c + np.arange(2048)
    own = pos // 64
    s = np.arange(256)[None, :]
    d["addm"] = np.where(s < own[:, None], 0.0, np.where(s == own[:, None], 1e9, -1e30)).astype(np.float32)
    thr = np.zeros((128, 128), np.float32)
    for t in range(16):
        for ct in range(8):
            thr[:, t * 8 + ct] = 2048 * c + 128 * t - 2048 * ct - 31
    d["cthr"] = thr
    wm = np.zeros((5, 128, 5, 512), np.float32)
    p = np.arange(128)[:, None, None]; j = np.arange(5)[None, :, None]; q = np.tile(np.arange(128), 4)[None, None, :]
    kp = (j - 4) * 128 + p
    for m in range(5):
        ok = (kp <= q) & (kp > q - 512)
        if m < 4:
            ok = ok & (2048 * c + 128 * m + kp >= 0)
        wm[m] = np.where(ok, 0.0, -BIG)
    d["wmask"] = wm.astype(BF)
    return d

def prep_B_nsa(hq, c):
    sl = slice(2048 * c, 2048 * (c + 1))
    d = {}
    d["nqT"] = np.ascontiguousarray(hq[sl, 2048:2560].reshape(16, 128, 4, 128).transpose(3, 0, 2, 1).reshape(128, 16, 512))
    d["nqrT"] = np.ascontiguousarray(hq[sl, 3852:4364].reshape(16, 128, 4, 128).transpose(3, 0, 2, 1).reshape(128, 16, 512))
    d["kcmpT"] = np.ascontiguousarray(hq[:, 2560:2688].T)
    d["vcmpT"] = np.ascontiguousarray(hq[:, 2688:2816].T)
    d["kselT"] = np.ascontiguousarray(hq[:, 2816:2944].T)
    d["vsel"] = np.ascontiguousarray(hq[:, 2944:3072].reshape(128, 128, 128).transpose(1, 0, 2))
    d["kselTl"] = np.ascontiguousarray(d["kselT"][:, sl])
    d["vsell"] = np.ascontiguousarray(d["vsel"][:, 16 * c:16 * (c + 1), :])
    kw = np.zeros((2560, 128), hq.dtype); vw = np.zeros((2560, 128), hq.dtype)
    lo = 2048 * c - 512
    src = slice(max(lo, 0), 2048 * (c + 1))
    kw[max(0, -lo):] = hq[src, 3072:3200]; vw[max(0, -lo):] = hq[src, 3200:3328]
    d["kwinT"] = np.ascontiguousarray(kw.T)
    d["vwin"] = np.ascontiguousarray(vw.reshape(20, 128, 128).transpose(1, 0, 2))
    d["gates"] = np.ascontiguousarray(hq[sl, 3328:3340])
    return d

def nsa_weights(dd, i):
    return {"ck1": dd["nsa_ck1"][i], "cv1": dd["nsa_cv1"][i], "ck2": dd["nsa_ck2"][i], "cv2": dd["nsa_cv2"][i],
            "poskT": np.ascontiguousarray(dd["nsa_pos_k"][i].T), "posvT": np.ascontiguousarray(dd["nsa_pos_v"][i].T)}


def prep_s5(dd, i, c, u_all):
    d = {}
    g0 = 4 * c
    are = dd["ssm_a_re"][i][g0:g0 + 4]; aim = dd["ssm_a_im"][i][g0:g0 + 4]; ldt = dd["ssm_log_dt"][i][g0:g0 + 4]
    col = np.zeros((128, 2, 3), np.float32); row = np.zeros((32, 3, 256), np.float32)
    for pr in range(2):
        for g2 in range(2):
            g = 2 * pr + g2
            col[g2 * 64:(g2 + 1) * 64, pr, 0] = are[g]; col[g2 * 64:(g2 + 1) * 64, pr, 1] = aim[g]; col[g2 * 64:(g2 + 1) * 64, pr, 2] = ldt[g]
            row[:, 0, pr * 128 + g2 * 64: pr * 128 + (g2 + 1) * 64] = are[g][None]
            row[:, 1, pr * 128 + g2 * 64: pr * 128 + (g2 + 1) * 64] = aim[g][None]
            row[:, 2, pr * 128 + g2 * 64: pr * 128 + (g2 + 1) * 64] = ldt[g]
    d["s5col"] = col; d["s5row"] = row
    bre = np.zeros((32, 256), np.float32); bim = np.zeros((32, 256), np.float32)
    cre = np.zeros((128, 2, 32), np.float32); cim = np.zeros((128, 2, 32), np.float32)
    dv = np.zeros((32, 2), np.float32)
    for pr in range(2):
        for g2 in range(2):
            g = g0 + 2 * pr + g2
            bre[g2 * 16:(g2 + 1) * 16, pr * 128 + g2 * 64: pr * 128 + (g2 + 1) * 64] = dd["ssm_b_re"][i][g].T
            bim[g2 * 16:(g2 + 1) * 16, pr * 128 + g2 * 64: pr * 128 + (g2 + 1) * 64] = dd["ssm_b_im"][i][g].T
            cre[g2 * 64:(g2 + 1) * 64, pr, g2 * 16:(g2 + 1) * 16] = dd["ssm_c_re"][i][g].T
            cim[g2 * 64:(g2 + 1) * 64, pr, g2 * 16:(g2 + 1) * 16] = dd["ssm_c_im"][i][g].T
            dv[g2 * 16:(g2 + 1) * 16, pr] = dd["ssm_d"][i][g * 16:(g + 1) * 16]
    d["bTre"] = bre; d["bTim"] = bim; d["cTre"] = cre; d["cTim"] = cim; d["dvec"] = dv
    d["kk"] = np.tile(np.arange(1025, dtype=np.float32)[None], (128, 1))
    d["uT"] = np.ascontiguousarray(u_all[:, 64 * c:64 * (c + 1)].T.reshape(2, 32, L))
    return d


D = 2048
TOK = 2048
NT = TOK // 128
INW = 3852
HQW = INW + 512
LN_EPS = 1e-5


def layer_norm_tiles(P, xin_d, n_add, lng_d, lnb_d, ident_d, xres_d, xres_r, xT, xT_r, want_T, NT, row0=0):
    nc = P.nc
    g_t, g_r = P.sb([128, D], F32, "ln_g")
    b_t, b_r = P.sb([128, D], F32, "ln_b")
    P.dma("sp", g_t[:], lng_d.ap()[0:1, :].to_broadcast([128, D]), writes=[g_r])
    P.dma("sp", b_t[:], lnb_d.ap()[0:1, :].to_broadcast([128, D]), writes=[b_r])
    eps_t, eps_r = P.sb([128, 1], F32, "eps")
    P.op("pool", lambda e: e.memset(eps_t[:], LN_EPS), writes=[eps_r])
    if want_T:
        id_t, id_r = P.sb([128, 128], BF16, "ident")
        P.dma("sp", id_t[:], ident_d.ap(), writes=[id_r])
    xb = [P.sb([128, D], F32, "xa%d" % i) for i in range(3)]
    xacc = [P.sb([128, D], F32, "xacc%d" % i) for i in range(2)] if n_add > 1 else None
    xo = [P.sb([128, D], F32, "xo%d" % i) for i in range(2)]
    xbf = [P.sb([128, D], BF16, "xbf%d" % i) for i in range(2)] if want_T else None
    st = [P.sb([128, 4, 6], F32, "bst%d" % i) for i in range(2)]
    mv = [P.sb([128, 2], F32, "mv%d" % i) for i in range(2)]
    sd = [P.sb([128, 1], F32, "sd%d" % i) for i in range(2)]
    tp = [P.ps([128, 4, 128], BF16, "tp%d" % i) for i in range(2)] if want_T else None
    ntp = 0
    items = [(t, a) for t in range(NT) for a in range(n_add)]

    def load(i):
        t, a = items[i]
        q_t, q_r = xb[i % 3]
        P.dma("sp", q_t[:], xin_d.ap()[a, row0 + t * 128:row0 + (t + 1) * 128, :], writes=[q_r])
    load(0)
    for i, (t, a) in enumerate(items):
        if i + 1 < len(items):
            load(i + 1)
        q_t, q_r = xb[i % 3]
        if n_add == 1:
            x_t, x_r = q_t, q_r
        else:
            x_t, x_r = xacc[t % 2]
            if a == 0:
                P.op("pool", lambda e: e.tensor_copy(out=x_t[:], in_=q_t[:]), reads=[q_r], writes=[x_r])
            else:
                eng = "dve" if a % 2 else "pool"
                P.op(eng, lambda e: e.tensor_tensor(out=x_t[:], in0=x_t[:], in1=q_t[:], op=ALU.add),
                     reads=[q_r, x_r], writes=[x_r])
            if a < n_add - 1:
                continue
        s_t, s_r = st[t % 2]
        for c in range(4):
            P.op("dve", lambda e: e.bn_stats(out=s_t[:, c, :], in_=x_t[:, c * 512:(c + 1) * 512]),
                 reads=[x_r], writes=[s_r])
        m_t, m_r = mv[t % 2]
        P.op("dve", lambda e: e.bn_aggr(out=m_t[:], in_=s_t[:].rearrange("p a b -> p (a b)")), reads=[s_r], writes=[m_r])
        d_t, d_r = sd[t % 2]
        P.op("act", lambda e: e.activation(out=d_t[:], in_=m_t[:, 1:2], func=AF.Sqrt, bias=eps_t[:], scale=1.0),
             reads=[m_r, eps_r], writes=[d_r])
        P.op("dve", lambda e: e.reciprocal(out=d_t[:], in_=d_t[:]), reads=[d_r], writes=[d_r])
        o_t, o_r = xo[t % 2]
        P.op("dve", lambda e: e.tensor_scalar(out=o_t[:], in0=x_t[:], scalar1=m_t[:, 0:1], scalar2=d_t[:, 0:1],
                                              op0=ALU.subtract, op1=ALU.mult), reads=[x_r, m_r, d_r], writes=[o_r])
        P.op("pool", lambda e: e.tensor_tensor(out=o_t[:], in0=o_t[:], in1=g_t[:], op=ALU.mult),
             reads=[o_r, g_r], writes=[o_r])
        P.op("dve", lambda e: e.tensor_tensor(out=o_t[:], in0=o_t[:], in1=b_t[:], op=ALU.add),
             reads=[o_r, b_r], writes=[o_r])
        P.dma("sp", xres_d.ap()[row0 + t * 128:row0 + (t + 1) * 128, :], o_t[:], reads=[o_r])
        if want_T:
            f_t, f_r = xbf[t % 2]
            P.op("act", lambda e: e.activation(out=f_t[:], in_=o_t[:], func=AF.Copy), reads=[o_r], writes=[f_r])
            for j in range(4):
                p_t, p_r = tp[ntp % 2]
                ntp += 1
                for k in range(4):
                    c = 4 * j + k
                    P.op("pe", lambda e: e.transpose(out=p_t[:, k, :], in_=f_t[:, c * 128:(c + 1) * 128], identity=id_t[:]),
                         reads=[f_r, id_r], writes=[p_r] if k == 0 else [])
                p_r.w = ("s_pe", P.engs["pe"].count)
                eng = "dve" if j % 2 == 0 else "act"
                if eng == "dve":
                    P.op("dve", lambda e: e.tensor_copy(out=xT[:, 4 * j:4 * j + 4, t * 128:(t + 1) * 128], in_=p_t[:]),
                         reads=[p_r], writes=[xT_r])
                else:
                    P.op("act", lambda e: e.activation(out=xT[:, 4 * j:4 * j + 4, t * 128:(t + 1) * 128], in_=p_t[:], func=AF.Copy),
                         reads=[p_r], writes=[xT_r])


ROPE_SLOTS = {1: [0, 1, 2, 3], 2: [0, 1, 2, 3], 5: [2], 6: [0]}


def rope_inplace(P, s_t, s_r, slots, cs_t, sn_t, cs_r, t, tmp):
    (ta, ra), (tb, rb) = tmp
    for s in slots:
        b = s * 128
        t1 = s_t[:, b:b + 16]
        t2 = s_t[:, b + 16:b + 32]
        c = cs_t[:, t, :]
        sn = sn_t[:, t, :]
        P.op("dve", lambda e: e.tensor_tensor(out=ta[:, 0:16], in0=t1, in1=c, op=ALU.mult), reads=[s_r, cs_r], writes=[ra])
        P.op("dve", lambda e: e.tensor_tensor(out=ta[:, 16:32], in0=t1, in1=sn, op=ALU.mult), reads=[s_r, cs_r], writes=[ra])
        P.op("dve", lambda e: e.tensor_tensor(out=tb[:, 0:16], in0=t2, in1=sn, op=ALU.mult), reads=[s_r, cs_r], writes=[rb])
        P.op("dve", lambda e: e.tensor_tensor(out=tb[:, 16:32], in0=t2, in1=c, op=ALU.mult), reads=[s_r, cs_r], writes=[rb])
        P.op("dve", lambda e: e.tensor_tensor(out=t1, in0=ta[:, 0:16], in1=tb[:, 0:16], op=ALU.subtract),
             reads=[ra, rb], writes=[s_r])
        P.op("dve", lambda e: e.tensor_tensor(out=t2, in0=ta[:, 16:32], in1=tb[:, 16:32], op=ALU.add),
             reads=[ra, rb], writes=[s_r])


def build_A(n_add, do_proj, TOK=2048):
    GTA = 2048
    NT = GTA // 128
    P = Prog()
    nc = P.nc
    xin_d, _ = P.dram("xin", [n_add, TOK, D], F32, kind="ExternalInput")
    lng_d, _ = P.dram("lng", [1, D], F32, kind="ExternalInput")
    lnb_d, _ = P.dram("lnb", [1, D], F32, kind="ExternalInput")
    xres_d, xres_r = P.dram("xres", [TOK, D], F32, kind="ExternalOutput")
    ident_d = None
    if do_proj:
        ident_d, _ = P.dram("ident", [128, 128], BF16, kind="ExternalInput")
        w_d, _ = P.dram("w", [D, INW], F32, kind="ExternalInput")
        cos_d, _ = P.dram("cos", [TOK, 16], F32, kind="ExternalInput")
        sin_d, _ = P.dram("sin", [TOK, 16], F32, kind="ExternalInput")
        hq_d, hq_r = P.dram("hq", [TOK, HQW], BF16, kind="ExternalOutput")
        u_d, u_r = P.dram("u", [TOK, 512], F32, kind="ExternalOutput")
    for grp in range(TOK // GTA):
        row0 = grp * GTA
        with P.scope():
            xT = xT_r = None
            if do_proj:
                xT, xT_r = P.sb([128, 16, GTA], BF16, "xT")
            with P.scope():
                layer_norm_tiles(P, xin_d, n_add, lng_d, lnb_d, ident_d, xres_d, xres_r, xT, xT_r, do_proj, NT, row0)
            if do_proj:
                cs_t, cs_r = P.sb([128, NT, 16], F32, "cos")
                sn_t, sn_r = P.sb([128, NT, 16], F32, "sin")
                P.dma("sp", cs_t[:], cos_d.ap()[row0:row0 + GTA, :].rearrange("(t p) f -> p t f", p=128), writes=[cs_r])
                P.dma("sp", sn_t[:], sin_d.ap()[row0:row0 + GTA, :].rearrange("(t p) f -> p t f", p=128), writes=[cs_r])
                wv = w_d.ap().rearrange("(c p) f -> p c f", p=128)
                wt = [P.sb([128, 16, 512], BF16, "wt%d" % i) for i in range(2)]
                acc = [P.ps([128, 512], F32, "acc%d" % i) for i in range(3)]
                stg = [P.sb([128, 512], F32, "stg%d" % i) for i in range(3)]
                ob = [P.sb([128, 512], BF16, "ob%d" % i) for i in range(3)]
                tmp = (P.sb([128, 32], F32, "rta"), P.sb([128, 32], F32, "rtb"))
                n = 0

                def load_w(j):
                    fw_ = min(512, INW - 512 * j)
                    w_t, w_r = wt[j % 2]
                    for q in range(4):
                        P.dma("pool", w_t[:, 4 * q:4 * q + 4, 0:fw_], wv[:, 4 * q:4 * q + 4, 512 * j:512 * j + fw_], writes=[w_r])
                load_w(0)
                for j in range(8):
                    fw_ = min(512, INW - 512 * j)
                    w_t, w_r = wt[j % 2]
                    if j + 1 < 8:
                        load_w(j + 1)
                    for t in range(NT):
                        rows = slice(row0 + t * 128, row0 + (t + 1) * 128)
                        a_t, a_r = acc[n % 3]
                        s_t, s_r = stg[n % 3]
                        o_t, o_r = ob[n % 3]
                        n += 1
                        P.mm_group(a_r, [(lambda e, st_, sp_, k=k: e.matmul(a_t[:, 0:fw_], lhsT=xT[:, k, t * 128:(t + 1) * 128],
                                                                           rhs=w_t[:, k, 0:fw_], start=st_, stop=sp_))
                                         for k in range(16)], reads=[xT_r, w_r])
                        P.op("act", lambda e: e.activation(out=s_t[:, 0:fw_], in_=a_t[:, 0:fw_], func=AF.Copy), reads=[a_r], writes=[s_r])
                        if j == 6:
                            P.op("act", lambda e: e.activation(out=s_t[:, 256:268], in_=s_t[:, 256:268], func=AF.Sigmoid),
                                 reads=[s_r], writes=[s_r])
                        if j == 0:
                            P.dma("sp", u_d.ap()[rows, :], s_t[:], reads=[s_r])
                        if j == 4:
                            P.op("dve", lambda e: e.tensor_copy(out=o_t[:], in_=s_t[:]), reads=[s_r], writes=[o_r])
                            P.dma("sp", hq_d.ap()[rows, 2048:2560], o_t[:], reads=[o_r])
                            rope_inplace(P, s_t, s_r, [0, 1, 2, 3], cs_t, sn_t, cs_r, t, tmp)
                            o_t, o_r = ob[n % 3]
                            P.op("dve", lambda e: e.tensor_copy(out=o_t[:], in_=s_t[:]), reads=[s_r], writes=[o_r])
                            P.dma("sp", hq_d.ap()[rows, INW:INW + 512], o_t[:], reads=[o_r])
                            continue
                        if j in ROPE_SLOTS:
                            rope_inplace(P, s_t, s_r, ROPE_SLOTS[j], cs_t, sn_t, cs_r, t, tmp)
                        P.op("dve", lambda e: e.tensor_copy(out=o_t[:, 0:fw_], in_=s_t[:, 0:fw_]), reads=[s_r], writes=[o_r])
                        P.dma("sp", hq_d.ap()[rows, 512 * j:512 * j + fw_], o_t[:, 0:fw_], reads=[o_r])
    P.finish()
    return P


SCALE = 128 ** -0.5
BIG = 30000.0
L = 16384
TOK = 2048
NKT = L // 128
TS5 = 1024


class Ctx:
    pass


def attn_ctx(P):
    C = Ctx()
    C.sp = [P.ps([128, 512], F32, "sp%d" % i) for i in range(2)]
    C.ob = [P.ps([128, 512], F32, "ob%d" % i) for i in range(4)]
    C.mp = P.ps([128, 512], F32, "mp")
    C.tpb = P.ps([128, 2, 128], BF16, "tpb")
    C.pt = [P.sb([128, 512], BF16, "pt%d" % i) for i in range(2)]
    C.den = P.sb([128, 8], F32, "den")
    C.nsp = 0
    C.npt = 0
    return C


def attn_run(P, C, qT, q_reads, steps, ncols):
    n = len(steps)

    def emit_qk(i):
        kT, extras, v, rd = steps[i][:4]
        if len(steps[i]) > 4:
            steps[i][4]()
        s_t, s_r = C.sp[C.nsp % 2]
        C.nsp += 1
        mms = [lambda e, st, sp_: e.matmul(s_t[:], lhsT=kT, rhs=qT, start=st, stop=sp_)]
        for (l, r) in extras:
            mms.append(lambda e, st, sp_, l=l, r=r: e.matmul(s_t[:], lhsT=l, rhs=r, start=st, stop=sp_))
        P.mm_group(s_r, mms, reads=list(q_reads) + list(rd))
        return s_t, s_r

    cur = emit_qk(0)
    for i in range(n):
        nxt = emit_qk(i + 1) if i + 1 < n else None
        s_t, s_r = cur
        p_t, p_r = C.pt[C.npt % 2]
        C.npt += 1
        P.op("act", lambda e: e.activation(out=p_t[:], in_=s_t[:], func=AF.Exp, scale=SCALE), reads=[s_r], writes=[p_r])
        kT, extras, v, rd = steps[i][:4]
        for r in range(4):
            o_t, o_r = C.ob[r]
            vr = v[r] if isinstance(v, list) else v
            fn = lambda e: e.matmul(o_t[:, 0:ncols], lhsT=p_t[:, 128 * r:128 * (r + 1)], rhs=vr, start=(i == 0), stop=(i == n - 1))
            if i == 0:
                P.op("pe", fn, reads=[p_r] + list(rd), writes=[o_r])
            else:
                ev = P.op("pe", fn, reads=[p_r] + list(rd), writes=[])
                if i == n - 1:
                    o_r.w = ev
        cur = nxt


def evac_den(P, C, r, col):
    o_t, o_r = C.ob[r]
    d_t, d_r = C.den
    P.op("dve", lambda e: e.tensor_scalar(out=d_t[:, r:r + 1], in0=o_t[:, col:col + 1], scalar1=1e-30, scalar2=None, op0=ALU.max),
         reads=[o_r], writes=[d_r])
    P.op("dve", lambda e: e.reciprocal(out=d_t[:, r:r + 1], in_=d_t[:, r:r + 1]), reads=[d_r], writes=[d_r])


def load_consts(P, C, d):
    C.ident = P.sb([128, 128], BF16, "identb")
    P.dma("sp", C.ident[0][:], d["ident"].ap(), writes=[C.ident[1]])
    C.eall = P.sb([128, 8192], BF16, "eall")
    P.dma("sp", C.eall[0][:], d["eall"].ap(), writes=[C.eall[1]])


def phase_cross(P, C, d, yx):
    with P.scope():
        memT = P.sb([128, 16, 256], BF16, "memT")
        wk = P.sb([128, 16, 512], BF16, "wk")
        wv = P.sb([128, 16, 512], BF16, "wv")
        for q in range(4):
            P.dma("pool", memT[0][:, 4 * q:4 * q + 4, :], d["memT"].ap().rearrange("(c p) m -> p c m", p=128)[:, 4 * q:4 * q + 4, :], writes=[memT[1]])
            P.dma("pool", wk[0][:, 4 * q:4 * q + 4, :], d["wk"].ap().rearrange("(c p) f -> p c f", p=128)[:, 4 * q:4 * q + 4, :], writes=[wk[1]])
            P.dma("pool", wv[0][:, 4 * q:4 * q + 4, :], d["wv"].ap().rearrange("(c p) f -> p c f", p=128)[:, 4 * q:4 * q + 4, :], writes=[wv[1]])
        kT = P.sb([128, 4, 256], BF16, "memkT")
        vv = P.sb([128, 2, 4, 129], BF16, "memv")
        P.op("pool", lambda e: e.memset(vv[0][:], 1.0), writes=[vv[1]])
        m_t, m_r = C.mp
        for h in range(4):
            P.mm_group(m_r, [(lambda e, st, sp_, c=c: e.matmul(m_t[:, 0:256], lhsT=wk[0][:, c, h * 128:(h + 1) * 128], rhs=memT[0][:, c, :],
                                                             start=st, stop=sp_)) for c in range(16)], reads=[wk[1], memT[1]])
            P.op("dve", lambda e: e.tensor_copy(out=kT[0][:, h, :], in_=m_t[:, 0:256]), reads=[m_r], writes=[kT[1]])
        for mt in range(2):
            P.mm_group(m_r, [(lambda e, st, sp_, c=c: e.matmul(m_t[:], lhsT=memT[0][:, c, mt * 128:(mt + 1) * 128], rhs=wv[0][:, c, :],
                                                             start=st, stop=sp_)) for c in range(16)], reads=[wv[1], memT[1]])
            P.op("dve", lambda e: e.tensor_copy(out=vv[0][:, mt, :, 0:128], in_=m_t[:].rearrange("p (h d) -> p h d", d=128)),
                 reads=[m_r], writes=[vv[1]])
        qs = [P.sb([128, TOK], BF16, "xq%d" % i) for i in range(2)]
        for h in range(4):
            q_t, q_r = qs[h % 2]
            P.dma("sp", q_t[:], d["xqT"].ap()[h], writes=[q_r])
            for a in range(4):
                steps = [(kT[0][:, h, mt * 128:(mt + 1) * 128], [], vv[0][:, mt, h, :], [kT[1], vv[1]]) for mt in range(2)]
                attn_run(P, C, q_t[:, a * 512:(a + 1) * 512], [q_r], steps, 129)
                for r in range(4):
                    evac_den(P, C, r, 128)
                    o_t, o_r = C.ob[r]
                    P.op("dve", lambda e: e.tensor_scalar(out=yx[0][:, 4 * a + r, h * 128:(h + 1) * 128], in0=o_t[:, 0:128],
                                                          scalar1=C.den[0][:, r:r + 1], scalar2=None, op0=ALU.mult),
                         reads=[o_r, C.den[1]], writes=[yx[1]])


def phase_moba(P, C, d, ym):
    with P.scope():
        KT = [P.sb([128, L], BF16, "KT%d" % i) for i in range(2)]
        VB = [P.sb([128, NKT, 129], BF16, "VB%d" % i) for i in range(2)]
        for i in range(2):
            P.op("pool", lambda e: e.memset(VB[i][0][:], 1.0), writes=[VB[i][1]])
        qs = [P.sb([128, TOK], BF16, "mq%d" % i) for i in range(2)]
        kl = [P.sb([128, TOK], BF16, "mkl%d" % i) for i in range(2)]
        vl = [P.sb([128, 16, 129], BF16, "mvl%d" % i) for i in range(2)]
        for i in range(2):
            P.op("pool", lambda e: e.memset(vl[i][0][:], 1.0), writes=[vl[i][1]])
        dm = P.sb([128, 4, 512], BF16, "dmoba")
        P.dma("sp", dm[0][:], d["dmoba"].ap().rearrange("j p q -> p j q"), writes=[dm[1]])
        gm = P.sb([128, 16, 64], F32, "gmask")
        P.dma("sp", gm[0][:], d["gmask"].ap().rearrange("(t p) b -> p t b", p=128), writes=[gm[1]])
        km32 = P.sb([128, 64], F32, "km32")
        kmT = P.sb([128, 64], BF16, "kmT")
        gsm = P.sb([128, 64], F32, "gsm")
        m8 = P.sb([128, 8], F32, "m8")
        t1 = P.sb([128, 64], F32, "t1")
        sel = P.sb([128, 64], F32, "sel")
        brep = P.sb([128, 256], BF16, "brep")
        bT = [[P.sb([128, 512], BF16, "bT%d_%d" % (i, ch)) for ch in range(2)] for i in range(2)]
        for h in range(4):
            K_t, K_r = KT[h % 2]
            V_t, V_r = VB[h % 2]
            for q in range(8):
                P.dma("sp", K_t[:, q * 2048:(q + 1) * 2048], d["mkT"].ap()[h, :, q * 2048:(q + 1) * 2048], writes=[K_r])
            for q in range(4):
                P.dma("sp", V_t[:, q * 32:(q + 1) * 32, 0:128], d["mv"].ap()[h, :, q * 32:(q + 1) * 32, :], writes=[V_r])
            q_t, q_r = qs[h % 2]
            kl_t, kl_r = kl[h % 2]
            vl_t, vl_r = vl[h % 2]
            P.dma("sp", q_t[:], d["mqT"].ap()[h], writes=[q_r])
            P.dma("sp", kl_t[:], d["mkTl"].ap()[h], writes=[kl_r])
            P.dma("sp", vl_t[:, :, 0:128], d["mvl"].ap()[h], writes=[vl_r])
            P.op("dve", lambda e: e.tensor_reduce(out=km32[0][:], in_=K_t[:].rearrange("p (b k) -> p b k", k=256), axis=AX.X, op=ALU.add),
                 reads=[K_r], writes=[km32[1]])
            P.op("dve", lambda e: e.tensor_scalar(out=kmT[0][:], in0=km32[0][:], scalar1=1.0 / 256, scalar2=None, op0=ALU.mult),
                 reads=[km32[1]], writes=[kmT[1]])
            for a in range(4):
                b_ = bT[a % 2]
                for r in range(4):
                    s = 4 * a + r
                    m_t, m_r = C.mp
                    P.op("pe", lambda e: e.matmul(m_t[:, 0:64], lhsT=q_t[:, s * 128:(s + 1) * 128], rhs=kmT[0][:], start=True, stop=True),
                         reads=[q_r, kmT[1]], writes=[m_r])
                    P.op("dve", lambda e: e.tensor_tensor(out=gsm[0][:], in0=m_t[:, 0:64], in1=gm[0][:, s, :], op=ALU.add),
                         reads=[m_r, gm[1]], writes=[gsm[1]])
                    P.op("dve", lambda e: e.max(out=m8[0][:], in_=gsm[0][:]), reads=[gsm[1]], writes=[m8[1]])
                    P.op("dve", lambda e: e.tensor_scalar(out=t1[0][:], in0=gsm[0][:], scalar1=-5e29, scalar2=None, op0=ALU.is_gt),
                         reads=[gsm[1]], writes=[t1[1]])
                    P.op("dve", lambda e: e.scalar_tensor_tensor(out=sel[0][:], in0=gsm[0][:], scalar=m8[0][:, 2:3], in1=t1[0][:],
                                                                 op0=ALU.is_ge, op1=ALU.mult), reads=[gsm[1], m8[1], t1[1]], writes=[sel[1]])
                    P.op("dve", lambda e: e.tensor_scalar(out=brep[0][:].rearrange("p (b j) -> p b j", j=4),
                                                          in0=sel[0][:].unsqueeze(2).to_broadcast([128, 64, 4]),
                                                          scalar1=1.0, scalar2=BIG, op0=ALU.subtract, op1=ALU.mult),
                         reads=[sel[1]], writes=[brep[1]])
                    tp_t, tp_r = C.tpb
                    for ch in range(2):
                        P.op("pe", lambda e: e.transpose(out=tp_t[:, ch, :], in_=brep[0][:, ch * 128:(ch + 1) * 128], identity=C.ident[0][:]),
                             reads=[brep[1], C.ident[1]], writes=[tp_r] if ch == 0 else [])
                    tp_r.w = ("s_pe", P.engs["pe"].count)
                    for ch in range(2):
                        P.op("dve", lambda e: e.tensor_copy(out=b_[ch][0][:, r * 128:(r + 1) * 128], in_=tp_t[:, ch, :]),
                             reads=[tp_r], writes=[b_[ch][1]])
                steps = []
                for kt in range(NKT):
                    ch, jt = kt // 64, kt % 64
                    steps.append((K_t[:, kt * 128:(kt + 1) * 128], [(C.eall[0][:, 128 * jt:128 * jt + 128], b_[ch][0][:])],
                                  V_t[:, kt, :], [K_r, V_r, C.eall[1], b_[ch][1]]))
                for j in range(4):
                    lt = 4 * a + j
                    steps.append((kl_t[:, lt * 128:(lt + 1) * 128], [(C.ident[0][:], dm[0][:, j, :])], vl_t[:, lt, :],
                                  [kl_r, vl_r, C.ident[1], dm[1]]))
                attn_run(P, C, q_t[:, a * 512:(a + 1) * 512], [q_r], steps, 129)
                for r in range(4):
                    evac_den(P, C, r, 128)
                    o_t, o_r = C.ob[r]
                    P.op("dve", lambda e: e.tensor_scalar(out=ym[0][:, 4 * a + r, h * 128:(h + 1) * 128], in0=o_t[:, 0:128],
                                                          scalar1=C.den[0][:, r:r + 1], scalar2=None, op0=ALU.mult),
                         reads=[o_r, C.den[1]], writes=[ym[1]])


def gelu_tanh(P, src_ps, src_r, hb, hb_r, dst, dst_r, tmp, n):
    (gu, gur), (gt, gtr), (gs_, gsr) = tmp
    P.op("act", lambda e: e.activation(out=gu[:, 0:n], in_=src_ps, func=AF.Identity, bias=hb, scale=1.0), reads=[src_r, hb_r], writes=[gur])
    P.op("dve", lambda e: e.tensor_tensor(out=gt[:, 0:n], in0=gu[:, 0:n], in1=gu[:, 0:n], op=ALU.mult), reads=[gur], writes=[gtr])
    P.op("dve", lambda e: e.tensor_scalar(out=gt[:, 0:n], in0=gt[:, 0:n], scalar1=0.044715, scalar2=1.0, op0=ALU.mult, op1=ALU.add),
         reads=[gtr], writes=[gtr])
    P.op("dve", lambda e: e.tensor_tensor(out=gt[:, 0:n], in0=gt[:, 0:n], in1=gu[:, 0:n], op=ALU.mult), reads=[gtr, gur], writes=[gtr])
    P.op("act", lambda e: e.activation(out=gs_[:, 0:n], in_=gt[:, 0:n], func=AF.Sigmoid, scale=1.5957691216057308), reads=[gtr], writes=[gsr])
    P.op("dve", lambda e: e.tensor_tensor(out=dst, in0=gu[:, 0:n], in1=gs_[:, 0:n], op=ALU.mult), reads=[gur, gsr], writes=[dst_r])


def nsa_compress(P, C, d, KTsrc, w1name, w2name, posname, gel):
    K_t, K_r = KTsrc
    with P.scope():
        w1 = P.sb([128, 32, 256], BF16, "w1")
        for q in range(4):
            P.dma("pool", w1[0][:, 8 * q:8 * q + 8, :], d[w1name].ap().rearrange("(i p) f -> p i f", p=128)[:, 8 * q:8 * q + 8, :], writes=[w1[1]])
        pos32 = P.sb([128, 32], F32, "pos32")
        posb = P.sb([128, 32], BF16, "posb")
        P.dma("sp", pos32[0][:], d[posname].ap(), writes=[pos32[1]])
        P.op("dve", lambda e: e.tensor_copy(out=posb[0][:], in_=pos32[0][:]), reads=[pos32[1]], writes=[posb[1]])
        hb = P.sb([128, 2], F32, "hb")
        tmp = [P.sb([128, 512], F32, "g%d" % i) for i in range(3)]
        m_t, m_r = C.mp
        for hh in range(2):
            P.mm_group(m_r, [(lambda e, st, sp_, i=i: e.matmul(m_t[:, hh:hh + 1], lhsT=w1[0][:, i, hh * 128:(hh + 1) * 128], rhs=posb[0][:, i:i + 1],
                                                             start=st, stop=sp_)) for i in range(32)], reads=[w1[1], posb[1]])
            P.op("dve", lambda e: e.tensor_copy(out=hb[0][:, hh:hh + 1], in_=m_t[:, hh:hh + 1]), reads=[m_r], writes=[hb[1]])
        for hh in range(2):
            P.op("pool", lambda e: e.memset(gel[hh][0][:, 1016:1024], 0.0), writes=[gel[hh][1]])
            for nt in range(2):
                n = 512 if nt == 0 else 511
                o_t, o_r = C.ob[2 * hh + nt]
                base = 8192 * nt
                P.mm_group(o_r, [(lambda e, st, sp_, i=i: e.matmul(o_t[:, 0:n], lhsT=w1[0][:, i, hh * 128:(hh + 1) * 128],
                                                                 rhs=K_t[:, base + i:base + i + 16 * (n - 1) + 1:16], start=st, stop=sp_))
                                 for i in range(32)], reads=[w1[1], K_r])
                gelu_tanh(P, o_t[:, 0:n], o_r, hb[0][:, hh:hh + 1], hb[1], gel[hh][0][:, nt * 512:nt * 512 + n], gel[hh][1], tmp, n)


def phase_nsa(P, C, d, yn):
    with P.scope():
        KT = [P.sb([128, L], BF16, "KTn%d" % i) for i in range(2)]
        VB = P.sb([128, NKT, 129], BF16, "VBn")
        P.op("pool", lambda e: e.memset(VB[0][:], 1.0), writes=[VB[1]])
        for q in range(8):
            P.dma("sp", KT[0][0][:, q * 2048:(q + 1) * 2048], d["kcmpT"].ap()[:, q * 2048:(q + 1) * 2048], writes=[KT[0][1]])
            P.dma("sp", KT[1][0][:, q * 2048:(q + 1) * 2048], d["vcmpT"].ap()[:, q * 2048:(q + 1) * 2048], writes=[KT[1][1]])
        for q in range(4):
            P.dma("sp", VB[0][:, q * 32:(q + 1) * 32, 0:128], d["vsel"].ap()[:, q * 32:(q + 1) * 32, :], writes=[VB[1]])
        kcT = P.sb([128, 1024], BF16, "kcT")
        RC = P.sb([128, 8, 385], BF16, "RC")
        P.op("pool", lambda e: e.memset(RC[0][:], 1.0), writes=[RC[1]])
        P.dma("sp", RC[0][:, :, 128:384], d["amat"].ap(), writes=[RC[1]])
        with P.scope():
            w2 = P.sb([128, 2, 128], BF16, "w2")
            gel = [P.sb([128, 1024], BF16, "gel%d" % i) for i in range(2)]
            P.dma("pool", w2[0][:], d["ck2"].ap().rearrange("(h p) d -> p h d", p=128), writes=[w2[1]])
            nsa_compress(P, C, d, KT[0], "ck1", "ck2", "poskT", gel)
            for nt in range(2):
                s_t, s_r = C.sp[nt]
                P.mm_group(s_r, [(lambda e, st, sp_, hh=hh: e.matmul(s_t[:], lhsT=w2[0][:, hh, :], rhs=gel[hh][0][:, nt * 512:(nt + 1) * 512],
                                                                   start=st, stop=sp_)) for hh in range(2)], reads=[w2[1], gel[0][1], gel[1][1]])
                P.op("dve", lambda e: e.tensor_copy(out=kcT[0][:, nt * 512:(nt + 1) * 512], in_=s_t[:]), reads=[s_r], writes=[kcT[1]])
            P.dma("pool", w2[0][:], d["cv2"].ap().rearrange("(h p) d -> p h d", p=128), writes=[w2[1]])
            nsa_compress(P, C, d, KT[1], "cv1", "cv2", "posvT", gel)
            m_t, m_r = C.mp
            for ct in range(8):
                P.mm_group(m_r, [(lambda e, st, sp_, hh=hh: e.matmul(m_t[:, 0:128], lhsT=gel[hh][0][:, ct * 128:(ct + 1) * 128], rhs=w2[0][:, hh, :],
                                                                   start=st, stop=sp_)) for hh in range(2)], reads=[w2[1], gel[0][1], gel[1][1]])
                P.op("dve", lambda e: e.tensor_copy(out=RC[0][:, ct, 0:128], in_=m_t[:, 0:128]), reads=[m_r], writes=[RC[1]])
        for q in range(8):
            P.dma("sp", KT[0][0][:, q * 2048:(q + 1) * 2048], d["kselT"].ap()[:, q * 2048:(q + 1) * 2048], writes=[KT[0][1]])
        ksl = P.sb([128, TOK], BF16, "ksl")
        vsl = P.sb([128, 16, 129], BF16, "vsl")
        kw = P.sb([128, 2560], BF16, "kw")
        vw = P.sb([128, 20, 129], BF16, "vw")
        P.op("pool", lambda e: e.memset(vsl[0][:], 1.0), writes=[vsl[1]])
        P.op("pool", lambda e: e.memset(vw[0][:], 1.0), writes=[vw[1]])
        P.dma("sp", ksl[0][:], d["kselTl"].ap(), writes=[ksl[1]])
        P.dma("sp", vsl[0][:, :, 0:128], d["vsell"].ap(), writes=[vsl[1]])
        P.dma("sp", kw[0][:], d["kwinT"].ap(), writes=[kw[1]])
        P.dma("sp", vw[0][:, :, 0:128], d["vwin"].ap(), writes=[vw[1]])
        wmg = P.sb([128, 5, 512], BF16, "wmg")
        wmt = P.sb([128, 5, 512], BF16, "wmt")
        P.dma("sp", wmg[0][:], d["wmask"].ap()[4], writes=[wmg[1]])
        dsel = P.sb([128, 512], BF16, "dsel")
        P.dma("sp", dsel[0][:], d["dsel"].ap(), writes=[dsel[1]])
        v16 = P.sb([128, 512], F32, "v16")
        P.dma("sp", v16[0][:], d["v16"].ap(), writes=[v16[1]])
        thr = P.sb([128, 128], F32, "thr")
        P.dma("sp", thr[0][:], d["cthr"].ap(), writes=[thr[1]])
        gts = P.sb([128, 16, 12], BF16, "gates")
        P.dma("sp", gts[0][:], d["gates"].ap().rearrange("(t p) g -> p t g", p=128), writes=[gts[1]])
        cm = [P.sb([128, 512], BF16, "cm%d" % i) for i in range(2)]
        qa = [P.sb([128, 512], BF16, "nq%d" % i) for i in range(2)]
        qr = [P.sb([128, 512], BF16, "nqr%d" % i) for i in range(2)]
        adm = [P.sb([128, 256], F32, "adm%d" % i) for i in range(2)]
        imp = P.sb([128, 256], F32, "imp")
        impm = P.sb([128, 256], F32, "impm")
        work = P.sb([128, 256], F32, "work")
        gt = P.sb([128, 256], F32, "gt")
        sl_ = P.sb([128, 256], F32, "sl")
        no = P.sb([128, 256], F32, "no")
        m8a = P.sb([128, 8], F32, "m8a")
        m8b = P.sb([128, 8], F32, "m8b")
        brow = P.sb([128, 256], BF16, "brow")
        bTn = [P.sb([128, 4, 128], BF16, "bTn%d" % i) for i in range(2)]
        acc = P.sb([128, 4, 128], F32, "acc")
        coef = P.sb([128, 4], F32, "coef")
        ncm = [0]
        for t in range(16):
            qa_t, qa_r = qa[t % 2]
            qr_t, qr_r = qr[t % 2]
            ad_t, ad_r = adm[t % 2]
            P.dma("sp", qa_t[:], d["nqT"].ap()[:, t, :], writes=[qa_r])
            P.dma("sp", qr_t[:], d["nqrT"].ap()[:, t, :], writes=[qr_r])
            P.dma("sp", ad_t[:], d["addm"].ap()[t * 128:(t + 1) * 128, :], writes=[ad_r])
            steps = []
            for ct in range(8):
                def pre(ct=ct):
                    c_t, c_r = cm[ncm[0] % 2]
                    P.op("dve", lambda e: e.tensor_scalar(out=c_t[:], in0=v16[0][:], scalar1=thr[0][:, t * 8 + ct:t * 8 + ct + 1], scalar2=-BIG,
                                                          op0=ALU.is_gt, op1=ALU.mult), reads=[v16[1], thr[1]], writes=[c_r])
                c_t, c_r = cm[(ncm[0] + ct) % 2]
                steps.append((kcT[0][:, ct * 128:(ct + 1) * 128], [(C.ident[0][:], c_t[:])], RC[0][:, ct, :], [kcT[1], RC[1], C.ident[1], c_r],
                              (lambda pre=pre: (pre(), ncm.__setitem__(0, ncm[0] + 1)))))
            attn_run(P, C, qa_t[:], [qa_r], steps, 385)
            for h in range(4):
                evac_den(P, C, h, 384)
                o_t, o_r = C.ob[h]
                if h == 0:
                    P.op("dve", lambda e: e.tensor_scalar(out=imp[0][:], in0=o_t[:, 128:384], scalar1=C.den[0][:, 0:1], scalar2=None, op0=ALU.mult),
                         reads=[o_r, C.den[1]], writes=[imp[1]])
                else:
                    P.op("dve", lambda e: e.scalar_tensor_tensor(out=imp[0][:], in0=o_t[:, 128:384], scalar=C.den[0][:, h:h + 1], in1=imp[0][:],
                                                                 op0=ALU.mult, op1=ALU.add), reads=[o_r, C.den[1], imp[1]], writes=[imp[1]])
                P.op("dve", lambda e: e.tensor_tensor(out=coef[0][:, h:h + 1], in0=C.den[0][:, h:h + 1], in1=gts[0][:, t, 3 * h:3 * h + 1], op=ALU.mult),
                     reads=[C.den[1], gts[1]], writes=[coef[1]])
                P.op("dve", lambda e: e.tensor_scalar(out=acc[0][:, h, :], in0=o_t[:, 0:128], scalar1=coef[0][:, h:h + 1], scalar2=None, op0=ALU.mult),
                     reads=[o_r, coef[1]], writes=[acc[1]])
            P.op("dve", lambda e: e.tensor_tensor(out=impm[0][:], in0=imp[0][:], in1=ad_t[:], op=ALU.add), reads=[imp[1], ad_r], writes=[impm[1]])
            P.op("dve", lambda e: e.max(out=m8a[0][:], in_=impm[0][:]), reads=[impm[1]], writes=[m8a[1]])
            P.op("dve", lambda e: e.match_replace(out=work[0][:], in_to_replace=m8a[0][:], in_values=impm[0][:], imm_value=-1e38),
                 reads=[m8a[1], impm[1]], writes=[work[1]])
            P.op("dve", lambda e: e.max(out=m8b[0][:], in_=work[0][:]), reads=[work[1]], writes=[m8b[1]])
            P.op("dve", lambda e: e.tensor_scalar(out=gt[0][:], in0=impm[0][:], scalar1=-5e29, scalar2=None, op0=ALU.is_gt),
                 reads=[impm[1]], writes=[gt[1]])
            P.op("dve", lambda e: e.scalar_tensor_tensor(out=sl_[0][:], in0=impm[0][:], scalar=m8b[0][:, 7:8], in1=gt[0][:],
                                                         op0=ALU.is_ge, op1=ALU.mult), reads=[impm[1], m8b[1], gt[1]], writes=[sl_[1]])
            P.op("dve", lambda e: e.tensor_scalar(out=no[0][:], in0=ad_t[:], scalar1=5e8, scalar2=None, op0=ALU.is_lt), reads=[ad_r], writes=[no[1]])
            P.op("dve", lambda e: e.tensor_tensor(out=sl_[0][:], in0=sl_[0][:], in1=no[0][:], op=ALU.mult), reads=[sl_[1], no[1]], writes=[sl_[1]])
            P.op("dve", lambda e: e.tensor_scalar(out=brow[0][:], in0=sl_[0][:], scalar1=1.0, scalar2=BIG, op0=ALU.subtract, op1=ALU.mult),
                 reads=[sl_[1]], writes=[brow[1]])
            tp_t, tp_r = C.tpb
            for ch in range(2):
                P.op("pe", lambda e: e.transpose(out=tp_t[:, ch, :], in_=brow[0][:, ch * 128:(ch + 1) * 128], identity=C.ident[0][:]),
                     reads=[brow[1], C.ident[1]], writes=[tp_r] if ch == 0 else [])
            tp_r.w = ("s_pe", P.engs["pe"].count)
            for ch in range(2):
                P.op("dve", lambda e: e.tensor_copy(out=bTn[ch][0][:], in_=tp_t[:, ch, :].unsqueeze(1).to_broadcast([128, 4, 128])),
                     reads=[tp_r], writes=[bTn[ch][1]])
            if t < 4:
                P.dma("sp", wmt[0][:], d["wmask"].ap()[t], writes=[wmt[1]])
                wm = wmt
            else:
                wm = wmg
            steps = [(kw[0][:, (t + j) * 128:(t + j + 1) * 128], [(C.ident[0][:], wm[0][:, j, :])], vw[0][:, t + j, :],
                      [kw[1], vw[1], C.ident[1], wm[1]]) for j in range(5)]
            attn_run(P, C, qr_t[:], [qr_r], steps, 129)
            for h in range(4):
                evac_den(P, C, h, 128)
                o_t, o_r = C.ob[h]
                P.op("dve", lambda e: e.tensor_tensor(out=coef[0][:, h:h + 1], in0=C.den[0][:, h:h + 1], in1=gts[0][:, t, 3 * h + 2:3 * h + 3], op=ALU.mult),
                     reads=[C.den[1], gts[1]], writes=[coef[1]])
                P.op("dve", lambda e: e.scalar_tensor_tensor(out=acc[0][:, h, :], in0=o_t[:, 0:128], scalar=coef[0][:, h:h + 1], in1=acc[0][:, h, :],
                                                             op0=ALU.mult, op1=ALU.add), reads=[o_r, coef[1], acc[1]], writes=[acc[1]])
            steps = []
            for kt in range(NKT):
                ch, jt = kt // 64, kt % 64
                steps.append((KT[0][0][:, kt * 128:(kt + 1) * 128], [(C.eall[0][:, 128 * jt:128 * jt + 128], bTn[ch][0][:].rearrange("p h q -> p (h q)"))],
                              VB[0][:, kt, :], [KT[0][1], VB[1], C.eall[1], bTn[ch][1]]))
            steps.append((ksl[0][:, t * 128:(t + 1) * 128], [(C.ident[0][:], dsel[0][:])], vsl[0][:, t, :], [ksl[1], vsl[1], C.ident[1], dsel[1]]))
            attn_run(P, C, qr_t[:], [qr_r], steps, 129)
            for h in range(4):
                evac_den(P, C, h, 128)
                o_t, o_r = C.ob[h]
                P.op("dve", lambda e: e.tensor_tensor(out=coef[0][:, h:h + 1], in0=C.den[0][:, h:h + 1], in1=gts[0][:, t, 3 * h + 1:3 * h + 2], op=ALU.mult),
                     reads=[C.den[1], gts[1]], writes=[coef[1]])
                P.op("dve", lambda e: e.scalar_tensor_tensor(out=yn[0][:, t, h * 128:(h + 1) * 128], in0=o_t[:, 0:128], scalar=coef[0][:, h:h + 1],
                                                             in1=acc[0][:, h, :], op0=ALU.mult, op1=ALU.add),
                     reads=[o_r, coef[1], acc[1]], writes=[yn[1]])


TWO_PI_S = 6.283185


def rr_sincos(P, x, xr, n, p, out_sin, out_cos, out_r, tmps):
    (r, rr), (ri, rir), (rf, rfr), (t1, t1r) = tmps
    P.op("dve", lambda e: e.tensor_scalar(out=r[0:p, 0:n], in0=x, scalar1=1.0 / (2 * np.pi), scalar2=None, op0=ALU.mult), reads=[xr], writes=[rr])
    P.op("dve", lambda e: e.tensor_copy(out=ri[0:p, 0:n], in_=r[0:p, 0:n]), reads=[rr], writes=[rir])
    P.op("dve", lambda e: e.tensor_copy(out=rf[0:p, 0:n], in_=ri[0:p, 0:n]), reads=[rir], writes=[rfr])
    P.op("dve", lambda e: e.tensor_tensor(out=r[0:p, 0:n], in0=r[0:p, 0:n], in1=rf[0:p, 0:n], op=ALU.subtract), reads=[rr, rfr], writes=[rr])
    for shift in (0.0, 0.25):
        if shift:
            P.op("dve", lambda e: e.tensor_scalar(out=r[0:p, 0:n], in0=r[0:p, 0:n], scalar1=shift, scalar2=None, op0=ALU.add), reads=[rr], writes=[rr])
        P.op("dve", lambda e: e.tensor_scalar(out=t1[0:p, 0:n], in0=r[0:p, 0:n], scalar1=0.5, scalar2=None, op0=ALU.is_gt), reads=[rr], writes=[t1r])
        P.op("dve", lambda e: e.tensor_tensor(out=r[0:p, 0:n], in0=r[0:p, 0:n], in1=t1[0:p, 0:n], op=ALU.subtract), reads=[rr, t1r], writes=[rr])
        P.op("dve", lambda e: e.tensor_scalar(out=t1[0:p, 0:n], in0=r[0:p, 0:n], scalar1=-0.5, scalar2=None, op0=ALU.is_lt), reads=[rr], writes=[t1r])
        P.op("dve", lambda e: e.tensor_tensor(out=r[0:p, 0:n], in0=r[0:p, 0:n], in1=t1[0:p, 0:n], op=ALU.add), reads=[rr, t1r], writes=[rr])
        dst = out_sin if not shift else out_cos
        P.op("act", lambda e: e.activation(out=dst, in_=r[0:p, 0:n], func=AF.Sin, scale=TWO_PI_S), reads=[rr], writes=[out_r])


def phase_s5(P, d, yT_d):
    T = TS5
    with P.scope():
        pc = P.sb([128, 2, 3], F32, "pc")
        prow = P.sb([32, 3, 256], F32, "prow")
        P.dma("sp", pc[0][:], d["s5col"].ap(), writes=[pc[1]])
        P.dma("sp", prow[0][:], d["s5row"].ap(), writes=[prow[1]])
        bre = P.sb([32, 256], F32, "bTre")
        bim = P.sb([32, 256], F32, "bTim")
        cre = P.sb([128, 2, 32], F32, "cTre")
        cim = P.sb([128, 2, 32], F32, "cTim")
        dv = P.sb([32, 2], F32, "dvec")
        P.dma("sp", bre[0][:], d["bTre"].ap(), writes=[bre[1]])
        P.dma("sp", bim[0][:], d["bTim"].ap(), writes=[bim[1]])
        P.dma("sp", cre[0][:], d["cTre"].ap(), writes=[cre[1]])
        P.dma("sp", cim[0][:], d["cTim"].ap(), writes=[cim[1]])
        P.dma("sp", dv[0][:], d["dvec"].ap(), writes=[dv[1]])
        P.op("dve", lambda e: e.tensor_scalar(out=cim[0][:], in0=cim[0][:], scalar1=-1.0, scalar2=None, op0=ALU.mult), reads=[cim[1]], writes=[cim[1]])
        rho = [P.sb([128, T], F32, "rho%d" % i) for i in range(2)]
        sinT = [P.sb([128, T + 1], F32, "sinT%d" % i) for i in range(2)]
        cosT = [P.sb([128, T + 1], F32, "cosT%d" % i) for i in range(2)]
        bbre = P.sb([32, 256], F32, "bbre")
        bbim = P.sb([32, 256], F32, "bbim")
        with P.scope():
            tmps = [P.sb([128, T + 1], F32, "rr0"), P.sb([128, T + 1], I32, "rr1"), P.sb([128, T + 1], F32, "rr2"), P.sb([128, T + 1], F32, "rr3")]
            kk = P.sb([128, T + 1], F32, "kk")
            ph = P.sb([128, T + 1], F32, "ph")
            P.dma("sp", kk[0][:], d["kk"].ap(), writes=[kk[1]])
            dtc = P.sb([128, 2], F32, "dtc")
            magc = P.sb([128, 2], F32, "magc")
            thc = P.sb([128, 2], F32, "thc")
            P.op("act", lambda e: e.activation(out=dtc[0][:], in_=pc[0][:, :, 2], func=AF.Exp), reads=[pc[1]], writes=[dtc[1]])
            P.op("dve", lambda e: e.tensor_tensor(out=magc[0][:], in0=pc[0][:, :, 0], in1=dtc[0][:], op=ALU.mult), reads=[pc[1], dtc[1]], writes=[magc[1]])
            P.op("act", lambda e: e.activation(out=magc[0][:], in_=magc[0][:], func=AF.Exp), reads=[magc[1]], writes=[magc[1]])
            P.op("dve", lambda e: e.tensor_tensor(out=thc[0][:], in0=pc[0][:, :, 1], in1=dtc[0][:], op=ALU.mult), reads=[pc[1], dtc[1]], writes=[thc[1]])
            for pr in range(2):
                P.op("dve", lambda e: e.tensor_copy(out=rho[pr][0][:], in_=magc[0][:, pr:pr + 1].to_broadcast([128, T])), reads=[magc[1]], writes=[rho[pr][1]])
                P.op("dve", lambda e: e.tensor_scalar(out=ph[0][:], in0=kk[0][:], scalar1=thc[0][:, pr:pr + 1], scalar2=None, op0=ALU.mult),
                     reads=[kk[1], thc[1]], writes=[ph[1]])
                rr_sincos(P, ph[0][:], ph[1], T + 1, 128, sinT[pr][0][:], cosT[pr][0][:], sinT[pr][1], tmps)
                cosT[pr][1].w = sinT[pr][1].w
            dtr = P.sb([32, 256], F32, "dtr")
            magr = P.sb([32, 256], F32, "magr")
            angr = P.sb([32, 256], F32, "angr")
            sr = P.sb([32, 256], F32, "sr")
            cr = P.sb([32, 256], F32, "cr")
            w = [P.sb([32, 256], F32, "w%d" % i) for i in range(6)]
            ar = prow[0][:, 0, :]
            ai = prow[0][:, 1, :]
            P.op("act", lambda e: e.activation(out=dtr[0][:], in_=prow[0][:, 2, :], func=AF.Exp), reads=[prow[1]], writes=[dtr[1]])
            P.op("dve", lambda e: e.tensor_tensor(out=magr[0][:], in0=ar, in1=dtr[0][:], op=ALU.mult), reads=[prow[1], dtr[1]], writes=[magr[1]])
            P.op("act", lambda e: e.activation(out=magr[0][:], in_=magr[0][:], func=AF.Exp), reads=[magr[1]], writes=[magr[1]])
            P.op("dve", lambda e: e.tensor_tensor(out=angr[0][:], in0=ai, in1=dtr[0][:], op=ALU.mult), reads=[prow[1], dtr[1]], writes=[angr[1]])
            rr_sincos(P, angr[0][:], angr[1], 256, 32, sr[0][:], cr[0][:], sr[1], tmps)
            cr[1].w = sr[1].w

            def tt(o, a, b, op, rd):
                P.op("dve", lambda e: e.tensor_tensor(out=o[0][:], in0=a, in1=b, op=op), reads=rd, writes=[o[1]])
            lre, lim, den, t0, cre_, cim_ = w
            tt(lre, magr[0][:], cr[0][:], ALU.mult, [magr[1], sr[1]])
            tt(lim, magr[0][:], sr[0][:], ALU.mult, [magr[1], sr[1]])
            P.op("dve", lambda e: e.tensor_scalar(out=lre[0][:], in0=lre[0][:], scalar1=-1.0, scalar2=None, op0=ALU.add), reads=[lre[1]], writes=[lre[1]])
            tt(den, ar, ar, ALU.mult, [prow[1]])
            tt(t0, ai, ai, ALU.mult, [prow[1]])
            tt(den, den[0][:], t0[0][:], ALU.add, [den[1], t0[1]])
            P.op("dve", lambda e: e.reciprocal(out=den[0][:], in_=den[0][:]), reads=[den[1]], writes=[den[1]])
            tt(cre_, lre[0][:], ar, ALU.mult, [lre[1], prow[1]])
            tt(t0, lim[0][:], ai, ALU.mult, [lim[1], prow[1]])
            tt(cre_, cre_[0][:], t0[0][:], ALU.add, [cre_[1], t0[1]])
            tt(cre_, cre_[0][:], den[0][:], ALU.mult, [cre_[1], den[1]])
            tt(cim_, lim[0][:], ar, ALU.mult, [lim[1], prow[1]])
            tt(t0, lre[0][:], ai, ALU.mult, [lre[1], prow[1]])
            tt(cim_, cim_[0][:], t0[0][:], ALU.subtract, [cim_[1], t0[1]])
            tt(cim_, cim_[0][:], den[0][:], ALU.mult, [cim_[1], den[1]])
            tt(bbre, cre_[0][:], bre[0][:], ALU.mult, [cre_[1], bre[1]])
            tt(t0, cim_[0][:], bim[0][:], ALU.mult, [cim_[1], bim[1]])
            tt(bbre, bbre[0][:], t0[0][:], ALU.subtract, [bbre[1], t0[1]])
            tt(bbim, cre_[0][:], bim[0][:], ALU.mult, [cre_[1], bim[1]])
            tt(t0, cim_[0][:], bre[0][:], ALU.mult, [cim_[1], bre[1]])
            tt(bbim, bbim[0][:], t0[0][:], ALU.add, [bbim[1], t0[1]])
        ut = [P.sb([32, T], F32, "ut%d" % i) for i in range(2)]
        yo = [P.sb([32, T], F32, "yo%d" % i) for i in range(2)]
        bpr = P.sb([128, T], F32, "bpr")
        bpi = P.sb([128, T], F32, "bpi")
        gre = P.sb([128, T], F32, "gre")
        gim = P.sb([128, T], F32, "gim")
        mm_ = [P.sb([128, T], F32, "m%d" % i) for i in range(4)]
        hre = P.sb([128, T], F32, "hre")
        him = P.sb([128, T], F32, "him")
        tq = [P.sb([128, 512], F32, "tq%d" % i) for i in range(4)]
        gi = P.sb([128, 4], F32, "gi")
        xx = P.sb([128, 4], F32, "xx")
        P.op("pool", lambda e: e.memset(gi[0][:], 0.0), writes=[gi[1]])
        pb = [P.ps([128, 512], F32, "pb%d" % i) for i in range(4)]
        py = [P.ps([32, 512], F32, "py%d" % i) for i in range(2)]
        n = 0
        for ci in range(L // T):
            for pr in range(2):
                u_t, u_r = ut[n % 2]
                y_t, y_r = yo[n % 2]
                n += 1
                P.dma("sp", u_t[:], d["uT"].ap()[pr, :, ci * T:(ci + 1) * T], writes=[u_r])
                cs, sn = cosT[pr], sinT[pr]
                for half in range(T // 512):
                    sl = slice(half * 512, (half + 1) * 512)
                    p_re, p_im = pb[2 * (half % 2)], pb[2 * (half % 2) + 1]
                    P.op("pe", lambda e: e.matmul(p_re[0][:], lhsT=bbre[0][:, pr * 128:(pr + 1) * 128], rhs=u_t[:, sl], start=True, stop=True),
                         reads=[bbre[1], u_r], writes=[p_re[1]])
                    P.op("pe", lambda e: e.matmul(p_im[0][:], lhsT=bbim[0][:, pr * 128:(pr + 1) * 128], rhs=u_t[:, sl], start=True, stop=True),
                         reads=[bbim[1], u_r], writes=[p_im[1]])
                    P.op("dve", lambda e: e.tensor_tensor(out=tq[0][0][:], in0=p_re[0][:], in1=cs[0][:, sl], op=ALU.mult), reads=[p_re[1], cs[1]], writes=[tq[0][1]])
                    P.op("dve", lambda e: e.tensor_tensor(out=tq[1][0][:], in0=p_im[0][:], in1=sn[0][:, sl], op=ALU.mult), reads=[p_im[1], sn[1]], writes=[tq[1][1]])
                    P.op("pool", lambda e: e.tensor_tensor(out=bpr[0][:, sl], in0=tq[0][0][:], in1=tq[1][0][:], op=ALU.add), reads=[tq[0][1], tq[1][1]], writes=[bpr[1]])
                    P.op("dve", lambda e: e.tensor_tensor(out=tq[2][0][:], in0=p_im[0][:], in1=cs[0][:, sl], op=ALU.mult), reads=[p_im[1], cs[1]], writes=[tq[2][1]])
                    P.op("dve", lambda e: e.tensor_tensor(out=tq[3][0][:], in0=p_re[0][:], in1=sn[0][:, sl], op=ALU.mult), reads=[p_re[1], sn[1]], writes=[tq[3][1]])
                    P.op("pool", lambda e: e.tensor_tensor(out=bpi[0][:, sl], in0=tq[2][0][:], in1=tq[3][0][:], op=ALU.subtract), reads=[tq[2][1], tq[3][1]], writes=[bpi[1]])
                P.op("dve", lambda e: e.tensor_tensor_scan(out=gre[0][:], data0=rho[pr][0][:], data1=bpr[0][:], initial=gi[0][:, 2 * pr:2 * pr + 1],
                                                           op0=ALU.mult, op1=ALU.add), reads=[rho[pr][1], bpr[1], gi[1]], writes=[gre[1]])
                P.op("dve", lambda e: e.tensor_tensor_scan(out=gim[0][:], data0=rho[pr][0][:], data1=bpi[0][:], initial=gi[0][:, 2 * pr + 1:2 * pr + 2],
                                                           op0=ALU.mult, op1=ALU.add), reads=[rho[pr][1], bpi[1], gi[1]], writes=[gim[1]])
                cT_, sT_ = cs[0][:, T:T + 1], sn[0][:, T:T + 1]
                gl_r, gl_i = gre[0][:, T - 1:T], gim[0][:, T - 1:T]
                P.op("dve", lambda e: e.tensor_tensor(out=xx[0][:, 0:1], in0=gl_r, in1=cT_, op=ALU.mult), reads=[gre[1], cs[1]], writes=[xx[1]])
                P.op("dve", lambda e: e.tensor_tensor(out=xx[0][:, 1:2], in0=gl_i, in1=sT_, op=ALU.mult), reads=[gim[1], sn[1]], writes=[xx[1]])
                P.op("dve", lambda e: e.tensor_tensor(out=xx[0][:, 2:3], in0=gl_r, in1=sT_, op=ALU.mult), reads=[gre[1], sn[1]], writes=[xx[1]])
                P.op("dve", lambda e: e.tensor_tensor(out=xx[0][:, 3:4], in0=gl_i, in1=cT_, op=ALU.mult), reads=[gim[1], cs[1]], writes=[xx[1]])
                P.op("dve", lambda e: e.tensor_tensor(out=gi[0][:, 2 * pr:2 * pr + 1], in0=xx[0][:, 0:1], in1=xx[0][:, 1:2], op=ALU.subtract), reads=[xx[1]], writes=[gi[1]])
                P.op("dve", lambda e: e.tensor_tensor(out=gi[0][:, 2 * pr + 1:2 * pr + 2], in0=xx[0][:, 2:3], in1=xx[0][:, 3:4], op=ALU.add), reads=[xx[1]], writes=[gi[1]])
                P.op("dve", lambda e: e.tensor_tensor(out=mm_[0][0][:], in0=gre[0][:], in1=cs[0][:, 0:T], op=ALU.mult), reads=[gre[1], cs[1]], writes=[mm_[0][1]])
                P.op("dve", lambda e: e.tensor_tensor(out=mm_[1][0][:], in0=gim[0][:], in1=sn[0][:, 0:T], op=ALU.mult), reads=[gim[1], sn[1]], writes=[mm_[1][1]])
                P.op("pool", lambda e: e.tensor_tensor(out=mm_[2][0][:], in0=gre[0][:], in1=sn[0][:, 0:T], op=ALU.mult), reads=[gre[1], sn[1]], writes=[mm_[2][1]])
                P.op("pool", lambda e: e.tensor_tensor(out=mm_[3][0][:], in0=gim[0][:], in1=cs[0][:, 0:T], op=ALU.mult), reads=[gim[1], cs[1]], writes=[mm_[3][1]])
                P.op("dve", lambda e: e.tensor_tensor(out=hre[0][:], in0=mm_[0][0][:], in1=mm_[1][0][:], op=ALU.subtract), reads=[mm_[0][1], mm_[1][1]], writes=[hre[1]])
                P.op("pool", lambda e: e.tensor_tensor(out=him[0][:], in0=mm_[2][0][:], in1=mm_[3][0][:], op=ALU.add), reads=[mm_[2][1], mm_[3][1]], writes=[him[1]])
                for half in range(T // 512):
                    sl = slice(half * 512, (half + 1) * 512)
                    y_p = py[half % 2]
                    P.mm_group(y_p[1], [lambda e, st, sp_: e.matmul(y_p[0][:], lhsT=cre[0][:, pr, :], rhs=hre[0][:, sl], start=st, stop=sp_),
                                        lambda e, st, sp_: e.matmul(y_p[0][:], lhsT=cim[0][:, pr, :], rhs=him[0][:, sl], start=st, stop=sp_)],
                               reads=[cre[1], cim[1], hre[1], him[1]])
                    P.op("dve", lambda e: e.scalar_tensor_tensor(out=y_t[:, sl], in0=u_t[:, sl], scalar=dv[0][:, pr:pr + 1], in1=y_p[0][:],
                                                                 op0=ALU.mult, op1=ALU.add), reads=[u_r, dv[1], y_p[1]], writes=[y_r])
                P.dma("sp", yT_d.ap()[pr * 32:(pr + 1) * 32, ci * T:(ci + 1) * T], y_t[:], reads=[y_r])


def build_B(do_cross=True, do_moba=True, do_nsa=True, do_s5=True):
    P = Prog()
    d = {}

    def inp(name, shape, dt):
        d[name] = P.dram(name, shape, dt, kind="ExternalInput")[0]

    inp("ident", [128, 128], BF16)
    inp("eall", [128, 8192], BF16)
    outs = {}
    if do_cross:
        inp("memT", [2048, 256], F32)
        inp("wk", [2048, 512], F32)
        inp("wv", [2048, 512], F32)
        inp("xqT", [4, 128, TOK], BF16)
        outs["yx"] = P.dram("yx", [TOK, 512], BF16, kind="ExternalOutput")[0]
    if do_moba:
        inp("mqT", [4, 128, TOK], BF16)
        inp("mkT", [4, 128, L], BF16)
        inp("mv", [4, 128, NKT, 128], BF16)
        inp("mkTl", [4, 128, TOK], BF16)
        inp("mvl", [4, 128, 16, 128], BF16)
        inp("gmask", [TOK, 64], F32)
        inp("dmoba", [4, 128, 512], BF16)
        outs["ym"] = P.dram("ym", [TOK, 512], BF16, kind="ExternalOutput")[0]
    if do_nsa:
        for nm, shp, dt in (("kcmpT", [128, L], BF16), ("vcmpT", [128, L], BF16), ("kselT", [128, L], BF16), ("vsel", [128, NKT, 128], BF16),
                            ("kselTl", [128, TOK], BF16), ("vsell", [128, 16, 128], BF16), ("kwinT", [128, 2560], BF16), ("vwin", [128, 20, 128], BF16),
                            ("nqT", [128, 16, 512], BF16), ("nqrT", [128, 16, 512], BF16), ("gates", [TOK, 12], BF16),
                            ("ck1", [4096, 256], F32), ("cv1", [4096, 256], F32), ("ck2", [256, 128], F32), ("cv2", [256, 128], F32),
                            ("poskT", [128, 32], F32), ("posvT", [128, 32], F32), ("amat", [128, 8, 256], BF16), ("addm", [TOK, 256], F32),
                            ("cthr", [128, 128], F32), ("v16", [128, 512], F32), ("dsel", [128, 512], BF16), ("wmask", [5, 128, 5, 512], BF16)):
            inp(nm, shp, dt)
        outs["yn"] = P.dram("yn", [TOK, 512], BF16, kind="ExternalOutput")[0]
    if do_s5:
        for nm, shp, dt in (("uT", [2, 32, L], F32), ("s5col", [128, 2, 3], F32), ("s5row", [32, 3, 256], F32), ("bTre", [32, 256], F32),
                            ("bTim", [32, 256], F32), ("cTre", [128, 2, 32], F32), ("cTim", [128, 2, 32], F32), ("dvec", [32, 2], F32),
                            ("kk", [128, TS5 + 1], F32)):
            inp(nm, shp, dt)
        outs["ysT"] = P.dram("ysT", [64, L], F32, kind="ExternalOutput")[0]
    if do_cross or do_moba or do_nsa:
      with P.scope():
        C = attn_ctx(P)
        load_consts(P, C, d)
        ystage = P.sb([128, 16, 512], BF16, "ystage")
        if do_cross:
            phase_cross(P, C, d, ystage)
            P.dma("sp", outs["yx"].ap().rearrange("(t p) f -> p t f", p=128), ystage[0][:], reads=[ystage[1]])
        if do_moba:
            phase_moba(P, C, d, ystage)
            P.dma("sp", outs["ym"].ap().rearrange("(t p) f -> p t f", p=128), ystage[0][:], reads=[ystage[1]])
        if do_nsa:
            phase_nsa(P, C, d, ystage)
            P.dma("sp", outs["yn"].ap().rearrange("(t p) f -> p t f", p=128), ystage[0][:], reads=[ystage[1]])
    if do_s5:
        phase_s5(P, d, outs["ysT"])
    P.finish()
    return P


D = 2048
DFF = 5632
NFC = DFF // 128
ALPHA = (2.0 * 4) ** 0.25
LN_EPS = 1e-5


def ffn_group(P, xT, xT_r, ntok, wg_d, wu_d, wd_d, aT, aT_r, outT_d, col0, gw=None, gw_r=None, pools=None):
    wgb, wub, wdb, pg, pu, po, sil, ost = pools
    nnt = ntok // 512
    wgv = wg_d.ap().rearrange("(c p) f -> p c f", p=128)
    wuv = wu_d.ap().rearrange("(c p) f -> p c f", p=128)
    wdv = wd_d.ap().rearrange("(fc p) d -> p fc d", p=128)

    def load_gu(i):
        g_t, g_r = wgb[i % 2]
        u_t, u_r = wub[i % 2]
        for q in range(2):
            P.dma("pool", g_t[:, 8 * q:8 * q + 8, :], wgv[:, 8 * q:8 * q + 8, 256 * i:256 * (i + 1)], writes=[g_r])
            P.dma("pool", u_t[:, 8 * q:8 * q + 8, :], wuv[:, 8 * q:8 * q + 8, 256 * i:256 * (i + 1)], writes=[u_r])
    load_gu(0)
    cnt = 0
    for i in range(NFC // 2):
        if i + 1 < NFC // 2:
            load_gu(i + 1)
        g_t, g_r = wgb[i % 2]
        u_t, u_r = wub[i % 2]
        for j in range(2):
            fc = 2 * i + j
            for nt in range(nnt):
                pg_t, pg_r = pg[cnt % 2]
                pu_t, pu_r = pu[cnt % 2]
                s_t, s_r = sil[cnt % 2]
                cnt += 1
                P.mm_group(pg_r, [(lambda e, st, sp_, c=c: e.matmul(pg_t[:], lhsT=g_t[:, c, j * 128:(j + 1) * 128], rhs=xT[:, c, nt * 512:(nt + 1) * 512],
                                                                  start=st, stop=sp_)) for c in range(16)], reads=[g_r, xT_r])
                P.mm_group(pu_r, [(lambda e, st, sp_, c=c: e.matmul(pu_t[:], lhsT=u_t[:, c, j * 128:(j + 1) * 128], rhs=xT[:, c, nt * 512:(nt + 1) * 512],
                                                                  start=st, stop=sp_)) for c in range(16)], reads=[u_r, xT_r])
                P.op("act", lambda e: e.activation(out=s_t[:], in_=pg_t[:], func=AF.Silu), reads=[pg_r], writes=[s_r])
                if gw is None:
                    P.op("dve", lambda e: e.tensor_tensor(out=aT[:, fc, nt * 512:(nt + 1) * 512], in0=s_t[:], in1=pu_t[:], op=ALU.mult),
                         reads=[s_r, pu_r], writes=[aT_r])
                else:
                    P.op("dve", lambda e: e.tensor_tensor(out=s_t[:], in0=s_t[:], in1=pu_t[:], op=ALU.mult), reads=[s_r, pu_r], writes=[s_r])
                    P.op("pool", lambda e: e.tensor_tensor(out=aT[:, fc, nt * 512:(nt + 1) * 512], in0=s_t[:], in1=gw[:, nt * 512:(nt + 1) * 512], op=ALU.mult),
                         reads=[s_r, gw_r], writes=[aT_r])

    def load_d(dt):
        w_t, w_r = wdb[dt % 2]
        for q in range(4):
            P.dma("pool", w_t[:, 11 * q:11 * q + 11, :], wdv[:, 11 * q:11 * q + 11, dt * 128:(dt + 1) * 128], writes=[w_r])
    load_d(0)
    cnt = 0
    for dt in range(16):
        if dt + 1 < 16:
            load_d(dt + 1)
        w_t, w_r = wdb[dt % 2]
        for nt in range(nnt):
            po_t, po_r = po[cnt % 2]
            o_t, o_r = ost[cnt % 2]
            cnt += 1
            P.mm_group(po_r, [(lambda e, st, sp_, fc=fc: e.matmul(po_t[:], lhsT=w_t[:, fc, :], rhs=aT[:, fc, nt * 512:(nt + 1) * 512],
                                                                start=st, stop=sp_)) for fc in range(NFC)], reads=[w_r, aT_r])
            P.op("act", lambda e: e.activation(out=o_t[:], in_=po_t[:], func=AF.Copy), reads=[po_r], writes=[o_r])
            P.dma("sp", outT_d.ap()[dt * 128:(dt + 1) * 128, col0 + nt * 512:col0 + (nt + 1) * 512], o_t[:], reads=[o_r])


def ffn_pools(P):
    wgb = [P.sb([128, 16, 256], BF16, "wgb%d" % i) for i in range(2)]
    wub = [P.sb([128, 16, 256], BF16, "wub%d" % i) for i in range(2)]
    wdb = [P.sb([128, NFC, 128], BF16, "wdb%d" % i) for i in range(2)]
    pg = [P.ps([128, 512], F32, "pg%d" % i) for i in range(2)]
    pu = [P.ps([128, 512], F32, "pu%d" % i) for i in range(2)]
    po = [P.ps([128, 512], F32, "po%d" % i) for i in range(2)]
    sil = [P.sb([128, 512], F32, "sil%d" % i) for i in range(2)]
    ost = [P.sb([128, 512], F32, "ost%d" % i) for i in range(2)]
    return wgb, wub, wdb, pg, pu, po, sil, ost


def gelu_tanh_sb(P, src, src_r, dst, dst_r, tmp, n):
    (gt, gtr), (gs_, gsr) = tmp
    P.op("dve", lambda e: e.tensor_tensor(out=gt[:, 0:n], in0=src, in1=src, op=ALU.mult), reads=[src_r], writes=[gtr])
    P.op("dve", lambda e: e.tensor_scalar(out=gt[:, 0:n], in0=gt[:, 0:n], scalar1=0.044715, scalar2=1.0, op0=ALU.mult, op1=ALU.add),
         reads=[gtr], writes=[gtr])
    P.op("dve", lambda e: e.tensor_tensor(out=gt[:, 0:n], in0=gt[:, 0:n], in1=src, op=ALU.mult), reads=[gtr, src_r], writes=[gtr])
    P.op("act", lambda e: e.activation(out=gs_[:, 0:n], in_=gt[:, 0:n], func=AF.Sigmoid, scale=1.5957691216057308), reads=[gtr], writes=[gsr])
    P.op("dve", lambda e: e.tensor_tensor(out=dst, in0=src, in1=gs_[:, 0:n], op=ALU.mult), reads=[src_r, gsr], writes=[dst_r])


def build_C(moe, TOK=2048):
    P = Prog()
    d = {}

    def inp(name, shape, dt):
        d[name] = P.dram(name, shape, dt, kind="ExternalInput")[0]

    def outp(name, shape, dt):
        d[name] = P.dram(name, shape, dt, kind="ExternalOutput")[0]
    GT = 1024
    inp("xres", [TOK, D], F32)
    inp("ysT", [512, TOK], F32)
    inp("yT", [1536, TOK], BF16)
    inp("wglu", [512, 512], F32)
    inp("wo", [D, D], F32)
    inp("lng", [1, D], F32)
    inp("lnb", [1, D], F32)
    inp("identb", [128, 128], BF16)
    outp("ax1", [TOK, D], F32)
    if moe:
        inp("identf", [128, 128], F32)
        inp("router", [D, 8], F32)
        outp("x1b", [TOK, D], BF16)
        outp("gates", [TOK, 8], F32)
    else:
        inp("wg", [D, DFF], F32)
        inp("wu", [D, DFF], F32)
        inp("wd", [DFF, D], F32)
        outp("fT", [D, TOK], F32)
    g_t, g_r = P.sb([128, D], F32, "ln_g")
    b_t, b_r = P.sb([128, D], F32, "ln_b")
    P.dma("sp", g_t[:], d["lng"].ap()[0:1, :].to_broadcast([128, D]), writes=[g_r])
    P.dma("sp", b_t[:], d["lnb"].ap()[0:1, :].to_broadcast([128, D]), writes=[b_r])
    eps_t, eps_r = P.sb([128, 1], F32, "eps")
    P.op("pool", lambda e: e.memset(eps_t[:], LN_EPS), writes=[eps_r])
    idb = P.sb([128, 128], BF16, "idb")
    P.dma("sp", idb[0][:], d["identb"].ap(), writes=[idb[1]])
    wglu = P.sb([128, 4, 512], BF16, "wglu")
    P.dma("pool", wglu[0][:], d["wglu"].ap().rearrange("(c p) f -> p c f", p=128), writes=[wglu[1]])
    if moe:
        idf = P.sb([128, 128], F32, "idf")
        P.dma("sp", idf[0][:], d["identf"].ap(), writes=[idf[1]])
        rt = P.sb([128, 16, 8], F32, "router")
        P.dma("sp", rt[0][:], d["router"].ap().rearrange("(c p) e -> p c e", p=128), writes=[rt[1]])
    for grp in range(TOK // GT):
        t0 = grp * GT
        with P.scope():
            x1T = P.sb([128, 16, GT], BF16, "x1T")
            with P.scope():
                ycT = P.sb([128, 16, GT], BF16, "ycT")
                for q in range(3):
                    P.dma("sp", ycT[0][:, 4 + 4 * q:8 + 4 * q, :], d["yT"].ap().rearrange("(c p) t -> p c t", p=128)[:, 4 * q:4 * q + 4, t0:t0 + GT], writes=[ycT[1]])
                with P.scope():
                    ys = P.sb([128, 4, GT], F32, "ys")
                    zf = P.sb([128, 4, GT], F32, "zf")
                    zb = P.sb([128, 4, GT], BF16, "zb")
                    tmp = [P.sb([128, GT], F32, "gl%d" % i) for i in range(2)]
                    sg = P.sb([128, 512], F32, "sg")
                    pgl = [P.ps([128, 512], F32, "pgl%d" % i) for i in range(2)]
                    P.dma("sp", ys[0][:], d["ysT"].ap().rearrange("(c p) t -> p c t", p=128)[:, :, t0:t0 + GT], writes=[ys[1]])
                    for c in range(4):
                        gelu_tanh_sb(P, ys[0][:, c, :], ys[1], zf[0][:, c, :], zf[1], tmp, GT)
                    P.op("act", lambda e: e.activation(out=zb[0][:], in_=zf[0][:], func=AF.Copy), reads=[zf[1]], writes=[zb[1]])
                    n = 0
                    for fo in range(4):
                        for nt in range(GT // 512):
                            p_t, p_r = pgl[n % 2]
                            n += 1
                            P.mm_group(p_r, [(lambda e, st, sp_, c=c: e.matmul(p_t[:], lhsT=wglu[0][:, c, fo * 128:(fo + 1) * 128],
                                                                             rhs=zb[0][:, c, nt * 512:(nt + 1) * 512], start=st, stop=sp_)) for c in range(4)],
                                       reads=[wglu[1], zb[1]])
                            P.op("act", lambda e: e.activation(out=sg[0][:], in_=p_t[:], func=AF.Sigmoid), reads=[p_r], writes=[sg[1]])
                            P.op("dve", lambda e: e.tensor_tensor(out=ycT[0][:, fo, nt * 512:(nt + 1) * 512], in0=zf[0][:, fo, nt * 512:(nt + 1) * 512],
                                                                  in1=sg[0][:], op=ALU.mult), reads=[zf[1], sg[1]], writes=[ycT[1]])
                xp = [P.sb([128, D], F32, "xp%d" % i) for i in range(GT // 128)]
                for t in range(GT // 128):
                    P.dma("sp", xp[t][0][:], d["xres"].ap()[t0 + t * 128:t0 + (t + 1) * 128, :], writes=[xp[t][1]])
                with P.scope():
                    wot = [P.sb([128, 16, 512], BF16, "wot%d" % i) for i in range(2)]
                    pw = [P.ps([128, 512], F32, "pw%d" % i) for i in range(2)]
                    wov = d["wo"].ap().rearrange("(c p) f -> p c f", p=128)

                    def load_wo(n_):
                        w_t, w_r = wot[n_ % 2]
                        for q in range(4):
                            P.dma("pool", w_t[:, 4 * q:4 * q + 4, :], wov[:, 4 * q:4 * q + 4, 512 * n_:512 * (n_ + 1)], writes=[w_r])
                    load_wo(0)
                    n = 0
                    for n_ in range(4):
                        if n_ + 1 < 4:
                            load_wo(n_ + 1)
                        w_t, w_r = wot[n_ % 2]
                        for t in range(GT // 128):
                            p_t, p_r = pw[n % 2]
                            n += 1
                            P.mm_group(p_r, [(lambda e, st, sp_, c=c: e.matmul(p_t[:], lhsT=ycT[0][:, c, t * 128:(t + 1) * 128], rhs=w_t[:, c, :],
                                                                             start=st, stop=sp_)) for c in range(16)], reads=[ycT[1], w_r])
                            P.op("dve", lambda e: e.scalar_tensor_tensor(out=xp[t][0][:, n_ * 512:(n_ + 1) * 512], in0=xp[t][0][:, n_ * 512:(n_ + 1) * 512],
                                                                         scalar=ALPHA, in1=p_t[:], op0=ALU.mult, op1=ALU.add),
                                 reads=[xp[t][1], p_r], writes=[xp[t][1]])
                with P.scope():
                    st = [P.sb([128, 4, 6], F32, "bst%d" % i) for i in range(2)]
                    mv = [P.sb([128, 2], F32, "mv%d" % i) for i in range(2)]
                    sd = [P.sb([128, 1], F32, "sd%d" % i) for i in range(2)]
                    xbf = [P.sb([128, D], BF16, "xbf%d" % i) for i in range(2)]
                    axs = [P.sb([128, D], F32, "axs%d" % i) for i in range(2)]
                    tp = [P.ps([128, 4, 128], BF16, "tp%d" % i) for i in range(2)]
                    if moe:
                        tpf = [P.ps([128, 4, 128], F32, "tpf%d" % i) for i in range(2)]
                        xTf = P.sb([128, 16, 128], F32, "xTf")
                        plg = P.ps([128, 8], F32, "plg")
                        lg = P.sb([128, 8], F32, "lg")
                        m8 = P.sb([128, 8], F32, "m8")
                        gv = P.sb([128, 4], F32, "gv")
                        go = [P.sb([128, 8], F32, "go%d" % i) for i in range(2)]
                        g2 = P.sb([128, 8], F32, "g2")
                    ntp = 0
                    for t in range(GT // 128):
                        x_t, x_r = xp[t]
                        s_t, s_r = st[t % 2]
                        m_t, m_r = mv[t % 2]
                        d_t, d_r = sd[t % 2]
                        for c in range(4):
                            P.op("dve", lambda e: e.bn_stats(out=s_t[:, c, :], in_=x_t[:, c * 512:(c + 1) * 512]), reads=[x_r], writes=[s_r])
                        P.op("dve", lambda e: e.bn_aggr(out=m_t[:], in_=s_t[:].rearrange("p a b -> p (a b)")), reads=[s_r], writes=[m_r])
                        P.op("act", lambda e: e.activation(out=d_t[:], in_=m_t[:, 1:2], func=AF.Sqrt, bias=eps_t[:], scale=1.0),
                             reads=[m_r, eps_r], writes=[d_r])
                        P.op("dve", lambda e: e.reciprocal(out=d_t[:], in_=d_t[:]), reads=[d_r], writes=[d_r])
                        P.op("dve", lambda e: e.tensor_scalar(out=x_t[:], in0=x_t[:], scalar1=m_t[:, 0:1], scalar2=d_t[:, 0:1],
                                                              op0=ALU.subtract, op1=ALU.mult), reads=[x_r, m_r, d_r], writes=[x_r])
                        P.op("pool", lambda e: e.tensor_tensor(out=x_t[:], in0=x_t[:], in1=g_t[:], op=ALU.mult), reads=[x_r, g_r], writes=[x_r])
                        P.op("dve", lambda e: e.tensor_tensor(out=x_t[:], in0=x_t[:], in1=b_t[:], op=ALU.add), reads=[x_r, b_r], writes=[x_r])
                        a_t, a_r = axs[t % 2]
                        P.op("pool", lambda e: e.tensor_scalar(out=a_t[:], in0=x_t[:], scalar1=ALPHA, scalar2=None, op0=ALU.mult), reads=[x_r], writes=[a_r])
                        P.dma("sp", d["ax1"].ap()[t0 + t * 128:t0 + (t + 1) * 128, :], a_t[:], reads=[a_r])
                        f_t, f_r = xbf[t % 2]
                        P.op("act", lambda e: e.activation(out=f_t[:], in_=x_t[:], func=AF.Copy), reads=[x_r], writes=[f_r])
                        if moe:
                            P.dma("sp", d["x1b"].ap()[t0 + t * 128:t0 + (t + 1) * 128, :], f_t[:], reads=[f_r])
                        for j in range(4):
                            p_t, p_r = tp[ntp % 2]
                            ntp += 1
                            for k in range(4):
                                c = 4 * j + k
                                P.op("pe", lambda e: e.transpose(out=p_t[:, k, :], in_=f_t[:, c * 128:(c + 1) * 128], identity=idb[0][:]),
                                     reads=[f_r, idb[1]], writes=[p_r] if k == 0 else [])
                            p_r.w = ("s_pe", P.engs["pe"].count)
                            P.op("dve", lambda e: e.tensor_copy(out=x1T[0][:, 4 * j:4 * j + 4, t * 128:(t + 1) * 128], in_=p_t[:]),
                                 reads=[p_r], writes=[x1T[1]])
                        if moe:
                            for j in range(4):
                                p_t, p_r = tpf[j % 2]
                                for k in range(4):
                                    c = 4 * j + k
                                    P.op("pe", lambda e: e.transpose(out=p_t[:, k, :], in_=x_t[:, c * 128:(c + 1) * 128], identity=idf[0][:]),
                                         reads=[x_r, idf[1]], writes=[p_r] if k == 0 else [])
                                p_r.w = ("s_pe", P.engs["pe"].count)
                                P.op("act", lambda e: e.activation(out=xTf[0][:, 4 * j:4 * j + 4, :], in_=p_t[:], func=AF.Copy), reads=[p_r], writes=[xTf[1]])
                            P.mm_group(plg[1], [(lambda e, st_, sp_, c=c: e.matmul(plg[0][:], lhsT=xTf[0][:, c, :], rhs=rt[0][:, c, :], start=st_, stop=sp_))
                                                for c in range(16)], reads=[xTf[1], rt[1]])
                            P.op("dve", lambda e: e.tensor_copy(out=lg[0][:], in_=plg[0][:]), reads=[plg[1]], writes=[lg[1]])
                            P.op("dve", lambda e: e.max(out=m8[0][:], in_=lg[0][:]), reads=[lg[1]], writes=[m8[1]])
                            P.op("dve", lambda e: e.tensor_tensor(out=gv[0][:, 0:1], in0=m8[0][:, 1:2], in1=m8[0][:, 0:1], op=ALU.subtract), reads=[m8[1]], writes=[gv[1]])
                            P.op("act", lambda e: e.activation(out=gv[0][:, 1:2], in_=gv[0][:, 0:1], func=AF.Exp), reads=[gv[1]], writes=[gv[1]])
                            P.op("dve", lambda e: e.tensor_scalar(out=gv[0][:, 2:3], in0=gv[0][:, 1:2], scalar1=1.0, scalar2=None, op0=ALU.add), reads=[gv[1]], writes=[gv[1]])
                            P.op("dve", lambda e: e.reciprocal(out=gv[0][:, 2:3], in_=gv[0][:, 2:3]), reads=[gv[1]], writes=[gv[1]])
                            P.op("dve", lambda e: e.tensor_tensor(out=gv[0][:, 3:4], in0=gv[0][:, 1:2], in1=gv[0][:, 2:3], op=ALU.mult), reads=[gv[1]], writes=[gv[1]])
                            o_t, o_r = go[t % 2]
                            P.op("dve", lambda e: e.tensor_scalar(out=o_t[:], in0=lg[0][:], scalar1=m8[0][:, 0:1], scalar2=gv[0][:, 2:3],
                                                                  op0=ALU.is_equal, op1=ALU.mult), reads=[lg[1], m8[1], gv[1]], writes=[o_r])
                            P.op("dve", lambda e: e.tensor_scalar(out=g2[0][:], in0=lg[0][:], scalar1=m8[0][:, 1:2], scalar2=gv[0][:, 3:4],
                                                                  op0=ALU.is_equal, op1=ALU.mult), reads=[lg[1], m8[1], gv[1]], writes=[g2[1]])
                            P.op("dve", lambda e: e.tensor_tensor(out=o_t[:], in0=o_t[:], in1=g2[0][:], op=ALU.add), reads=[o_r, g2[1]], writes=[o_r])
                            P.dma("sp", d["gates"].ap()[t0 + t * 128:t0 + (t + 1) * 128, :], o_t[:], reads=[o_r])
            if not moe:
                with P.scope():
                    aT = P.sb([128, NFC, GT], BF16, "aT")
                    pools = ffn_pools(P)
                    ffn_group(P, x1T[0], x1T[1], GT, d["wg"], d["wu"], d["wd"], aT[0], aT[1], d["fT"], t0, pools=pools)
    P.finish()
    return P


def build_D(N):
    P = Prog()
    d = {}
    d["xgT"] = P.dram("xgT", [D, N], BF16, kind="ExternalInput")[0]
    d["gw"] = P.dram("gw", [128, N], F32, kind="ExternalInput")[0]
    d["wg"] = P.dram("wg", [D, DFF], F32, kind="ExternalInput")[0]
    d["wu"] = P.dram("wu", [D, DFF], F32, kind="ExternalInput")[0]
    d["wd"] = P.dram("wd", [DFF, D], F32, kind="ExternalInput")[0]
    d["yT"] = P.dram("yT", [D, N], F32, kind="ExternalOutput")[0]
    aT = P.sb([128, NFC, 1024], BF16, "aT")
    pools = ffn_pools(P)
    xs = [P.sb([128, 16, 1024], BF16, "xg%d" % i) for i in range(1)]
    gws = [P.sb([128, 1024], F32, "gw%d" % i) for i in range(1)]
    t0 = 0
    gi = 0
    while t0 < N:
        n = min(1024, N - t0)
        x_t, x_r = xs[0]
        w_t, w_r = gws[0]
        gi += 1
        for q in range(4):
            P.dma("sp", x_t[:, 4 * q:4 * q + 4, 0:n], d["xgT"].ap().rearrange("(c p) t -> p c t", p=128)[:, 4 * q:4 * q + 4, t0:t0 + n], writes=[x_r])
        P.dma("sp", w_t[:, 0:n], d["gw"].ap()[:, t0:t0 + n], writes=[w_r])
        ffn_group(P, x_t, x_r, n, d["wg"], d["wu"], d["wd"], aT[0], aT[1], d["yT"], t0, gw=w_t, gw_r=w_r, pools=pools)
        t0 += n
    P.finish()
    return P


NC_A = 4
NC_C = 4
ROPE_THETA = 500000.0
_LOG = []


def _run(P, ims):
    import time as _t
    t0 = _t.time()
    res = run_bass_kernel_spmd(P.nc, ims, core_ids=list(range(len(ims))))
    _LOG.append(("run", len(ims), P.n_inst, round(_t.time() - t0, 1)))
    return res.results


def _rope_tables():
    pos = np.arange(L, dtype=np.float32)
    inv = (1.0 / (ROPE_THETA ** (np.arange(0, 32, 2, dtype=np.float32) / 32))).astype(np.float32)
    ang = pos[:, None] * inv[None, :]
    return np.cos(ang).astype(np.float32), np.sin(ang).astype(np.float32)


def kernel(**inp):
    x = np.asarray(inp["x"])[0]
    cos, sin = _rope_tables()
    identb = np.eye(128, dtype=np.float32).astype(BF)
    identf = np.eye(128, dtype=np.float32)
    progs = {}

    def prog(key, fn):
        if key not in progs:
            progs[key] = fn()
        return progs[key]

    cc = consts()
    cn = consts_nsa()
    core_c = [dict(core_consts(c), **core_consts_nsa(c)) for c in range(8)]
    memT = np.ascontiguousarray(np.asarray(inp["mem"])[0].T)
    addends = [x]
    lng, lnb = np.asarray(inp["ln_in_g"]), np.asarray(inp["ln_in_b"])
    tokA = L // NC_A
    tokC = L // NC_C
    for i in range(4):
        PA = prog(("A", len(addends), True), lambda: build_A(len(addends), True, tokA))
        w_in = np.ascontiguousarray(np.asarray(inp["w_in"])[i])
        ims = []
        for c in range(NC_A):
            sl = slice(c * tokA, (c + 1) * tokA)
            ims.append({"xin": np.stack([a[sl] for a in addends]), "lng": lng[None], "lnb": lnb[None], "ident": identb, "w": w_in,
                        "cos": cos[sl], "sin": sin[sl]})
        r = _run(PA, ims)
        xres = np.concatenate([q["xres"] for q in r], 0)
        hq = np.concatenate([q["hq"] for q in r], 0)
        u = np.concatenate([q["u"] for q in r], 0)
        del ims, r
        PB = prog(("B",), lambda: build_B())
        nw = nsa_weights(inp, i)
        wk = np.asarray(inp["mem_wk"])[i]
        wv = np.asarray(inp["mem_wv"])[i]
        ims = []
        for c in range(8):
            im = {}
            im.update(cc); im.update(cn); im.update(nw); im.update(core_c[c])
            im.update(prep_B_moba_cross(hq, c)); im.update(prep_B_nsa(hq, c)); im.update(prep_s5(inp, i, c, u))
            im["memT"] = memT; im["wk"] = wk; im["wv"] = wv
            ims.append(im)
        r = _run(PB, ims)
        ycat = np.concatenate([np.concatenate([q[k] for q in r], 0) for k in ("ym", "yn", "yx")], 1)
        ysT = np.concatenate([q["ysT"] for q in r], 0)
        del ims, r, hq, u
        moe = (i % 2 == 1)
        PC = prog(("C", moe), lambda: build_C(moe, tokC))
        ims = []
        for c in range(NC_C):
            sl = slice(c * tokC, (c + 1) * tokC)
            im = {"xres": xres[sl], "ysT": np.ascontiguousarray(ysT[:, sl]), "yT": np.ascontiguousarray(ycat[sl].T),
                  "wglu": np.asarray(inp["ssm_w_glu"])[i], "wo": np.asarray(inp["w_o"])[i],
                  "lng": np.asarray(inp["ln1_g"])[i][None], "lnb": np.asarray(inp["ln1_b"])[i][None], "identb": identb}
            if moe:
                im["identf"] = identf
                im["router"] = np.asarray(inp["moe_router"])[i // 2]
            else:
                im["wg"] = np.asarray(inp["ffn_w_gate"])[i // 2]
                im["wu"] = np.asarray(inp["ffn_w_up"])[i // 2]
                im["wd"] = np.asarray(inp["ffn_w_down"])[i // 2]
            ims.append(im)
        r = _run(PC, ims)
        ax1 = np.concatenate([q["ax1"] for q in r], 0)
        del xres, ycat, ysT
        if not moe:
            f = np.concatenate([np.ascontiguousarray(q["fT"].T) for q in r], 0)
            addends = [ax1, f]
        else:
            x1b = np.concatenate([q["x1b"] for q in r], 0)
            gates = np.concatenate([q["gates"] for q in r], 0)
            del ims, r
            tok_idx, exp_idx = np.nonzero(gates)
            assert tok_idx.shape[0] == 2 * L, "router: expected exactly two experts per token"
            pair = [np.nonzero(exp_idx == e)[0] for e in range(8)]
            nmax = max(len(p) for p in pair)
            N = -(-nmax // 512) * 512
            PD = prog(("D", N), lambda: build_D(N))
            ims = []
            for e in range(8):
                toks = tok_idx[pair[e]]
                xg = np.zeros((N, D), BF)
                xg[:len(toks)] = x1b[toks]
                gw = np.zeros((N,), np.float32)
                gw[:len(toks)] = gates[toks, e]
                ims.append({"xgT": np.ascontiguousarray(xg.T), "gw": np.ascontiguousarray(np.broadcast_to(gw[None], (128, N))),
                            "wg": np.asarray(inp["moe_w_gate"])[i // 2][e], "wu": np.asarray(inp["moe_w_up"])[i // 2][e],
                            "wd": np.asarray(inp["moe_w_down"])[i // 2][e]})
            r = _run(PD, ims)
            Y = [np.zeros((L, D), np.float32), np.zeros((L, D), np.float32)]
            for e in range(8):
                toks = tok_idx[pair[e]]
                slot = pair[e] % 2
                ye = r[e]["yT"].T[:len(toks)]
                for k in range(2):
                    m = slot == k
                    Y[k][toks[m]] = ye[m]
            addends = [ax1, Y[0], Y[1]]
        del ims, r
        lng, lnb = np.asarray(inp["ln2_g"])[i], np.asarray(inp["ln2_b"])[i]
    PF = prog(("A", len(addends), False), lambda: build_A(len(addends), False, tokA))
    ims = []
    for c in range(NC_A):
        sl = slice(c * tokA, (c + 1) * tokA)
        ims.append({"xin": np.stack([a[sl] for a in addends]), "lng": lng[None], "lnb": lnb[None]})
    r = _run(PF, ims)
    out = np.concatenate([q["xres"] for q in r], 0)
    return out[None].astype(np.float32)
```

```python
from contextlib import ExitStack
import numpy as np
import concourse.bass as bass
import concourse.mybir as mybir
from concourse.bass_utils import run_bass_kernel_spmd

F32 = mybir.dt.float32
BF16 = mybir.dt.bfloat16
I32 = mybir.dt.int32
U32 = mybir.dt.uint32
ALU = mybir.AluOpType
AF = mybir.ActivationFunctionType
AX = mybir.AxisListType

N_DMA_SEMS = 48


class Res:
    __slots__ = ("name", "w", "r")

    def __init__(self, name):
        self.name = name
        self.w = None
        self.r = {}


class Eng:
    def __init__(self, name, be, sem):
        self.name = name
        self.be = be
        self.sem = sem
        self.count = 0
        self.waited = {}


class Prog:
    def __init__(self):
        self.nc = bass.Bass("TRN2", target_bir_lowering=False)
        self.es = ExitStack()
        self.cur = self.es
        nc = self.nc
        self.sems = {}
        self.engs = {}
        for name, be in (("pe", nc.tensor), ("dve", nc.vector), ("act", nc.scalar),
                         ("pool", nc.gpsimd), ("sp", nc.sync)):
            sem = self.es.enter_context(nc.semaphore("s_" + name))
            self.sems["s_" + name] = sem
            self.engs[name] = Eng(name, be, sem)
        self.dma_sems = []
        self.dma_q = {}
        for qn, cnt in (("sp", 24), ("pool", 12), ("act", 8)):
            lst = []
            for i in range(cnt):
                k = "d%s%d" % (qn, i)
                self.sems[k] = self.es.enter_context(nc.semaphore(k))
                slot = [k, 0]
                self.dma_sems.append(slot)
                lst.append(slot)
            self.dma_q[qn] = [lst, 0]
        self.n_inst = 0
        self.n_wait = 0
        self._uid = 0

    def sb(self, shape, dt, name=None):
        self._uid += 1
        name = "sb_%s_%d" % (name or "t", self._uid)
        t = self.cur.enter_context(self.nc.sbuf_tensor(name, list(shape), dt))
        return t, Res(name)

    def ps(self, shape, dt, name=None):
        self._uid += 1
        name = "ps_%s_%d" % (name or "p", self._uid)
        t = self.cur.enter_context(self.nc.psum_tensor(name, list(shape), dt))
        return t, Res(name)

    def barrier(self):
        evs = [("s_" + n, en.count) for n, en in self.engs.items() if en.count]
        evs += [(k, v) for k, v in self.dma_sems if v]
        for e in self.engs.values():
            for ev in evs:
                self._wait(e, ev)

    def scope(self):
        prog = self

        class _S:
            def __enter__(s):
                s.prev = prog.cur
                prog.cur = ExitStack()
                return s

            def __exit__(s, *a):
                prog.barrier()
                prog.cur.close()
                prog.cur = s.prev
                return False
        return _S()

    def dram(self, name, shape, dt, kind="Internal"):
        t = self.nc.dram_tensor(name, list(shape), dt, kind=kind)
        return t, Res(name)

    def _wait(self, e, ev):
        if ev is None:
            return
        k, v = ev
        if e.waited.get(k, 0) >= v:
            return
        e.be.wait_ge(self.sems[k], v)
        e.waited[k] = v
        self.n_wait += 1

    def _deps(self, e, reads, writes):
        for r in reads:
            self._wait(e, r.w)
        for r in writes:
            self._wait(e, r.w)
            for k, v in r.r.items():
                self._wait(e, (k, v))

    def _mark(self, ev, reads, writes):
        k, v = ev
        for r in reads:
            if r.r.get(k, 0) < v:
                r.r[k] = v
        for r in writes:
            r.w = ev
            r.r = {}

    def op(self, eng, fn, reads=(), writes=()):
        e = self.engs[eng]
        self._deps(e, reads, writes)
        ins = fn(e.be)
        e.count += 1
        ins.then_inc(e.sem, 1)
        ev = ("s_" + eng, e.count)
        self._mark(ev, reads, writes)
        self.n_inst += 1
        return ev

    def op_nosync(self, eng, fn):
        e = self.engs[eng]
        ins = fn(e.be)
        self.n_inst += 1
        return ins

    def mm_group(self, out_res, mms, reads, eng="pe"):
        n = len(mms)
        for i, fn in enumerate(mms):
            f = (lambda e, fn=fn, i=i: fn(e, i == 0, i == n - 1))
            if i == 0:
                ev = self.op(eng, f, reads=reads, writes=[out_res])
            elif i == n - 1:
                ev = self.op(eng, f, reads=reads, writes=[])
                out_res.w = ev
            else:
                self.op_nosync(eng, f)
        return ev

    def dma(self, eng, out, in_, reads=(), writes=(), **kw):
        e = self.engs[eng]
        self._deps(e, reads, writes)
        q = self.dma_q[eng]
        slot = q[0][q[1] % len(q[0])]
        q[1] += 1
        k, v = slot
        if v:
            self._wait(e, (k, v))
        ins = e.be.dma_start(out=out, in_=in_, **kw)
        slot[1] = v + 16
        ins.then_inc(self.sems[k], 16)
        ev = (k, v + 16)
        self._mark(ev, reads, writes)
        self.n_inst += 1
        return ev

    def finish(self, final_res=()):
        e = self.engs["sp"]
        for k, v in self.dma_sems:
            if v:
                self._wait(e, (k, v))
        for r in final_res:
            self._wait(e, r.w)
        for name, en in self.engs.items():
            if en.count:
                self._wait(e, ("s_" + name, en.count))
        self.es.close()
        return self.nc


import numpy as np, ml_dtypes
BF = ml_dtypes.bfloat16
BIG = 30000.0
L = 16384; TOK = 2048; INW = 3852

def consts():
    c = {}
    c["ident"] = np.eye(128, dtype=np.float32).astype(BF)
    x = np.arange(8192)
    c["eall"] = (x[None, :] // 64 == np.arange(128)[:, None]).astype(np.float32).astype(BF)
    j = np.arange(4)[:, None, None]; p = np.arange(128)[None, :, None]; q = np.arange(512)[None, None, :]
    key = 128 * j + p
    c["dmoba"] = np.where((key // 256 == q // 256) & (key <= q), 0.0, -BIG).astype(np.float32).astype(BF)
    return c

def core_idx(c):
    segs = [c, 15 - c, 16 + c, 31 - c]
    return np.concatenate([np.arange(512 * s, 512 * (s + 1)) for s in segs])


def core_consts(c):
    d = {}
    pos = core_idx(c)
    own = pos // 256
    d["gmask"] = np.where(np.arange(64)[None, :] < own[:, None], 0.0, -1e30).astype(np.float32)
    return d

def prep_B_moba_cross(hq, c):
    sl = core_idx(c)
    d = {}
    mq = hq[sl, 512:1024]; mk = hq[:, 1024:1536]; mv = hq[:, 1536:2048]
    d["mqT"] = np.ascontiguousarray(mq.reshape(2048, 4, 128).transpose(1, 2, 0))
    d["mkT"] = np.ascontiguousarray(mk.reshape(L, 4, 128).transpose(1, 2, 0))
    d["mv"] = np.ascontiguousarray(mv.reshape(128, 128, 4, 128).transpose(2, 1, 0, 3))
    d["mkTl"] = np.ascontiguousarray(d["mkT"][:, :, sl])
    d["mvl"] = np.ascontiguousarray(mv[sl].reshape(16, 128, 4, 128).transpose(2, 1, 0, 3))
    d["xqT"] = np.ascontiguousarray(hq[sl, 3340:3852].reshape(2048, 4, 128).transpose(1, 2, 0))
    return d


def consts_nsa():
    c = {}
    p = np.arange(128)[:, None, None]; ct = np.arange(8)[None, :, None]; s = np.arange(256)[None, None, :]
    cc = 128 * ct + p
    c["amat"] = ((cc >= 4 * s - 1) & (cc <= 4 * s + 3) & (cc <= 1022)).astype(np.float32).astype(BF)
    p = np.arange(128)[:, None]; q = np.tile(np.arange(128), 4)[None, :]
    c["v16"] = (16.0 * p - q).astype(np.float32)
    c["dsel"] = np.where((p // 64 == q // 64) & (p <= q), 0.0, -BIG).astype(np.float32).astype(BF)
    return c

def core_consts_nsa(c):
    d = {}
    pos = core_idx(c)
    own = pos // 64
    s = np.arange(256)[None, :]
    d["addm"] = np.where(s < own[:, None], 0.0, np.where(s == own[:, None], 1e9, -1e30)).astype(np.float32)
    thr = np.zeros((128, 128), np.float32)
    for t in range(16):
        for ct in range(8):
            thr[:, t * 8 + ct] = pos[128 * t] - 2048 * ct - 31
    d["cthr"] = thr
    wm = np.zeros((5, 128, 5, 512), np.float32)
    p = np.arange(128)[:, None, None]; j = np.arange(5)[None, :, None]; q = np.tile(np.arange(128), 4)[None, None, :]
    kp = (j - 4) * 128 + p
    for m in range(5):
        ok = (kp <= q) & (kp > q - 512)
        if m < 4:
            ok = ok & (pos[128 * m] + kp >= 0)
        wm[m] = np.where(ok, 0.0, -BIG)
    d["wmask"] = wm.astype(BF)
    return d

def prep_B_nsa(hq, c):
    sl = core_idx(c)
    d = {}
    d["nqT"] = np.ascontiguousarray(hq[sl, 2048:2560].reshape(16, 128, 4, 128).transpose(3, 0, 2, 1).reshape(128, 16, 512))
    d["nqrT"] = np.ascontiguousarray(hq[sl, 3852:4364].reshape(16, 128, 4, 128).transpose(3, 0, 2, 1).reshape(128, 16, 512))
    d["kcmpT"] = np.ascontiguousarray(hq[:, 2560:2688].T)
    d["vcmpT"] = np.ascontiguousarray(hq[:, 2688:2816].T)
    d["kselT"] = np.ascontiguousarray(hq[:, 2816:2944].T)
    d["vsel"] = np.ascontiguousarray(hq[:, 2944:3072].reshape(128, 128, 128).transpose(1, 0, 2))
    d["kselTl"] = np.ascontiguousarray(d["kselT"][:, sl])
    d["vsell"] = np.ascontiguousarray(hq[sl, 2944:3072].reshape(16, 128, 128).transpose(1, 0, 2))
    kw = np.zeros((4096, 128), hq.dtype); vw = np.zeros((4096, 128), hq.dtype)
    for a in range(4):
        lo = int(sl[512 * a]) - 512
        src = slice(max(lo, 0), lo + 1024)
        kw[1024 * a + max(0, -lo):1024 * (a + 1)] = hq[src, 3072:3200]; vw[1024 * a + max(0, -lo):1024 * (a + 1)] = hq[src, 3200:3328]
    d["kwinT"] = np.ascontiguousarray(kw.T)
    d["vwin"] = np.ascontiguousarray(vw.reshape(32, 128, 128).transpose(1, 0, 2))
    d["gates"] = np.ascontiguousarray(hq[sl, 3328:3340])
    return d

def nsa_weights(dd, i):
    return {"ck1": dd["nsa_ck1"][i], "cv1": dd["nsa_cv1"][i], "ck2": dd["nsa_ck2"][i], "cv2": dd["nsa_cv2"][i],
            "poskT": np.ascontiguousarray(dd["nsa_pos_k"][i].T), "posvT": np.ascontiguousarray(dd["nsa_pos_v"][i].T)}


def prep_s5(dd, i, c, u_all):
    d = {}
    g0 = 4 * c
    are = dd["ssm_a_re"][i][g0:g0 + 4]; aim = dd["ssm_a_im"][i][g0:g0 + 4]; ldt = dd["ssm_log_dt"][i][g0:g0 + 4]
    col = np.zeros((128, 2, 3), np.float32); row = np.zeros((32, 3, 256), np.float32)
    for pr in range(2):
        for g2 in range(2):
            g = 2 * pr + g2
            col[g2 * 64:(g2 + 1) * 64, pr, 0] = are[g]; col[g2 * 64:(g2 + 1) * 64, pr, 1] = aim[g]; col[g2 * 64:(g2 + 1) * 64, pr, 2] = ldt[g]
            row[:, 0, pr * 128 + g2 * 64: pr * 128 + (g2 + 1) * 64] = are[g][None]
            row[:, 1, pr * 128 + g2 * 64: pr * 128 + (g2 + 1) * 64] = aim[g][None]
            row[:, 2, pr * 128 + g2 * 64: pr * 128 + (g2 + 1) * 64] = ldt[g]
    d["s5col"] = col; d["s5row"] = row
    bre = np.zeros((32, 256), np.float32); bim = np.zeros((32, 256), np.float32)
    cre = np.zeros((128, 2, 32), np.float32); cim = np.zeros((128, 2, 32), np.float32)
    dv = np.zeros((32, 2), np.float32)
    for pr in range(2):
        for g2 in range(2):
            g = g0 + 2 * pr + g2
            bre[g2 * 16:(g2 + 1) * 16, pr * 128 + g2 * 64: pr * 128 + (g2 + 1) * 64] = dd["ssm_b_re"][i][g].T
            bim[g2 * 16:(g2 + 1) * 16, pr * 128 + g2 * 64: pr * 128 + (g2 + 1) * 64] = dd["ssm_b_im"][i][g].T
            cre[g2 * 64:(g2 + 1) * 64, pr, g2 * 16:(g2 + 1) * 16] = dd["ssm_c_re"][i][g].T
            cim[g2 * 64:(g2 + 1) * 64, pr, g2 * 16:(g2 + 1) * 16] = dd["ssm_c_im"][i][g].T
            dv[g2 * 16:(g2 + 1) * 16, pr] = dd["ssm_d"][i][g * 16:(g + 1) * 16]
    d["bTre"] = bre; d["bTim"] = bim; d["cTre"] = cre; d["cTim"] = cim; d["dvec"] = dv
    d["kk"] = np.tile(np.arange(1025, dtype=np.float32)[None], (128, 1))
    d["uT"] = np.ascontiguousarray(u_all[:, 64 * c:64 * (c + 1)].T.reshape(2, 32, L))
    return d


D = 2048
TOK = 2048
NT = TOK // 128
INW = 3852
HQW = INW + 512
LN_EPS = 1e-5


def layer_norm_tiles(P, xin_d, n_add, lng_d, lnb_d, ident_d, xres_d, xres_r, xT, xT_r, want_T, NT, row0=0):
    nc = P.nc
    g_t, g_r = P.sb([128, D], F32, "ln_g")
    b_t, b_r = P.sb([128, D], F32, "ln_b")
    P.dma("sp", g_t[:], lng_d.ap()[0:1, :].to_broadcast([128, D]), writes=[g_r])
    P.dma("sp", b_t[:], lnb_d.ap()[0:1, :].to_broadcast([128, D]), writes=[b_r])
    eps_t, eps_r = P.sb([128, 1], F32, "eps")
    P.op("pool", lambda e: e.memset(eps_t[:], LN_EPS), writes=[eps_r])
    if want_T:
        id_t, id_r = P.sb([128, 128], BF16, "ident")
        P.dma("sp", id_t[:], ident_d.ap(), writes=[id_r])
    xb = [P.sb([128, D], F32, "xa%d" % i) for i in range(3)]
    xacc = [P.sb([128, D], F32, "xacc%d" % i) for i in range(2)] if n_add > 1 else None
    xo = [P.sb([128, D], F32, "xo%d" % i) for i in range(2)]
    xbf = [P.sb([128, D], BF16, "xbf%d" % i) for i in range(2)] if want_T else None
    st = [P.sb([128, 4, 6], F32, "bst%d" % i) for i in range(2)]
    mv = [P.sb([128, 2], F32, "mv%d" % i) for i in range(2)]
    sd = [P.sb([128, 1], F32, "sd%d" % i) for i in range(2)]
    tp = [P.ps([128, 4, 128], BF16, "tp%d" % i) for i in range(2)] if want_T else None
    ntp = 0
    items = [(t, a) for t in range(NT) for a in range(n_add)]

    def load(i):
        t, a = items[i]
        q_t, q_r = xb[i % 3]
        P.dma("sp", q_t[:], xin_d.ap()[a, row0 + t * 128:row0 + (t + 1) * 128, :], writes=[q_r])
    load(0)
    for i, (t, a) in enumerate(items):
        if i + 1 < len(items):
            load(i + 1)
        q_t, q_r = xb[i % 3]
        if n_add == 1:
            x_t, x_r = q_t, q_r
        else:
            x_t, x_r = xacc[t % 2]
            if a == 0:
                P.op("pool", lambda e: e.tensor_copy(out=x_t[:], in_=q_t[:]), reads=[q_r], writes=[x_r])
            else:
                eng = "dve" if a % 2 else "pool"
                P.op(eng, lambda e: e.tensor_tensor(out=x_t[:], in0=x_t[:], in1=q_t[:], op=ALU.add),
                     reads=[q_r, x_r], writes=[x_r])
            if a < n_add - 1:
                continue
        s_t, s_r = st[t % 2]
        for c in range(4):
            P.op("dve", lambda e: e.bn_stats(out=s_t[:, c, :], in_=x_t[:, c * 512:(c + 1) * 512]),
                 reads=[x_r], writes=[s_r])
        m_t, m_r = mv[t % 2]
        P.op("dve", lambda e: e.bn_aggr(out=m_t[:], in_=s_t[:].rearrange("p a b -> p (a b)")), reads=[s_r], writes=[m_r])
        d_t, d_r = sd[t % 2]
        P.op("act", lambda e: e.activation(out=d_t[:], in_=m_t[:, 1:2], func=AF.Sqrt, bias=eps_t[:], scale=1.0),
             reads=[m_r, eps_r], writes=[d_r])
        P.op("dve", lambda e: e.reciprocal(out=d_t[:], in_=d_t[:]), reads=[d_r], writes=[d_r])
        o_t, o_r = xo[t % 2]
        P.op("dve", lambda e: e.tensor_scalar(out=o_t[:], in0=x_t[:], scalar1=m_t[:, 0:1], scalar2=d_t[:, 0:1],
                                              op0=ALU.subtract, op1=ALU.mult), reads=[x_r, m_r, d_r], writes=[o_r])
        P.op("pool", lambda e: e.tensor_tensor(out=o_t[:], in0=o_t[:], in1=g_t[:], op=ALU.mult),
             reads=[o_r, g_r], writes=[o_r])
        P.op("dve", lambda e: e.tensor_tensor(out=o_t[:], in0=o_t[:], in1=b_t[:], op=ALU.add),
             reads=[o_r, b_r], writes=[o_r])
        P.dma("sp", xres_d.ap()[row0 + t * 128:row0 + (t + 1) * 128, :], o_t[:], reads=[o_r])
        if want_T:
            f_t, f_r = xbf[t % 2]
            P.op("act", lambda e: e.activation(out=f_t[:], in_=o_t[:], func=AF.Copy), reads=[o_r], writes=[f_r])
            for j in range(4):
                p_t, p_r = tp[ntp % 2]
                ntp += 1
                for k in range(4):
                    c = 4 * j + k
                    P.op("pe", lambda e: e.transpose(out=p_t[:, k, :], in_=f_t[:, c * 128:(c + 1) * 128], identity=id_t[:]),
                         reads=[f_r, id_r], writes=[p_r] if k == 0 else [])
                p_r.w = ("s_pe", P.engs["pe"].count)
                eng = "dve" if j % 2 == 0 else "act"
                if eng == "dve":
                    P.op("dve", lambda e: e.tensor_copy(out=xT[:, 4 * j:4 * j + 4, t * 128:(t + 1) * 128], in_=p_t[:]),
                         reads=[p_r], writes=[xT_r])
                else:
                    P.op("act", lambda e: e.activation(out=xT[:, 4 * j:4 * j + 4, t * 128:(t + 1) * 128], in_=p_t[:], func=AF.Copy),
                         reads=[p_r], writes=[xT_r])


ROPE_SLOTS = {1: [0, 1, 2, 3], 2: [0, 1, 2, 3], 5: [2], 6: [0]}


def rope_inplace(P, s_t, s_r, slots, cs_t, sn_t, cs_r, t, tmp):
    (ta, ra), (tb, rb) = tmp
    for s in slots:
        b = s * 128
        t1 = s_t[:, b:b + 16]
        t2 = s_t[:, b + 16:b + 32]
        c = cs_t[:, t, :]
        sn = sn_t[:, t, :]
        P.op("dve", lambda e: e.tensor_tensor(out=ta[:, 0:16], in0=t1, in1=c, op=ALU.mult), reads=[s_r, cs_r], writes=[ra])
        P.op("dve", lambda e: e.tensor_tensor(out=ta[:, 16:32], in0=t1, in1=sn, op=ALU.mult), reads=[s_r, cs_r], writes=[ra])
        P.op("dve", lambda e: e.tensor_tensor(out=tb[:, 0:16], in0=t2, in1=sn, op=ALU.mult), reads=[s_r, cs_r], writes=[rb])
        P.op("dve", lambda e: e.tensor_tensor(out=tb[:, 16:32], in0=t2, in1=c, op=ALU.mult), reads=[s_r, cs_r], writes=[rb])
        P.op("dve", lambda e: e.tensor_tensor(out=t1, in0=ta[:, 0:16], in1=tb[:, 0:16], op=ALU.subtract),
             reads=[ra, rb], writes=[s_r])
        P.op("dve", lambda e: e.tensor_tensor(out=t2, in0=ta[:, 16:32], in1=tb[:, 16:32], op=ALU.add),
             reads=[ra, rb], writes=[s_r])


def build_A(n_add, do_proj, TOK=2048):
    GTA = 2048
    NT = GTA // 128
    P = Prog()
    nc = P.nc
    xin_d, _ = P.dram("xin", [n_add, TOK, D], F32, kind="ExternalInput")
    lng_d, _ = P.dram("lng", [1, D], F32, kind="ExternalInput")
    lnb_d, _ = P.dram("lnb", [1, D], F32, kind="ExternalInput")
    xres_d, xres_r = P.dram("xres", [TOK, D], F32, kind="ExternalOutput")
    ident_d = None
    if do_proj:
        ident_d, _ = P.dram("ident", [128, 128], BF16, kind="ExternalInput")
        w_d, _ = P.dram("w", [D, INW], F32, kind="ExternalInput")
        cos_d, _ = P.dram("cos", [TOK, 16], F32, kind="ExternalInput")
        sin_d, _ = P.dram("sin", [TOK, 16], F32, kind="ExternalInput")
        hq_d, hq_r = P.dram("hq", [TOK, HQW], BF16, kind="ExternalOutput")
        u_d, u_r = P.dram("u", [TOK, 512], F32, kind="ExternalOutput")
    for grp in range(TOK // GTA):
        row0 = grp * GTA
        with P.scope():
            xT = xT_r = None
            if do_proj:
                xT, xT_r = P.sb([128, 16, GTA], BF16, "xT")
            with P.scope():
                layer_norm_tiles(P, xin_d, n_add, lng_d, lnb_d, ident_d, xres_d, xres_r, xT, xT_r, do_proj, NT, row0)
            if do_proj:
                cs_t, cs_r = P.sb([128, NT, 16], F32, "cos")
                sn_t, sn_r = P.sb([128, NT, 16], F32, "sin")
                P.dma("sp", cs_t[:], cos_d.ap()[row0:row0 + GTA, :].rearrange("(t p) f -> p t f", p=128), writes=[cs_r])
                P.dma("sp", sn_t[:], sin_d.ap()[row0:row0 + GTA, :].rearrange("(t p) f -> p t f", p=128), writes=[cs_r])
                wv = w_d.ap().rearrange("(c p) f -> p c f", p=128)
                wt = [P.sb([128, 16, 512], BF16, "wt%d" % i) for i in range(2)]
                acc = [P.ps([128, 512], F32, "acc%d" % i) for i in range(3)]
                stg = [P.sb([128, 512], F32, "stg%d" % i) for i in range(3)]
                ob = [P.sb([128, 512], BF16, "ob%d" % i) for i in range(3)]
                tmp = (P.sb([128, 32], F32, "rta"), P.sb([128, 32], F32, "rtb"))
                n = 0

                def load_w(j):
                    fw_ = min(512, INW - 512 * j)
                    w_t, w_r = wt[j % 2]
                    for q in range(4):
                        P.dma("pool", w_t[:, 4 * q:4 * q + 4, 0:fw_], wv[:, 4 * q:4 * q + 4, 512 * j:512 * j + fw_], writes=[w_r])
                load_w(0)
                for j in range(8):
                    fw_ = min(512, INW - 512 * j)
                    w_t, w_r = wt[j % 2]
                    if j + 1 < 8:
                        load_w(j + 1)
                    for t in range(NT):
                        rows = slice(row0 + t * 128, row0 + (t + 1) * 128)
                        a_t, a_r = acc[n % 3]
                        s_t, s_r = stg[n % 3]
                        o_t, o_r = ob[n % 3]
                        n += 1
                        P.mm_group(a_r, [(lambda e, st_, sp_, k=k: e.matmul(a_t[:, 0:fw_], lhsT=xT[:, k, t * 128:(t + 1) * 128],
                                                                           rhs=w_t[:, k, 0:fw_], start=st_, stop=sp_))
                                         for k in range(16)], reads=[xT_r, w_r])
                        P.op("act", lambda e: e.activation(out=s_t[:, 0:fw_], in_=a_t[:, 0:fw_], func=AF.Copy), reads=[a_r], writes=[s_r])
                        if j == 6:
                            P.op("act", lambda e: e.activation(out=s_t[:, 256:268], in_=s_t[:, 256:268], func=AF.Sigmoid),
                                 reads=[s_r], writes=[s_r])
                        if j == 0:
                            P.dma("sp", u_d.ap()[rows, :], s_t[:], reads=[s_r])
                        if j == 4:
                            P.op("dve", lambda e: e.tensor_copy(out=o_t[:], in_=s_t[:]), reads=[s_r], writes=[o_r])
                            P.dma("sp", hq_d.ap()[rows, 2048:2560], o_t[:], reads=[o_r])
                            rope_inplace(P, s_t, s_r, [0, 1, 2, 3], cs_t, sn_t, cs_r, t, tmp)
                            o_t, o_r = ob[n % 3]
                            P.op("dve", lambda e: e.tensor_copy(out=o_t[:], in_=s_t[:]), reads=[s_r], writes=[o_r])
                            P.dma("sp", hq_d.ap()[rows, INW:INW + 512], o_t[:], reads=[o_r])
                            continue
                        if j in ROPE_SLOTS:
                            rope_inplace(P, s_t, s_r, ROPE_SLOTS[j], cs_t, sn_t, cs_r, t, tmp)
                        P.op("dve", lambda e: e.tensor_copy(out=o_t[:, 0:fw_], in_=s_t[:, 0:fw_]), reads=[s_r], writes=[o_r])
                        P.dma("sp", hq_d.ap()[rows, 512 * j:512 * j + fw_], o_t[:, 0:fw_], reads=[o_r])
    P.finish()
    return P


SCALE = 128 ** -0.5
BIG = 30000.0
L = 16384
TOK = 2048
NKT = L // 128
TS5 = 1024
KMAX = [32, 64, 96, 128]
CMAX = [2, 4, 6, 8]


class Ctx:
    pass


def attn_ctx(P):
    C = Ctx()
    C.sp = [P.ps([128, 512], F32, "sp%d" % i) for i in range(2)]
    C.ob = [P.ps([128, 512], F32, "ob%d" % i) for i in range(4)]
    C.mp = P.ps([128, 512], F32, "mp")
    C.tpb = P.ps([128, 2, 128], BF16, "tpb")
    C.pt = [P.sb([128, 512], BF16, "pt%d" % i) for i in range(2)]
    C.den = P.sb([128, 8], F32, "den")
    C.nsp = 0
    C.npt = 0
    return C


def attn_run(P, C, qT, q_reads, steps, ncols):
    n = len(steps)

    def emit_qk(i):
        kT, extras, v, rd = steps[i][:4]
        if len(steps[i]) > 4:
            steps[i][4]()
        s_t, s_r = C.sp[C.nsp % 2]
        C.nsp += 1
        mms = [lambda e, st, sp_: e.matmul(s_t[:], lhsT=kT, rhs=qT, start=st, stop=sp_)]
        for (l, r) in extras:
            mms.append(lambda e, st, sp_, l=l, r=r: e.matmul(s_t[:], lhsT=l, rhs=r, start=st, stop=sp_))
        P.mm_group(s_r, mms, reads=list(q_reads) + list(rd))
        return s_t, s_r

    cur = emit_qk(0)
    for i in range(n):
        nxt = emit_qk(i + 1) if i + 1 < n else None
        s_t, s_r = cur
        p_t, p_r = C.pt[C.npt % 2]
        C.npt += 1
        P.op("act", lambda e: e.activation(out=p_t[:], in_=s_t[:], func=AF.Exp, scale=SCALE), reads=[s_r], writes=[p_r])
        kT, extras, v, rd = steps[i][:4]
        for r in range(4):
            o_t, o_r = C.ob[r]
            vr = v[r] if isinstance(v, list) else v
            fn = lambda e: e.matmul(o_t[:, 0:ncols], lhsT=p_t[:, 128 * r:128 * (r + 1)], rhs=vr, start=(i == 0), stop=(i == n - 1))
            if i == 0:
                P.op("pe", fn, reads=[p_r] + list(rd), writes=[o_r])
            else:
                ev = P.op("pe", fn, reads=[p_r] + list(rd), writes=[])
                if i == n - 1:
                    o_r.w = ev
        cur = nxt


def evac_den(P, C, r, col):
    o_t, o_r = C.ob[r]
    d_t, d_r = C.den
    P.op("dve", lambda e: e.tensor_scalar(out=d_t[:, r:r + 1], in0=o_t[:, col:col + 1], scalar1=1e-30, scalar2=None, op0=ALU.max),
         reads=[o_r], writes=[d_r])
    P.op("dve", lambda e: e.reciprocal(out=d_t[:, r:r + 1], in_=d_t[:, r:r + 1]), reads=[d_r], writes=[d_r])


def load_consts(P, C, d):
    C.ident = P.sb([128, 128], BF16, "identb")
    P.dma("sp", C.ident[0][:], d["ident"].ap(), writes=[C.ident[1]])
    C.eall = P.sb([128, 8192], BF16, "eall")
    P.dma("sp", C.eall[0][:], d["eall"].ap(), writes=[C.eall[1]])


def phase_cross(P, C, d, yx):
    with P.scope():
        memT = P.sb([128, 16, 256], BF16, "memT")
        wk = P.sb([128, 16, 512], BF16, "wk")
        wv = P.sb([128, 16, 512], BF16, "wv")
        for q in range(4):
            P.dma("pool", memT[0][:, 4 * q:4 * q + 4, :], d["memT"].ap().rearrange("(c p) m -> p c m", p=128)[:, 4 * q:4 * q + 4, :], writes=[memT[1]])
            P.dma("pool", wk[0][:, 4 * q:4 * q + 4, :], d["wk"].ap().rearrange("(c p) f -> p c f", p=128)[:, 4 * q:4 * q + 4, :], writes=[wk[1]])
            P.dma("pool", wv[0][:, 4 * q:4 * q + 4, :], d["wv"].ap().rearrange("(c p) f -> p c f", p=128)[:, 4 * q:4 * q + 4, :], writes=[wv[1]])
        kT = P.sb([128, 4, 256], BF16, "memkT")
        vv = P.sb([128, 2, 4, 129], BF16, "memv")
        P.op("pool", lambda e: e.memset(vv[0][:], 1.0), writes=[vv[1]])
        m_t, m_r = C.mp
        for h in range(4):
            P.mm_group(m_r, [(lambda e, st, sp_, c=c: e.matmul(m_t[:, 0:256], lhsT=wk[0][:, c, h * 128:(h + 1) * 128], rhs=memT[0][:, c, :],
                                                             start=st, stop=sp_)) for c in range(16)], reads=[wk[1], memT[1]])
            P.op("dve", lambda e: e.tensor_copy(out=kT[0][:, h, :], in_=m_t[:, 0:256]), reads=[m_r], writes=[kT[1]])
        for mt in range(2):
            P.mm_group(m_r, [(lambda e, st, sp_, c=c: e.matmul(m_t[:], lhsT=memT[0][:, c, mt * 128:(mt + 1) * 128], rhs=wv[0][:, c, :],
                                                             start=st, stop=sp_)) for c in range(16)], reads=[wv[1], memT[1]])
            P.op("dve", lambda e: e.tensor_copy(out=vv[0][:, mt, :, 0:128], in_=m_t[:].rearrange("p (h d) -> p h d", d=128)),
                 reads=[m_r], writes=[vv[1]])
        qs = [P.sb([128, TOK], BF16, "xq%d" % i) for i in range(2)]
        for h in range(4):
            q_t, q_r = qs[h % 2]
            P.dma("sp", q_t[:], d["xqT"].ap()[h], writes=[q_r])
            for a in range(4):
                steps = [(kT[0][:, h, mt * 128:(mt + 1) * 128], [], vv[0][:, mt, h, :], [kT[1], vv[1]]) for mt in range(2)]
                attn_run(P, C, q_t[:, a * 512:(a + 1) * 512], [q_r], steps, 129)
                for r in range(4):
                    evac_den(P, C, r, 128)
                    o_t, o_r = C.ob[r]
                    P.op("dve", lambda e: e.tensor_scalar(out=yx[0][:, 4 * a + r, h * 128:(h + 1) * 128], in0=o_t[:, 0:128],
                                                          scalar1=C.den[0][:, r:r + 1], scalar2=None, op0=ALU.mult),
                         reads=[o_r, C.den[1]], writes=[yx[1]])


def phase_moba(P, C, d, ym):
    with P.scope():
        KT = [P.sb([128, L], BF16, "KT%d" % i) for i in range(2)]
        VB = [P.sb([128, NKT, 129], BF16, "VB%d" % i) for i in range(2)]
        for i in range(2):
            P.op("pool", lambda e: e.memset(VB[i][0][:], 1.0), writes=[VB[i][1]])
        qs = [P.sb([128, TOK], BF16, "mq%d" % i) for i in range(2)]
        kl = [P.sb([128, TOK], BF16, "mkl%d" % i) for i in range(2)]
        vl = [P.sb([128, 16, 129], BF16, "mvl%d" % i) for i in range(2)]
        for i in range(2):
            P.op("pool", lambda e: e.memset(vl[i][0][:], 1.0), writes=[vl[i][1]])
        dm = P.sb([128, 4, 512], BF16, "dmoba")
        P.dma("sp", dm[0][:], d["dmoba"].ap().rearrange("j p q -> p j q"), writes=[dm[1]])
        gm = P.sb([128, 16, 64], F32, "gmask")
        P.dma("sp", gm[0][:], d["gmask"].ap().rearrange("(t p) b -> p t b", p=128), writes=[gm[1]])
        km32 = P.sb([128, 64], F32, "km32")
        kmT = P.sb([128, 64], BF16, "kmT")
        gsm = P.sb([128, 64], F32, "gsm")
        m8 = P.sb([128, 8], F32, "m8")
        t1 = P.sb([128, 64], F32, "t1")
        sel = P.sb([128, 64], F32, "sel")
        brep = P.sb([128, 256], BF16, "brep")
        bT = [[P.sb([128, 512], BF16, "bT%d_%d" % (i, ch)) for ch in range(2)] for i in range(2)]
        for h in range(4):
            K_t, K_r = KT[h % 2]
            V_t, V_r = VB[h % 2]
            for q in range(8):
                P.dma("sp", K_t[:, q * 2048:(q + 1) * 2048], d["mkT"].ap()[h, :, q * 2048:(q + 1) * 2048], writes=[K_r])
            for q in range(4):
                P.dma("sp", V_t[:, q * 32:(q + 1) * 32, 0:128], d["mv"].ap()[h, :, q * 32:(q + 1) * 32, :], writes=[V_r])
            q_t, q_r = qs[h % 2]
            kl_t, kl_r = kl[h % 2]
            vl_t, vl_r = vl[h % 2]
            P.dma("sp", q_t[:], d["mqT"].ap()[h], writes=[q_r])
            P.dma("sp", kl_t[:], d["mkTl"].ap()[h], writes=[kl_r])
            P.dma("sp", vl_t[:, :, 0:128], d["mvl"].ap()[h], writes=[vl_r])
            P.op("dve", lambda e: e.tensor_reduce(out=km32[0][:], in_=K_t[:].rearrange("p (b k) -> p b k", k=256), axis=AX.X, op=ALU.add),
                 reads=[K_r], writes=[km32[1]])
            P.op("dve", lambda e: e.tensor_scalar(out=kmT[0][:], in0=km32[0][:], scalar1=1.0 / 256, scalar2=None, op0=ALU.mult),
                 reads=[km32[1]], writes=[kmT[1]])
            for a in range(4):
                b_ = bT[a % 2]
                for r in range(4):
                    s = 4 * a + r
                    m_t, m_r = C.mp
                    P.op("pe", lambda e: e.matmul(m_t[:, 0:64], lhsT=q_t[:, s * 128:(s + 1) * 128], rhs=kmT[0][:], start=True, stop=True),
                         reads=[q_r, kmT[1]], writes=[m_r])
                    P.op("dve", lambda e: e.tensor_tensor(out=gsm[0][:], in0=m_t[:, 0:64], in1=gm[0][:, s, :], op=ALU.add),
                         reads=[m_r, gm[1]], writes=[gsm[1]])
                    P.op("dve", lambda e: e.max(out=m8[0][:], in_=gsm[0][:]), reads=[gsm[1]], writes=[m8[1]])
                    P.op("dve", lambda e: e.tensor_scalar(out=t1[0][:], in0=gsm[0][:], scalar1=-5e29, scalar2=None, op0=ALU.is_gt),
                         reads=[gsm[1]], writes=[t1[1]])
                    P.op("dve", lambda e: e.scalar_tensor_tensor(out=sel[0][:], in0=gsm[0][:], scalar=m8[0][:, 2:3], in1=t1[0][:],
                                                                 op0=ALU.is_ge, op1=ALU.mult), reads=[gsm[1], m8[1], t1[1]], writes=[sel[1]])
                    P.op("dve", lambda e: e.tensor_scalar(out=brep[0][:].rearrange("p (b j) -> p b j", j=4),
                                                          in0=sel[0][:].unsqueeze(2).to_broadcast([128, 64, 4]),
                                                          scalar1=1.0, scalar2=BIG, op0=ALU.subtract, op1=ALU.mult),
                         reads=[sel[1]], writes=[brep[1]])
                    tp_t, tp_r = C.tpb
                    for ch in range(2):
                        P.op("pe", lambda e: e.transpose(out=tp_t[:, ch, :], in_=brep[0][:, ch * 128:(ch + 1) * 128], identity=C.ident[0][:]),
                             reads=[brep[1], C.ident[1]], writes=[tp_r] if ch == 0 else [])
                    tp_r.w = ("s_pe", P.engs["pe"].count)
                    for ch in range(2):
                        P.op("dve", lambda e: e.tensor_copy(out=b_[ch][0][:, r * 128:(r + 1) * 128], in_=tp_t[:, ch, :]),
                             reads=[tp_r], writes=[b_[ch][1]])
                steps = []
                for kt in range(KMAX[a]):
                    ch, jt = kt // 64, kt % 64
                    steps.append((K_t[:, kt * 128:(kt + 1) * 128], [(C.eall[0][:, 128 * jt:128 * jt + 128], b_[ch][0][:])],
                                  V_t[:, kt, :], [K_r, V_r, C.eall[1], b_[ch][1]]))
                for j in range(4):
                    lt = 4 * a + j
                    steps.append((kl_t[:, lt * 128:(lt + 1) * 128], [(C.ident[0][:], dm[0][:, j, :])], vl_t[:, lt, :],
                                  [kl_r, vl_r, C.ident[1], dm[1]]))
                attn_run(P, C, q_t[:, a * 512:(a + 1) * 512], [q_r], steps, 129)
                for r in range(4):
                    evac_den(P, C, r, 128)
                    o_t, o_r = C.ob[r]
                    P.op("dve", lambda e: e.tensor_scalar(out=ym[0][:, 4 * a + r, h * 128:(h + 1) * 128], in0=o_t[:, 0:128],
                                                          scalar1=C.den[0][:, r:r + 1], scalar2=None, op0=ALU.mult),
                         reads=[o_r, C.den[1]], writes=[ym[1]])


def gelu_tanh(P, src_ps, src_r, hb, hb_r, dst, dst_r, tmp, n):
    (gu, gur), (gt, gtr), (gs_, gsr) = tmp
    P.op("act", lambda e: e.activation(out=gu[:, 0:n], in_=src_ps, func=AF.Identity, bias=hb, scale=1.0), reads=[src_r, hb_r], writes=[gur])
    P.op("dve", lambda e: e.tensor_tensor(out=gt[:, 0:n], in0=gu[:, 0:n], in1=gu[:, 0:n], op=ALU.mult), reads=[gur], writes=[gtr])
    P.op("dve", lambda e: e.tensor_scalar(out=gt[:, 0:n], in0=gt[:, 0:n], scalar1=0.044715, scalar2=1.0, op0=ALU.mult, op1=ALU.add),
         reads=[gtr], writes=[gtr])
    P.op("dve", lambda e: e.tensor_tensor(out=gt[:, 0:n], in0=gt[:, 0:n], in1=gu[:, 0:n], op=ALU.mult), reads=[gtr, gur], writes=[gtr])
    P.op("act", lambda e: e.activation(out=gs_[:, 0:n], in_=gt[:, 0:n], func=AF.Sigmoid, scale=1.5957691216057308), reads=[gtr], writes=[gsr])
    P.op("dve", lambda e: e.tensor_tensor(out=dst, in0=gu[:, 0:n], in1=gs_[:, 0:n], op=ALU.mult), reads=[gur, gsr], writes=[dst_r])


def nsa_compress(P, C, d, KTsrc, w1name, w2name, posname, gel):
    K_t, K_r = KTsrc
    with P.scope():
        w1 = P.sb([128, 32, 256], BF16, "w1")
        for q in range(4):
            P.dma("pool", w1[0][:, 8 * q:8 * q + 8, :], d[w1name].ap().rearrange("(i p) f -> p i f", p=128)[:, 8 * q:8 * q + 8, :], writes=[w1[1]])
        pos32 = P.sb([128, 32], F32, "pos32")
        posb = P.sb([128, 32], BF16, "posb")
        P.dma("sp", pos32[0][:], d[posname].ap(), writes=[pos32[1]])
        P.op("dve", lambda e: e.tensor_copy(out=posb[0][:], in_=pos32[0][:]), reads=[pos32[1]], writes=[posb[1]])
        hb = P.sb([128, 2], F32, "hb")
        tmp = [P.sb([128, 512], F32, "g%d" % i) for i in range(3)]
        m_t, m_r = C.mp
        for hh in range(2):
            P.mm_group(m_r, [(lambda e, st, sp_, i=i: e.matmul(m_t[:, hh:hh + 1], lhsT=w1[0][:, i, hh * 128:(hh + 1) * 128], rhs=posb[0][:, i:i + 1],
                                                             start=st, stop=sp_)) for i in range(32)], reads=[w1[1], posb[1]])
            P.op("dve", lambda e: e.tensor_copy(out=hb[0][:, hh:hh + 1], in_=m_t[:, hh:hh + 1]), reads=[m_r], writes=[hb[1]])
        for hh in range(2):
            P.op("pool", lambda e: e.memset(gel[hh][0][:, 1016:1024], 0.0), writes=[gel[hh][1]])
            for nt in range(2):
                n = 512 if nt == 0 else 511
                o_t, o_r = C.ob[2 * hh + nt]
                base = 8192 * nt
                P.mm_group(o_r, [(lambda e, st, sp_, i=i: e.matmul(o_t[:, 0:n], lhsT=w1[0][:, i, hh * 128:(hh + 1) * 128],
                                                                 rhs=K_t[:, base + i:base + i + 16 * (n - 1) + 1:16], start=st, stop=sp_))
                                 for i in range(32)], reads=[w1[1], K_r])
                gelu_tanh(P, o_t[:, 0:n], o_r, hb[0][:, hh:hh + 1], hb[1], gel[hh][0][:, nt * 512:nt * 512 + n], gel[hh][1], tmp, n)


def phase_nsa(P, C, d, yn):
    with P.scope():
        KT = [P.sb([128, L], BF16, "KTn%d" % i) for i in range(2)]
        VB = P.sb([128, NKT, 129], BF16, "VBn")
        P.op("pool", lambda e: e.memset(VB[0][:], 1.0), writes=[VB[1]])
        for q in range(8):
            P.dma("sp", KT[0][0][:, q * 2048:(q + 1) * 2048], d["kcmpT"].ap()[:, q * 2048:(q + 1) * 2048], writes=[KT[0][1]])
            P.dma("sp", KT[1][0][:, q * 2048:(q + 1) * 2048], d["vcmpT"].ap()[:, q * 2048:(q + 1) * 2048], writes=[KT[1][1]])
        for q in range(4):
            P.dma("sp", VB[0][:, q * 32:(q + 1) * 32, 0:128], d["vsel"].ap()[:, q * 32:(q + 1) * 32, :], writes=[VB[1]])
        kcT = P.sb([128, 1024], BF16, "kcT")
        RC = P.sb([128, 8, 385], BF16, "RC")
        P.op("pool", lambda e: e.memset(RC[0][:], 1.0), writes=[RC[1]])
        P.dma("sp", RC[0][:, :, 128:384], d["amat"].ap(), writes=[RC[1]])
        with P.scope():
            w2 = P.sb([128, 2, 128], BF16, "w2")
            gel = [P.sb([128, 1024], BF16, "gel%d" % i) for i in range(2)]
            P.dma("pool", w2[0][:], d["ck2"].ap().rearrange("(h p) d -> p h d", p=128), writes=[w2[1]])
            nsa_compress(P, C, d, KT[0], "ck1", "ck2", "poskT", gel)
            for nt in range(2):
                s_t, s_r = C.sp[nt]
                P.mm_group(s_r, [(lambda e, st, sp_, hh=hh: e.matmul(s_t[:], lhsT=w2[0][:, hh, :], rhs=gel[hh][0][:, nt * 512:(nt + 1) * 512],
                                                                   start=st, stop=sp_)) for hh in range(2)], reads=[w2[1], gel[0][1], gel[1][1]])
                P.op("dve", lambda e: e.tensor_copy(out=kcT[0][:, nt * 512:(nt + 1) * 512], in_=s_t[:]), reads=[s_r], writes=[kcT[1]])
            P.dma("pool", w2[0][:], d["cv2"].ap().rearrange("(h p) d -> p h d", p=128), writes=[w2[1]])
            nsa_compress(P, C, d, KT[1], "cv1", "cv2", "posvT", gel)
            m_t, m_r = C.mp
            for ct in range(8):
                P.mm_group(m_r, [(lambda e, st, sp_, hh=hh: e.matmul(m_t[:, 0:128], lhsT=gel[hh][0][:, ct * 128:(ct + 1) * 128], rhs=w2[0][:, hh, :],
                                                                   start=st, stop=sp_)) for hh in range(2)], reads=[w2[1], gel[0][1], gel[1][1]])
                P.op("dve", lambda e: e.tensor_copy(out=RC[0][:, ct, 0:128], in_=m_t[:, 0:128]), reads=[m_r], writes=[RC[1]])
        for q in range(8):
            P.dma("sp", KT[0][0][:, q * 2048:(q + 1) * 2048], d["kselT"].ap()[:, q * 2048:(q + 1) * 2048], writes=[KT[0][1]])
        ksl = P.sb([128, TOK], BF16, "ksl")
        vsl = P.sb([128, 16, 129], BF16, "vsl")
        kw = P.sb([128, 4096], BF16, "kw")
        vw = P.sb([128, 32, 129], BF16, "vw")
        P.op("pool", lambda e: e.memset(vsl[0][:], 1.0), writes=[vsl[1]])
        P.op("pool", lambda e: e.memset(vw[0][:], 1.0), writes=[vw[1]])
        P.dma("sp", ksl[0][:], d["kselTl"].ap(), writes=[ksl[1]])
        P.dma("sp", vsl[0][:, :, 0:128], d["vsell"].ap(), writes=[vsl[1]])
        P.dma("sp", kw[0][:], d["kwinT"].ap(), writes=[kw[1]])
        P.dma("sp", vw[0][:, :, 0:128], d["vwin"].ap(), writes=[vw[1]])
        wmg = P.sb([128, 5, 512], BF16, "wmg")
        wmt = P.sb([128, 5, 512], BF16, "wmt")
        P.dma("sp", wmg[0][:], d["wmask"].ap()[4], writes=[wmg[1]])
        dsel = P.sb([128, 512], BF16, "dsel")
        P.dma("sp", dsel[0][:], d["dsel"].ap(), writes=[dsel[1]])
        v16 = P.sb([128, 512], F32, "v16")
        P.dma("sp", v16[0][:], d["v16"].ap(), writes=[v16[1]])
        thr = P.sb([128, 128], F32, "thr")
        P.dma("sp", thr[0][:], d["cthr"].ap(), writes=[thr[1]])
        gts = P.sb([128, 16, 12], BF16, "gates")
        P.dma("sp", gts[0][:], d["gates"].ap().rearrange("(t p) g -> p t g", p=128), writes=[gts[1]])
        cm = [P.sb([128, 512], BF16, "cm%d" % i) for i in range(2)]
        qa = [P.sb([128, 512], BF16, "nq%d" % i) for i in range(2)]
        qr = [P.sb([128, 512], BF16, "nqr%d" % i) for i in range(2)]
        adm = [P.sb([128, 256], F32, "adm%d" % i) for i in range(2)]
        imp = P.sb([128, 256], F32, "imp")
        impm = P.sb([128, 256], F32, "impm")
        work = P.sb([128, 256], F32, "work")
        gt = P.sb([128, 256], F32, "gt")
        sl_ = P.sb([128, 256], F32, "sl")
        no = P.sb([128, 256], F32, "no")
        m8a = P.sb([128, 8], F32, "m8a")
        m8b = P.sb([128, 8], F32, "m8b")
        brow = P.sb([128, 256], BF16, "brow")
        bTn = [P.sb([128, 4, 128], BF16, "bTn%d" % i) for i in range(2)]
        acc = P.sb([128, 4, 128], F32, "acc")
        coef = P.sb([128, 4], F32, "coef")
        ncm = [0]
        for t in range(16):
            qa_t, qa_r = qa[t % 2]
            qr_t, qr_r = qr[t % 2]
            ad_t, ad_r = adm[t % 2]
            P.dma("sp", qa_t[:], d["nqT"].ap()[:, t, :], writes=[qa_r])
            P.dma("sp", qr_t[:], d["nqrT"].ap()[:, t, :], writes=[qr_r])
            P.dma("sp", ad_t[:], d["addm"].ap()[t * 128:(t + 1) * 128, :], writes=[ad_r])
            steps = []
            for ct in range(CMAX[t // 4]):
                def pre(ct=ct):
                    c_t, c_r = cm[ncm[0] % 2]
                    P.op("dve", lambda e: e.tensor_scalar(out=c_t[:], in0=v16[0][:], scalar1=thr[0][:, t * 8 + ct:t * 8 + ct + 1], scalar2=-BIG,
                                                          op0=ALU.is_gt, op1=ALU.mult), reads=[v16[1], thr[1]], writes=[c_r])
                c_t, c_r = cm[(ncm[0] + ct) % 2]
                steps.append((kcT[0][:, ct * 128:(ct + 1) * 128], [(C.ident[0][:], c_t[:])], RC[0][:, ct, :], [kcT[1], RC[1], C.ident[1], c_r],
                              (lambda pre=pre: (pre(), ncm.__setitem__(0, ncm[0] + 1)))))
            attn_run(P, C, qa_t[:], [qa_r], steps, 385)
            for h in range(4):
                evac_den(P, C, h, 384)
                o_t, o_r = C.ob[h]
                if h == 0:
                    P.op("dve", lambda e: e.tensor_scalar(out=imp[0][:], in0=o_t[:, 128:384], scalar1=C.den[0][:, 0:1], scalar2=None, op0=ALU.mult),
                         reads=[o_r, C.den[1]], writes=[imp[1]])
                else:
                    P.op("dve", lambda e: e.scalar_tensor_tensor(out=imp[0][:], in0=o_t[:, 128:384], scalar=C.den[0][:, h:h + 1], in1=imp[0][:],
                                                                 op0=ALU.mult, op1=ALU.add), reads=[o_r, C.den[1], imp[1]], writes=[imp[1]])
                P.op("dve", lambda e: e.tensor_tensor(out=coef[0][:, h:h + 1], in0=C.den[0][:, h:h + 1], in1=gts[0][:, t, 3 * h:3 * h + 1], op=ALU.mult),
                     reads=[C.den[1], gts[1]], writes=[coef[1]])
                P.op("dve", lambda e: e.tensor_scalar(out=acc[0][:, h, :], in0=o_t[:, 0:128], scalar1=coef[0][:, h:h + 1], scalar2=None, op0=ALU.mult),
                     reads=[o_r, coef[1]], writes=[acc[1]])
            P.op("dve", lambda e: e.tensor_tensor(out=impm[0][:], in0=imp[0][:], in1=ad_t[:], op=ALU.add), reads=[imp[1], ad_r], writes=[impm[1]])
            P.op("dve", lambda e: e.max(out=m8a[0][:], in_=impm[0][:]), reads=[impm[1]], writes=[m8a[1]])
            P.op("dve", lambda e: e.match_replace(out=work[0][:], in_to_replace=m8a[0][:], in_values=impm[0][:], imm_value=-1e38),
                 reads=[m8a[1], impm[1]], writes=[work[1]])
            P.op("dve", lambda e: e.max(out=m8b[0][:], in_=work[0][:]), reads=[work[1]], writes=[m8b[1]])
            P.op("dve", lambda e: e.tensor_scalar(out=gt[0][:], in0=impm[0][:], scalar1=-5e29, scalar2=None, op0=ALU.is_gt),
                 reads=[impm[1]], writes=[gt[1]])
            P.op("dve", lambda e: e.scalar_tensor_tensor(out=sl_[0][:], in0=impm[0][:], scalar=m8b[0][:, 7:8], in1=gt[0][:],
                                                         op0=ALU.is_ge, op1=ALU.mult), reads=[impm[1], m8b[1], gt[1]], writes=[sl_[1]])
            P.op("dve", lambda e: e.tensor_scalar(out=no[0][:], in0=ad_t[:], scalar1=5e8, scalar2=None, op0=ALU.is_lt), reads=[ad_r], writes=[no[1]])
            P.op("dve", lambda e: e.tensor_tensor(out=sl_[0][:], in0=sl_[0][:], in1=no[0][:], op=ALU.mult), reads=[sl_[1], no[1]], writes=[sl_[1]])
            P.op("dve", lambda e: e.tensor_scalar(out=brow[0][:], in0=sl_[0][:], scalar1=1.0, scalar2=BIG, op0=ALU.subtract, op1=ALU.mult),
                 reads=[sl_[1]], writes=[brow[1]])
            tp_t, tp_r = C.tpb
            for ch in range(2):
                P.op("pe", lambda e: e.transpose(out=tp_t[:, ch, :], in_=brow[0][:, ch * 128:(ch + 1) * 128], identity=C.ident[0][:]),
                     reads=[brow[1], C.ident[1]], writes=[tp_r] if ch == 0 else [])
            tp_r.w = ("s_pe", P.engs["pe"].count)
            for ch in range(2):
                P.op("dve", lambda e: e.tensor_copy(out=bTn[ch][0][:], in_=tp_t[:, ch, :].unsqueeze(1).to_broadcast([128, 4, 128])),
                     reads=[tp_r], writes=[bTn[ch][1]])
            if t < 4:
                P.dma("sp", wmt[0][:], d["wmask"].ap()[t], writes=[wmt[1]])
                wm = wmt
            else:
                wm = wmg
            hb_ = (t // 4) * 8 + (t % 4)
            steps = [(kw[0][:, (hb_ + j) * 128:(hb_ + j + 1) * 128], [(C.ident[0][:], wm[0][:, j, :])], vw[0][:, hb_ + j, :],
                      [kw[1], vw[1], C.ident[1], wm[1]]) for j in range(5)]
            attn_run(P, C, qr_t[:], [qr_r], steps, 129)
            for h in range(4):
                evac_den(P, C, h, 128)
                o_t, o_r = C.ob[h]
                P.op("dve", lambda e: e.tensor_tensor(out=coef[0][:, h:h + 1], in0=C.den[0][:, h:h + 1], in1=gts[0][:, t, 3 * h + 2:3 * h + 3], op=ALU.mult),
                     reads=[C.den[1], gts[1]], writes=[coef[1]])
                P.op("dve", lambda e: e.scalar_tensor_tensor(out=acc[0][:, h, :], in0=o_t[:, 0:128], scalar=coef[0][:, h:h + 1], in1=acc[0][:, h, :],
                                                             op0=ALU.mult, op1=ALU.add), reads=[o_r, coef[1], acc[1]], writes=[acc[1]])
            steps = []
            for kt in range(KMAX[t // 4]):
                ch, jt = kt // 64, kt % 64
                steps.append((KT[0][0][:, kt * 128:(kt + 1) * 128], [(C.eall[0][:, 128 * jt:128 * jt + 128], bTn[ch][0][:].rearrange("p h q -> p (h q)"))],
                              VB[0][:, kt, :], [KT[0][1], VB[1], C.eall[1], bTn[ch][1]]))
            steps.append((ksl[0][:, t * 128:(t + 1) * 128], [(C.ident[0][:], dsel[0][:])], vsl[0][:, t, :], [ksl[1], vsl[1], C.ident[1], dsel[1]]))
            attn_run(P, C, qr_t[:], [qr_r], steps, 129)
            for h in range(4):
                evac_den(P, C, h, 128)
                o_t, o_r = C.ob[h]
                P.op("dve", lambda e: e.tensor_tensor(out=coef[0][:, h:h + 1], in0=C.den[0][:, h:h + 1], in1=gts[0][:, t, 3 * h + 1:3 * h + 2], op=ALU.mult),
                     reads=[C.den[1], gts[1]], writes=[coef[1]])
                P.op("dve", lambda e: e.scalar_tensor_tensor(out=yn[0][:, t, h * 128:(h + 1) * 128], in0=o_t[:, 0:128], scalar=coef[0][:, h:h + 1],
                                                             in1=acc[0][:, h, :], op0=ALU.mult, op1=ALU.add),
                     reads=[o_r, coef[1], acc[1]], writes=[yn[1]])


TWO_PI_S = 6.283185


def rr_sincos(P, x, xr, n, p, out_sin, out_cos, out_r, tmps):
    (r, rr), (ri, rir), (rf, rfr), (t1, t1r) = tmps
    P.op("dve", lambda e: e.tensor_scalar(out=r[0:p, 0:n], in0=x, scalar1=1.0 / (2 * np.pi), scalar2=None, op0=ALU.mult), reads=[xr], writes=[rr])
    P.op("dve", lambda e: e.tensor_copy(out=ri[0:p, 0:n], in_=r[0:p, 0:n]), reads=[rr], writes=[rir])
    P.op("dve", lambda e: e.tensor_copy(out=rf[0:p, 0:n], in_=ri[0:p, 0:n]), reads=[rir], writes=[rfr])
    P.op("dve", lambda e: e.tensor_tensor(out=r[0:p, 0:n], in0=r[0:p, 0:n], in1=rf[0:p, 0:n], op=ALU.subtract), reads=[rr, rfr], writes=[rr])
    for shift in (0.0, 0.25):
        if shift:
            P.op("dve", lambda e: e.tensor_scalar(out=r[0:p, 0:n], in0=r[0:p, 0:n], scalar1=shift, scalar2=None, op0=ALU.add), reads=[rr], writes=[rr])
        P.op("dve", lambda e: e.tensor_scalar(out=t1[0:p, 0:n], in0=r[0:p, 0:n], scalar1=0.5, scalar2=None, op0=ALU.is_gt), reads=[rr], writes=[t1r])
        P.op("dve", lambda e: e.tensor_tensor(out=r[0:p, 0:n], in0=r[0:p, 0:n], in1=t1[0:p, 0:n], op=ALU.subtract), reads=[rr, t1r], writes=[rr])
        P.op("dve", lambda e: e.tensor_scalar(out=t1[0:p, 0:n], in0=r[0:p, 0:n], scalar1=-0.5, scalar2=None, op0=ALU.is_lt), reads=[rr], writes=[t1r])
        P.op("dve", lambda e: e.tensor_tensor(out=r[0:p, 0:n], in0=r[0:p, 0:n], in1=t1[0:p, 0:n], op=ALU.add), reads=[rr, t1r], writes=[rr])
        dst = out_sin if not shift else out_cos
        P.op("act", lambda e: e.activation(out=dst, in_=r[0:p, 0:n], func=AF.Sin, scale=TWO_PI_S), reads=[rr], writes=[out_r])


def phase_s5(P, d, yT_d):
    T = TS5
    with P.scope():
        pc = P.sb([128, 2, 3], F32, "pc")
        prow = P.sb([32, 3, 256], F32, "prow")
        P.dma("sp", pc[0][:], d["s5col"].ap(), writes=[pc[1]])
        P.dma("sp", prow[0][:], d["s5row"].ap(), writes=[prow[1]])
        bre = P.sb([32, 256], F32, "bTre")
        bim = P.sb([32, 256], F32, "bTim")
        cre = P.sb([128, 2, 32], F32, "cTre")
        cim = P.sb([128, 2, 32], F32, "cTim")
        dv = P.sb([32, 2], F32, "dvec")
        P.dma("sp", bre[0][:], d["bTre"].ap(), writes=[bre[1]])
        P.dma("sp", bim[0][:], d["bTim"].ap(), writes=[bim[1]])
        P.dma("sp", cre[0][:], d["cTre"].ap(), writes=[cre[1]])
        P.dma("sp", cim[0][:], d["cTim"].ap(), writes=[cim[1]])
        P.dma("sp", dv[0][:], d["dvec"].ap(), writes=[dv[1]])
        P.op("dve", lambda e: e.tensor_scalar(out=cim[0][:], in0=cim[0][:], scalar1=-1.0, scalar2=None, op0=ALU.mult), reads=[cim[1]], writes=[cim[1]])
        rho = [P.sb([128, T], F32, "rho%d" % i) for i in range(2)]
        sinT = [P.sb([128, T + 1], F32, "sinT%d" % i) for i in range(2)]
        cosT = [P.sb([128, T + 1], F32, "cosT%d" % i) for i in range(2)]
        bbre = P.sb([32, 256], F32, "bbre")
        bbim = P.sb([32, 256], F32, "bbim")
        with P.scope():
            tmps = [P.sb([128, T + 1], F32, "rr0"), P.sb([128, T + 1], I32, "rr1"), P.sb([128, T + 1], F32, "rr2"), P.sb([128, T + 1], F32, "rr3")]
            kk = P.sb([128, T + 1], F32, "kk")
            ph = P.sb([128, T + 1], F32, "ph")
            P.dma("sp", kk[0][:], d["kk"].ap(), writes=[kk[1]])
            dtc = P.sb([128, 2], F32, "dtc")
            magc = P.sb([128, 2], F32, "magc")
            thc = P.sb([128, 2], F32, "thc")
            P.op("act", lambda e: e.activation(out=dtc[0][:], in_=pc[0][:, :, 2], func=AF.Exp), reads=[pc[1]], writes=[dtc[1]])
            P.op("dve", lambda e: e.tensor_tensor(out=magc[0][:], in0=pc[0][:, :, 0], in1=dtc[0][:], op=ALU.mult), reads=[pc[1], dtc[1]], writes=[magc[1]])
            P.op("act", lambda e: e.activation(out=magc[0][:], in_=magc[0][:], func=AF.Exp), reads=[magc[1]], writes=[magc[1]])
            P.op("dve", lambda e: e.tensor_tensor(out=thc[0][:], in0=pc[0][:, :, 1], in1=dtc[0][:], op=ALU.mult), reads=[pc[1], dtc[1]], writes=[thc[1]])
            for pr in range(2):
                P.op("dve", lambda e: e.tensor_copy(out=rho[pr][0][:], in_=magc[0][:, pr:pr + 1].to_broadcast([128, T])), reads=[magc[1]], writes=[rho[pr][1]])
                P.op("dve", lambda e: e.tensor_scalar(out=ph[0][:], in0=kk[0][:], scalar1=thc[0][:, pr:pr + 1], scalar2=None, op0=ALU.mult),
                     reads=[kk[1], thc[1]], writes=[ph[1]])
                rr_sincos(P, ph[0][:], ph[1], T + 1, 128, sinT[pr][0][:], cosT[pr][0][:], sinT[pr][1], tmps)
                cosT[pr][1].w = sinT[pr][1].w
            dtr = P.sb([32, 256], F32, "dtr")
            magr = P.sb([32, 256], F32, "magr")
            angr = P.sb([32, 256], F32, "angr")
            sr = P.sb([32, 256], F32, "sr")
            cr = P.sb([32, 256], F32, "cr")
            w = [P.sb([32, 256], F32, "w%d" % i) for i in range(6)]
            ar = prow[0][:, 0, :]
            ai = prow[0][:, 1, :]
            P.op("act", lambda e: e.activation(out=dtr[0][:], in_=prow[0][:, 2, :], func=AF.Exp), reads=[prow[1]], writes=[dtr[1]])
            P.op("dve", lambda e: e.tensor_tensor(out=magr[0][:], in0=ar, in1=dtr[0][:], op=ALU.mult), reads=[prow[1], dtr[1]], writes=[magr[1]])
            P.op("act", lambda e: e.activation(out=magr[0][:], in_=magr[0][:], func=AF.Exp), reads=[magr[1]], writes=[magr[1]])
            P.op("dve", lambda e: e.tensor_tensor(out=angr[0][:], in0=ai, in1=dtr[0][:], op=ALU.mult), reads=[prow[1], dtr[1]], writes=[angr[1]])
            rr_sincos(P, angr[0][:], angr[1], 256, 32, sr[0][:], cr[0][:], sr[1], tmps)
            cr[1].w = sr[1].w

            def tt(o, a, b, op, rd):
                P.op("dve", lambda e: e.tensor_tensor(out=o[0][:], in0=a, in1=b, op=op), reads=rd, writes=[o[1]])
            lre, lim, den, t0, cre_, cim_ = w
            tt(lre, magr[0][:], cr[0][:], ALU.mult, [magr[1], sr[1]])
            tt(lim, magr[0][:], sr[0][:], ALU.mult, [magr[1], sr[1]])
            P.op("dve", lambda e: e.tensor_scalar(out=lre[0][:], in0=lre[0][:], scalar1=-1.0, scalar2=None, op0=ALU.add), reads=[lre[1]], writes=[lre[1]])
            tt(den, ar, ar, ALU.mult, [prow[1]])
            tt(t0, ai, ai, ALU.mult, [prow[1]])
            tt(den, den[0][:], t0[0][:], ALU.add, [den[1], t0[1]])
            P.op("dve", lambda e: e.reciprocal(out=den[0][:], in_=den[0][:]), reads=[den[1]], writes=[den[1]])
            tt(cre_, lre[0][:], ar, ALU.mult, [lre[1], prow[1]])
            tt(t0, lim[0][:], ai, ALU.mult, [lim[1], prow[1]])
            tt(cre_, cre_[0][:], t0[0][:], ALU.add, [cre_[1], t0[1]])
            tt(cre_, cre_[0][:], den[0][:], ALU.mult, [cre_[1], den[1]])
            tt(cim_, lim[0][:], ar, ALU.mult, [lim[1], prow[1]])
            tt(t0, lre[0][:], ai, ALU.mult, [lre[1], prow[1]])
            tt(cim_, cim_[0][:], t0[0][:], ALU.subtract, [cim_[1], t0[1]])
            tt(cim_, cim_[0][:], den[0][:], ALU.mult, [cim_[1], den[1]])
            tt(bbre, cre_[0][:], bre[0][:], ALU.mult, [cre_[1], bre[1]])
            tt(t0, cim_[0][:], bim[0][:], ALU.mult, [cim_[1], bim[1]])
            tt(bbre, bbre[0][:], t0[0][:], ALU.subtract, [bbre[1], t0[1]])
            tt(bbim, cre_[0][:], bim[0][:], ALU.mult, [cre_[1], bim[1]])
            tt(t0, cim_[0][:], bre[0][:], ALU.mult, [cim_[1], bre[1]])
            tt(bbim, bbim[0][:], t0[0][:], ALU.add, [bbim[1], t0[1]])
        ut = [P.sb([32, T], F32, "ut%d" % i) for i in range(2)]
        yo = [P.sb([32, T], F32, "yo%d" % i) for i in range(2)]
        bpr = P.sb([128, T], F32, "bpr")
        bpi = P.sb([128, T], F32, "bpi")
        gre = P.sb([128, T], F32, "gre")
        gim = P.sb([128, T], F32, "gim")
        mm_ = [P.sb([128, T], F32, "m%d" % i) for i in range(4)]
        hre = P.sb([128, T], F32, "hre")
        him = P.sb([128, T], F32, "him")
        tq = [P.sb([128, 512], F32, "tq%d" % i) for i in range(4)]
        gi = P.sb([128, 4], F32, "gi")
        xx = P.sb([128, 4], F32, "xx")
        P.op("pool", lambda e: e.memset(gi[0][:], 0.0), writes=[gi[1]])
        pb = [P.ps([128, 512], F32, "pb%d" % i) for i in range(4)]
        py = [P.ps([32, 512], F32, "py%d" % i) for i in range(2)]
        n = 0
        for ci in range(L // T):
            for pr in range(2):
                u_t, u_r = ut[n % 2]
                y_t, y_r = yo[n % 2]
                n += 1
                P.dma("sp", u_t[:], d["uT"].ap()[pr, :, ci * T:(ci + 1) * T], writes=[u_r])
                cs, sn = cosT[pr], sinT[pr]
                for half in range(T // 512):
                    sl = slice(half * 512, (half + 1) * 512)
                    p_re, p_im = pb[2 * (half % 2)], pb[2 * (half % 2) + 1]
                    P.op("pe", lambda e: e.matmul(p_re[0][:], lhsT=bbre[0][:, pr * 128:(pr + 1) * 128], rhs=u_t[:, sl], start=True, stop=True),
                         reads=[bbre[1], u_r], writes=[p_re[1]])
                    P.op("pe", lambda e: e.matmul(p_im[0][:], lhsT=bbim[0][:, pr * 128:(pr + 1) * 128], rhs=u_t[:, sl], start=True, stop=True),
                         reads=[bbim[1], u_r], writes=[p_im[1]])
                    P.op("dve", lambda e: e.tensor_tensor(out=tq[0][0][:], in0=p_re[0][:], in1=cs[0][:, sl], op=ALU.mult), reads=[p_re[1], cs[1]], writes=[tq[0][1]])
                    P.op("dve", lambda e: e.tensor_tensor(out=tq[1][0][:], in0=p_im[0][:], in1=sn[0][:, sl], op=ALU.mult), reads=[p_im[1], sn[1]], writes=[tq[1][1]])
                    P.op("pool", lambda e: e.tensor_tensor(out=bpr[0][:, sl], in0=tq[0][0][:], in1=tq[1][0][:], op=ALU.add), reads=[tq[0][1], tq[1][1]], writes=[bpr[1]])
                    P.op("dve", lambda e: e.tensor_tensor(out=tq[2][0][:], in0=p_im[0][:], in1=cs[0][:, sl], op=ALU.mult), reads=[p_im[1], cs[1]], writes=[tq[2][1]])
                    P.op("dve", lambda e: e.tensor_tensor(out=tq[3][0][:], in0=p_re[0][:], in1=sn[0][:, sl], op=ALU.mult), reads=[p_re[1], sn[1]], writes=[tq[3][1]])
                    P.op("pool", lambda e: e.tensor_tensor(out=bpi[0][:, sl], in0=tq[2][0][:], in1=tq[3][0][:], op=ALU.subtract), reads=[tq[2][1], tq[3][1]], writes=[bpi[1]])
                P.op("dve", lambda e: e.tensor_tensor_scan(out=gre[0][:], data0=rho[pr][0][:], data1=bpr[0][:], initial=gi[0][:, 2 * pr:2 * pr + 1],
                                                           op0=ALU.mult, op1=ALU.add), reads=[rho[pr][1], bpr[1], gi[1]], writes=[gre[1]])
                P.op("dve", lambda e: e.tensor_tensor_scan(out=gim[0][:], data0=rho[pr][0][:], data1=bpi[0][:], initial=gi[0][:, 2 * pr + 1:2 * pr + 2],
                                                           op0=ALU.mult, op1=ALU.add), reads=[rho[pr][1], bpi[1], gi[1]], writes=[gim[1]])
                cT_, sT_ = cs[0][:, T:T + 1], sn[0][:, T:T + 1]
                gl_r, gl_i = gre[0][:, T - 1:T], gim[0][:, T - 1:T]
                P.op("dve", lambda e: e.tensor_tensor(out=xx[0][:, 0:1], in0=gl_r, in1=cT_, op=ALU.mult), reads=[gre[1], cs[1]], writes=[xx[1]])
                P.op("dve", lambda e: e.tensor_tensor(out=xx[0][:, 1:2], in0=gl_i, in1=sT_, op=ALU.mult), reads=[gim[1], sn[1]], writes=[xx[1]])
                P.op("dve", lambda e: e.tensor_tensor(out=xx[0][:, 2:3], in0=gl_r, in1=sT_, op=ALU.mult), reads=[gre[1], sn[1]], writes=[xx[1]])
                P.op("dve", lambda e: e.tensor_tensor(out=xx[0][:, 3:4], in0=gl_i, in1=cT_, op=ALU.mult), reads=[gim[1], cs[1]], writes=[xx[1]])
                P.op("dve", lambda e: e.tensor_tensor(out=gi[0][:, 2 * pr:2 * pr + 1], in0=xx[0][:, 0:1], in1=xx[0][:, 1:2], op=ALU.subtract), reads=[xx[1]], writes=[gi[1]])
                P.op("dve", lambda e: e.tensor_tensor(out=gi[0][:, 2 * pr + 1:2 * pr + 2], in0=xx[0][:, 2:3], in1=xx[0][:, 3:4], op=ALU.add), reads=[xx[1]], writes=[gi[1]])
                P.op("dve", lambda e: e.tensor_tensor(out=mm_[0][0][:], in0=gre[0][:], in1=cs[0][:, 0:T], op=ALU.mult), reads=[gre[1], cs[1]], writes=[mm_[0][1]])
                P.op("dve", lambda e: e.tensor_tensor(out=mm_[1][0][:], in0=gim[0][:], in1=sn[0][:, 0:T], op=ALU.mult), reads=[gim[1], sn[1]], writes=[mm_[1][1]])
                P.op("pool", lambda e: e.tensor_tensor(out=mm_[2][0][:], in0=gre[0][:], in1=sn[0][:, 0:T], op=ALU.mult), reads=[gre[1], sn[1]], writes=[mm_[2][1]])
                P.op("pool", lambda e: e.tensor_tensor(out=mm_[3][0][:], in0=gim[0][:], in1=cs[0][:, 0:T], op=ALU.mult), reads=[gim[1], cs[1]], writes=[mm_[3][1]])
                P.op("dve", lambda e: e.tensor_tensor(out=hre[0][:], in0=mm_[0][0][:], in1=mm_[1][0][:], op=ALU.subtract), reads=[mm_[0][1], mm_[1][1]], writes=[hre[1]])
                P.op("pool", lambda e: e.tensor_tensor(out=him[0][:], in0=mm_[2][0][:], in1=mm_[3][0][:], op=ALU.add), reads=[mm_[2][1], mm_[3][1]], writes=[him[1]])
                for half in range(T // 512):
                    sl = slice(half * 512, (half + 1) * 512)
                    y_p = py[half % 2]
                    P.mm_group(y_p[1], [lambda e, st, sp_: e.matmul(y_p[0][:], lhsT=cre[0][:, pr, :], rhs=hre[0][:, sl], start=st, stop=sp_),
                                        lambda e, st, sp_: e.matmul(y_p[0][:], lhsT=cim[0][:, pr, :], rhs=him[0][:, sl], start=st, stop=sp_)],
                               reads=[cre[1], cim[1], hre[1], him[1]])
                    P.op("dve", lambda e: e.scalar_tensor_tensor(out=y_t[:, sl], in0=u_t[:, sl], scalar=dv[0][:, pr:pr + 1], in1=y_p[0][:],
                                                                 op0=ALU.mult, op1=ALU.add), reads=[u_r, dv[1], y_p[1]], writes=[y_r])
                P.dma("sp", yT_d.ap()[pr * 32:(pr + 1) * 32, ci * T:(ci + 1) * T], y_t[:], reads=[y_r])


def build_B(do_cross=True, do_moba=True, do_nsa=True, do_s5=True):
    P = Prog()
    d = {}

    def inp(name, shape, dt):
        d[name] = P.dram(name, shape, dt, kind="ExternalInput")[0]

    inp("ident", [128, 128], BF16)
    inp("eall", [128, 8192], BF16)
    outs = {}
    if do_cross:
        inp("memT", [2048, 256], F32)
        inp("wk", [2048, 512], F32)
        inp("wv", [2048, 512], F32)
        inp("xqT", [4, 128, TOK], BF16)
        outs["yx"] = P.dram("yx", [TOK, 512], BF16, kind="ExternalOutput")[0]
    if do_moba:
        inp("mqT", [4, 128, TOK], BF16)
        inp("mkT", [4, 128, L], BF16)
        inp("mv", [4, 128, NKT, 128], BF16)
        inp("mkTl", [4, 128, TOK], BF16)
        inp("mvl", [4, 128, 16, 128], BF16)
        inp("gmask", [TOK, 64], F32)
        inp("dmoba", [4, 128, 512], BF16)
        outs["ym"] = P.dram("ym", [TOK, 512], BF16, kind="ExternalOutput")[0]
    if do_nsa:
        for nm, shp, dt in (("kcmpT", [128, L], BF16), ("vcmpT", [128, L], BF16), ("kselT", [128, L], BF16), ("vsel", [128, NKT, 128], BF16),
                            ("kselTl", [128, TOK], BF16), ("vsell", [128, 16, 128], BF16), ("kwinT", [128, 4096], BF16), ("vwin", [128, 32, 128], BF16),
                            ("nqT", [128, 16, 512], BF16), ("nqrT", [128, 16, 512], BF16), ("gates", [TOK, 12], BF16),
                            ("ck1", [4096, 256], F32), ("cv1", [4096, 256], F32), ("ck2", [256, 128], F32), ("cv2", [256, 128], F32),
                            ("poskT", [128, 32], F32), ("posvT", [128, 32], F32), ("amat", [128, 8, 256], BF16), ("addm", [TOK, 256], F32),
                            ("cthr", [128, 128], F32), ("v16", [128, 512], F32), ("dsel", [128, 512], BF16), ("wmask", [5, 128, 5, 512], BF16)):
            inp(nm, shp, dt)
        outs["yn"] = P.dram("yn", [TOK, 512], BF16, kind="ExternalOutput")[0]
    if do_s5:
        for nm, shp, dt in (("uT", [2, 32, L], F32), ("s5col", [128, 2, 3], F32), ("s5row", [32, 3, 256], F32), ("bTre", [32, 256], F32),
                            ("bTim", [32, 256], F32), ("cTre", [128, 2, 32], F32), ("cTim", [128, 2, 32], F32), ("dvec", [32, 2], F32),
                            ("kk", [128, TS5 + 1], F32)):
            inp(nm, shp, dt)
        outs["ysT"] = P.dram("ysT", [64, L], F32, kind="ExternalOutput")[0]
    if do_cross or do_moba or do_nsa:
      with P.scope():
        C = attn_ctx(P)
        load_consts(P, C, d)
        ystage = P.sb([128, 16, 512], BF16, "ystage")
        if do_cross:
            phase_cross(P, C, d, ystage)
            P.dma("sp", outs["yx"].ap().rearrange("(t p) f -> p t f", p=128), ystage[0][:], reads=[ystage[1]])
        if do_moba:
            phase_moba(P, C, d, ystage)
            P.dma("sp", outs["ym"].ap().rearrange("(t p) f -> p t f", p=128), ystage[0][:], reads=[ystage[1]])
        if do_nsa:
            phase_nsa(P, C, d, ystage)
            P.dma("sp", outs["yn"].ap().rearrange("(t p) f -> p t f", p=128), ystage[0][:], reads=[ystage[1]])
    if do_s5:
        phase_s5(P, d, outs["ysT"])
    P.finish()
    return P


D = 2048
DFF = 5632
NFC = DFF // 128
ALPHA = (2.0 * 4) ** 0.25
LN_EPS = 1e-5


def ffn_group(P, xT, xT_r, ntok, wg_d, wu_d, wd_d, aT, aT_r, outT_d, col0, gw=None, gw_r=None, pools=None):
    wgb, wub, wdb, pg, pu, po, sil, ost = pools
    nnt = ntok // 512
    wgv = wg_d.ap().rearrange("(c p) f -> p c f", p=128)
    wuv = wu_d.ap().rearrange("(c p) f -> p c f", p=128)
    wdv = wd_d.ap().rearrange("(fc p) d -> p fc d", p=128)

    def load_gu(i):
        g_t, g_r = wgb[i % 2]
        u_t, u_r = wub[i % 2]
        for q in range(2):
            P.dma("pool", g_t[:, 8 * q:8 * q + 8, :], wgv[:, 8 * q:8 * q + 8, 256 * i:256 * (i + 1)], writes=[g_r])
            P.dma("pool", u_t[:, 8 * q:8 * q + 8, :], wuv[:, 8 * q:8 * q + 8, 256 * i:256 * (i + 1)], writes=[u_r])
    load_gu(0)
    cnt = 0
    for i in range(NFC // 2):
        if i + 1 < NFC // 2:
            load_gu(i + 1)
        g_t, g_r = wgb[i % 2]
        u_t, u_r = wub[i % 2]
        for j in range(2):
            fc = 2 * i + j
            for nt in range(nnt):
                pg_t, pg_r = pg[cnt % 2]
                pu_t, pu_r = pu[cnt % 2]
                s_t, s_r = sil[cnt % 2]
                cnt += 1
                P.mm_group(pg_r, [(lambda e, st, sp_, c=c: e.matmul(pg_t[:], lhsT=g_t[:, c, j * 128:(j + 1) * 128], rhs=xT[:, c, nt * 512:(nt + 1) * 512],
                                                                  start=st, stop=sp_)) for c in range(16)], reads=[g_r, xT_r])
                P.mm_group(pu_r, [(lambda e, st, sp_, c=c: e.matmul(pu_t[:], lhsT=u_t[:, c, j * 128:(j + 1) * 128], rhs=xT[:, c, nt * 512:(nt + 1) * 512],
                                                                  start=st, stop=sp_)) for c in range(16)], reads=[u_r, xT_r])
                P.op("act", lambda e: e.activation(out=s_t[:], in_=pg_t[:], func=AF.Silu), reads=[pg_r], writes=[s_r])
                if gw is None:
                    P.op("dve", lambda e: e.tensor_tensor(out=aT[:, fc, nt * 512:(nt + 1) * 512], in0=s_t[:], in1=pu_t[:], op=ALU.mult),
                         reads=[s_r, pu_r], writes=[aT_r])
                else:
                    P.op("dve", lambda e: e.tensor_tensor(out=s_t[:], in0=s_t[:], in1=pu_t[:], op=ALU.mult), reads=[s_r, pu_r], writes=[s_r])
                    P.op("pool", lambda e: e.tensor_tensor(out=aT[:, fc, nt * 512:(nt + 1) * 512], in0=s_t[:], in1=gw[:, nt * 512:(nt + 1) * 512], op=ALU.mult),
                         reads=[s_r, gw_r], writes=[aT_r])

    def load_d(dt):
        w_t, w_r = wdb[dt % 2]
        for q in range(4):
            P.dma("pool", w_t[:, 11 * q:11 * q + 11, :], wdv[:, 11 * q:11 * q + 11, dt * 128:(dt + 1) * 128], writes=[w_r])
    load_d(0)
    cnt = 0
    for dt in range(16):
        if dt + 1 < 16:
            load_d(dt + 1)
        w_t, w_r = wdb[dt % 2]
        for nt in range(nnt):
            po_t, po_r = po[cnt % 2]
            o_t, o_r = ost[cnt % 2]
            cnt += 1
            P.mm_group(po_r, [(lambda e, st, sp_, fc=fc: e.matmul(po_t[:], lhsT=w_t[:, fc, :], rhs=aT[:, fc, nt * 512:(nt + 1) * 512],
                                                                start=st, stop=sp_)) for fc in range(NFC)], reads=[w_r, aT_r])
            P.op("act", lambda e: e.activation(out=o_t[:], in_=po_t[:], func=AF.Copy), reads=[po_r], writes=[o_r])
            P.dma("sp", outT_d.ap()[dt * 128:(dt + 1) * 128, col0 + nt * 512:col0 + (nt + 1) * 512], o_t[:], reads=[o_r])


def ffn_pools(P):
    wgb = [P.sb([128, 16, 256], BF16, "wgb%d" % i) for i in range(2)]
    wub = [P.sb([128, 16, 256], BF16, "wub%d" % i) for i in range(2)]
    wdb = [P.sb([128, NFC, 128], BF16, "wdb%d" % i) for i in range(2)]
    pg = [P.ps([128, 512], F32, "pg%d" % i) for i in range(2)]
    pu = [P.ps([128, 512], F32, "pu%d" % i) for i in range(2)]
    po = [P.ps([128, 512], F32, "po%d" % i) for i in range(2)]
    sil = [P.sb([128, 512], F32, "sil%d" % i) for i in range(2)]
    ost = [P.sb([128, 512], F32, "ost%d" % i) for i in range(2)]
    return wgb, wub, wdb, pg, pu, po, sil, ost


def gelu_tanh_sb(P, src, src_r, dst, dst_r, tmp, n):
    (gt, gtr), (gs_, gsr) = tmp
    P.op("dve", lambda e: e.tensor_tensor(out=gt[:, 0:n], in0=src, in1=src, op=ALU.mult), reads=[src_r], writes=[gtr])
    P.op("dve", lambda e: e.tensor_scalar(out=gt[:, 0:n], in0=gt[:, 0:n], scalar1=0.044715, scalar2=1.0, op0=ALU.mult, op1=ALU.add),
         reads=[gtr], writes=[gtr])
    P.op("dve", lambda e: e.tensor_tensor(out=gt[:, 0:n], in0=gt[:, 0:n], in1=src, op=ALU.mult), reads=[gtr, src_r], writes=[gtr])
    P.op("act", lambda e: e.activation(out=gs_[:, 0:n], in_=gt[:, 0:n], func=AF.Sigmoid, scale=1.5957691216057308), reads=[gtr], writes=[gsr])
    P.op("dve", lambda e: e.tensor_tensor(out=dst, in0=src, in1=gs_[:, 0:n], op=ALU.mult), reads=[src_r, gsr], writes=[dst_r])


def build_C(moe, TOK=2048):
    P = Prog()
    d = {}

    def inp(name, shape, dt):
        d[name] = P.dram(name, shape, dt, kind="ExternalInput")[0]

    def outp(name, shape, dt):
        d[name] = P.dram(name, shape, dt, kind="ExternalOutput")[0]
    GT = 1024
    inp("xres", [TOK, D], F32)
    inp("ysT", [512, TOK], F32)
    inp("yT", [1536, TOK], BF16)
    inp("wglu", [512, 512], F32)
    inp("wo", [D, D], F32)
    inp("lng", [1, D], F32)
    inp("lnb", [1, D], F32)
    inp("identb", [128, 128], BF16)
    outp("ax1", [TOK, D], F32)
    if moe:
        inp("identf", [128, 128], F32)
        inp("router", [D, 8], F32)
        outp("x1b", [TOK, D], BF16)
        outp("gates", [TOK, 8], F32)
    else:
        inp("wg", [D, DFF], F32)
        inp("wu", [D, DFF], F32)
        inp("wd", [DFF, D], F32)
        outp("fT", [D, TOK], F32)
    g_t, g_r = P.sb([128, D], F32, "ln_g")
    b_t, b_r = P.sb([128, D], F32, "ln_b")
    P.dma("sp", g_t[:], d["lng"].ap()[0:1, :].to_broadcast([128, D]), writes=[g_r])
    P.dma("sp", b_t[:], d["lnb"].ap()[0:1, :].to_broadcast([128, D]), writes=[b_r])
    eps_t, eps_r = P.sb([128, 1], F32, "eps")
    P.op("pool", lambda e: e.memset(eps_t[:], LN_EPS), writes=[eps_r])
    idb = P.sb([128, 128], BF16, "idb")
    P.dma("sp", idb[0][:], d["identb"].ap(), writes=[idb[1]])
    wglu = P.sb([128, 4, 512], BF16, "wglu")
    P.dma("pool", wglu[0][:], d["wglu"].ap().rearrange("(c p) f -> p c f", p=128), writes=[wglu[1]])
    if moe:
        idf = P.sb([128, 128], F32, "idf")
        P.dma("sp", idf[0][:], d["identf"].ap(), writes=[idf[1]])
        rt = P.sb([128, 16, 8], F32, "router")
        P.dma("sp", rt[0][:], d["router"].ap().rearrange("(c p) e -> p c e", p=128), writes=[rt[1]])
    for grp in range(TOK // GT):
        t0 = grp * GT
        with P.scope():
            x1T = P.sb([128, 16, GT], BF16, "x1T")
            with P.scope():
                ycT = P.sb([128, 16, GT], BF16, "ycT")
                for q in range(3):
                    P.dma("sp", ycT[0][:, 4 + 4 * q:8 + 4 * q, :], d["yT"].ap().rearrange("(c p) t -> p c t", p=128)[:, 4 * q:4 * q + 4, t0:t0 + GT], writes=[ycT[1]])
                with P.scope():
                    ys = P.sb([128, 4, GT], F32, "ys")
                    zf = P.sb([128, 4, GT], F32, "zf")
                    zb = P.sb([128, 4, GT], BF16, "zb")
                    tmp = [P.sb([128, GT], F32, "gl%d" % i) for i in range(2)]
                    sg = P.sb([128, 512], F32, "sg")
                    pgl = [P.ps([128, 512], F32, "pgl%d" % i) for i in range(2)]
                    P.dma("sp", ys[0][:], d["ysT"].ap().rearrange("(c p) t -> p c t", p=128)[:, :, t0:t0 + GT], writes=[ys[1]])
                    for c in range(4):
                        gelu_tanh_sb(P, ys[0][:, c, :], ys[1], zf[0][:, c, :], zf[1], tmp, GT)
                    P.op("act", lambda e: e.activation(out=zb[0][:], in_=zf[0][:], func=AF.Copy), reads=[zf[1]], writes=[zb[1]])
                    n = 0
                    for fo in range(4):
                        for nt in range(GT // 512):
                            p_t, p_r = pgl[n % 2]
                            n += 1
                            P.mm_group(p_r, [(lambda e, st, sp_, c=c: e.matmul(p_t[:], lhsT=wglu[0][:, c, fo * 128:(fo + 1) * 128],
                                                                             rhs=zb[0][:, c, nt * 512:(nt + 1) * 512], start=st, stop=sp_)) for c in range(4)],
                                       reads=[wglu[1], zb[1]])
                            P.op("act", lambda e: e.activation(out=sg[0][:], in_=p_t[:], func=AF.Sigmoid), reads=[p_r], writes=[sg[1]])
                            P.op("dve", lambda e: e.tensor_tensor(out=ycT[0][:, fo, nt * 512:(nt + 1) * 512], in0=zf[0][:, fo, nt * 512:(nt + 1) * 512],
                                                                  in1=sg[0][:], op=ALU.mult), reads=[zf[1], sg[1]], writes=[ycT[1]])
                xp = [P.sb([128, D], F32, "xp%d" % i) for i in range(GT // 128)]
                for t in range(GT // 128):
                    P.dma("sp", xp[t][0][:], d["xres"].ap()[t0 + t * 128:t0 + (t + 1) * 128, :], writes=[xp[t][1]])
                with P.scope():
                    wot = [P.sb([128, 16, 512], BF16, "wot%d" % i) for i in range(2)]
                    pw = [P.ps([128, 512], F32, "pw%d" % i) for i in range(2)]
                    wov = d["wo"].ap().rearrange("(c p) f -> p c f", p=128)

                    def load_wo(n_):
                        w_t, w_r = wot[n_ % 2]
                        for q in range(4):
                            P.dma("pool", w_t[:, 4 * q:4 * q + 4, :], wov[:, 4 * q:4 * q + 4, 512 * n_:512 * (n_ + 1)], writes=[w_r])
                    load_wo(0)
                    n = 0
                    for n_ in range(4):
                        if n_ + 1 < 4:
                            load_wo(n_ + 1)
                        w_t, w_r = wot[n_ % 2]
                        for t in range(GT // 128):
                            p_t, p_r = pw[n % 2]
                            n += 1
                            P.mm_group(p_r, [(lambda e, st, sp_, c=c: e.matmul(p_t[:], lhsT=ycT[0][:, c, t * 128:(t + 1) * 128], rhs=w_t[:, c, :],
                                                                             start=st, stop=sp_)) for c in range(16)], reads=[ycT[1], w_r])
                            P.op("dve", lambda e: e.scalar_tensor_tensor(out=xp[t][0][:, n_ * 512:(n_ + 1) * 512], in0=xp[t][0][:, n_ * 512:(n_ + 1) * 512],
                                                                         scalar=ALPHA, in1=p_t[:], op0=ALU.mult, op1=ALU.add),
                                 reads=[xp[t][1], p_r], writes=[xp[t][1]])
                with P.scope():
                    st = [P.sb([128, 4, 6], F32, "bst%d" % i) for i in range(2)]
                    mv = [P.sb([128, 2], F32, "mv%d" % i) for i in range(2)]
                    sd = [P.sb([128, 1], F32, "sd%d" % i) for i in range(2)]
                    xbf = [P.sb([128, D], BF16, "xbf%d" % i) for i in range(2)]
                    axs = [P.sb([128, D], F32, "axs%d" % i) for i in range(2)]
                    tp = [P.ps([128, 4, 128], BF16, "tp%d" % i) for i in range(2)]
                    if moe:
                        tpf = [P.ps([128, 4, 128], F32, "tpf%d" % i) for i in range(2)]
                        xTf = P.sb([128, 16, 128], F32, "xTf")
                        plg = P.ps([128, 8], F32, "plg")
                        lg = P.sb([128, 8], F32, "lg")
                        m8 = P.sb([128, 8], F32, "m8")
                        gv = P.sb([128, 4], F32, "gv")
                        go = [P.sb([128, 8], F32, "go%d" % i) for i in range(2)]
                        g2 = P.sb([128, 8], F32, "g2")
                    ntp = 0
                    for t in range(GT // 128):
                        x_t, x_r = xp[t]
                        s_t, s_r = st[t % 2]
                        m_t, m_r = mv[t % 2]
                        d_t, d_r = sd[t % 2]
                        for c in range(4):
                            P.op("dve", lambda e: e.bn_stats(out=s_t[:, c, :], in_=x_t[:, c * 512:(c + 1) * 512]), reads=[x_r], writes=[s_r])
                        P.op("dve", lambda e: e.bn_aggr(out=m_t[:], in_=s_t[:].rearrange("p a b -> p (a b)")), reads=[s_r], writes=[m_r])
                        P.op("act", lambda e: e.activation(out=d_t[:], in_=m_t[:, 1:2], func=AF.Sqrt, bias=eps_t[:], scale=1.0),
                             reads=[m_r, eps_r], writes=[d_r])
                        P.op("dve", lambda e: e.reciprocal(out=d_t[:], in_=d_t[:]), reads=[d_r], writes=[d_r])
                        P.op("dve", lambda e: e.tensor_scalar(out=x_t[:], in0=x_t[:], scalar1=m_t[:, 0:1], scalar2=d_t[:, 0:1],
                                                              op0=ALU.subtract, op1=ALU.mult), reads=[x_r, m_r, d_r], writes=[x_r])
                        P.op("pool", lambda e: e.tensor_tensor(out=x_t[:], in0=x_t[:], in1=g_t[:], op=ALU.mult), reads=[x_r, g_r], writes=[x_r])
                        P.op("dve", lambda e: e.tensor_tensor(out=x_t[:], in0=x_t[:], in1=b_t[:], op=ALU.add), reads=[x_r, b_r], writes=[x_r])
                        a_t, a_r = axs[t % 2]
                        P.op("pool", lambda e: e.tensor_scalar(out=a_t[:], in0=x_t[:], scalar1=ALPHA, scalar2=None, op0=ALU.mult), reads=[x_r], writes=[a_r])
                        P.dma("sp", d["ax1"].ap()[t0 + t * 128:t0 + (t + 1) * 128, :], a_t[:], reads=[a_r])
                        f_t, f_r = xbf[t % 2]
                        P.op("act", lambda e: e.activation(out=f_t[:], in_=x_t[:], func=AF.Copy), reads=[x_r], writes=[f_r])
                        if moe:
                            P.dma("sp", d["x1b"].ap()[t0 + t * 128:t0 + (t + 1) * 128, :], f_t[:], reads=[f_r])
                        for j in range(4):
                            p_t, p_r = tp[ntp % 2]
                            ntp += 1
                            for k in range(4):
                                c = 4 * j + k
                                P.op("pe", lambda e: e.transpose(out=p_t[:, k, :], in_=f_t[:, c * 128:(c + 1) * 128], identity=idb[0][:]),
                                     reads=[f_r, idb[1]], writes=[p_r] if k == 0 else [])
                            p_r.w = ("s_pe", P.engs["pe"].count)
                            P.op("dve", lambda e: e.tensor_copy(out=x1T[0][:, 4 * j:4 * j + 4, t * 128:(t + 1) * 128], in_=p_t[:]),
                                 reads=[p_r], writes=[x1T[1]])
                        if moe:
                            for j in range(4):
                                p_t, p_r = tpf[j % 2]
                                for k in range(4):
                                    c = 4 * j + k
                                    P.op("pe", lambda e: e.transpose(out=p_t[:, k, :], in_=x_t[:, c * 128:(c + 1) * 128], identity=idf[0][:]),
                                         reads=[x_r, idf[1]], writes=[p_r] if k == 0 else [])
                                p_r.w = ("s_pe", P.engs["pe"].count)
                                P.op("act", lambda e: e.activation(out=xTf[0][:, 4 * j:4 * j + 4, :], in_=p_t[:], func=AF.Copy), reads=[p_r], writes=[xTf[1]])
                            P.mm_group(plg[1], [(lambda e, st_, sp_, c=c: e.matmul(plg[0][:], lhsT=xTf[0][:, c, :], rhs=rt[0][:, c, :], start=st_, stop=sp_))
                                                for c in range(16)], reads=[xTf[1], rt[1]])
                            P.op("dve", lambda e: e.tensor_copy(out=lg[0][:], in_=plg[0][:]), reads=[plg[1]], writes=[lg[1]])
                            P.op("dve", lambda e: e.max(out=m8[0][:], in_=lg[0][:]), reads=[lg[1]], writes=[m8[1]])
                            P.op("dve", lambda e: e.tensor_tensor(out=gv[0][:, 0:1], in0=m8[0][:, 1:2], in1=m8[0][:, 0:1], op=ALU.subtract), reads=[m8[1]], writes=[gv[1]])
                            P.op("act", lambda e: e.activation(out=gv[0][:, 1:2], in_=gv[0][:, 0:1], func=AF.Exp), reads=[gv[1]], writes=[gv[1]])
                            P.op("dve", lambda e: e.tensor_scalar(out=gv[0][:, 2:3], in0=gv[0][:, 1:2], scalar1=1.0, scalar2=None, op0=ALU.add), reads=[gv[1]], writes=[gv[1]])
                            P.op("dve", lambda e: e.reciprocal(out=gv[0][:, 2:3], in_=gv[0][:, 2:3]), reads=[gv[1]], writes=[gv[1]])
                            P.op("dve", lambda e: e.tensor_tensor(out=gv[0][:, 3:4], in0=gv[0][:, 1:2], in1=gv[0][:, 2:3], op=ALU.mult), reads=[gv[1]], writes=[gv[1]])
                            o_t, o_r = go[t % 2]
                            P.op("dve", lambda e: e.tensor_scalar(out=o_t[:], in0=lg[0][:], scalar1=m8[0][:, 0:1], scalar2=gv[0][:, 2:3],
                                                                  op0=ALU.is_equal, op1=ALU.mult), reads=[lg[1], m8[1], gv[1]], writes=[o_r])
                            P.op("dve", lambda e: e.tensor_scalar(out=g2[0][:], in0=lg[0][:], scalar1=m8[0][:, 1:2], scalar2=gv[0][:, 3:4],
                                                                  op0=ALU.is_equal, op1=ALU.mult), reads=[lg[1], m8[1], gv[1]], writes=[g2[1]])
                            P.op("dve", lambda e: e.tensor_tensor(out=o_t[:], in0=o_t[:], in1=g2[0][:], op=ALU.add), reads=[o_r, g2[1]], writes=[o_r])
                            P.dma("sp", d["gates"].ap()[t0 + t * 128:t0 + (t + 1) * 128, :], o_t[:], reads=[o_r])
            if not moe:
                with P.scope():
                    aT = P.sb([128, NFC, GT], BF16, "aT")
                    pools = ffn_pools(P)
                    ffn_group(P, x1T[0], x1T[1], GT, d["wg"], d["wu"], d["wd"], aT[0], aT[1], d["fT"], t0, pools=pools)
    P.finish()
    return P


def build_D(N):
    P = Prog()
    d = {}
    d["xgT"] = P.dram("xgT", [D, N], BF16, kind="ExternalInput")[0]
    d["gw"] = P.dram("gw", [128, N], F32, kind="ExternalInput")[0]
    d["wg"] = P.dram("wg", [D, DFF], F32, kind="ExternalInput")[0]
    d["wu"] = P.dram("wu", [D, DFF], F32, kind="ExternalInput")[0]
    d["wd"] = P.dram("wd", [DFF, D], F32, kind="ExternalInput")[0]
    d["yT"] = P.dram("yT", [D, N], F32, kind="ExternalOutput")[0]
    aT = P.sb([128, NFC, 1024], BF16, "aT")
    pools = ffn_pools(P)
    xs = [P.sb([128, 16, 1024], BF16, "xg%d" % i) for i in range(1)]
    gws = [P.sb([128, 1024], F32, "gw%d" % i) for i in range(1)]
    t0 = 0
    gi = 0
    while t0 < N:
        n = min(1024, N - t0)
        x_t, x_r = xs[0]
        w_t, w_r = gws[0]
        gi += 1
        for q in range(4):
            P.dma("sp", x_t[:, 4 * q:4 * q + 4, 0:n], d["xgT"].ap().rearrange("(c p) t -> p c t", p=128)[:, 4 * q:4 * q + 4, t0:t0 + n], writes=[x_r])
        P.dma("sp", w_t[:, 0:n], d["gw"].ap()[:, t0:t0 + n], writes=[w_r])
        ffn_group(P, x_t, x_r, n, d["wg"], d["wu"], d["wd"], aT[0], aT[1], d["yT"], t0, gw=w_t, gw_r=w_r, pools=pools)
        t0 += n
    P.finish()
    return P


NC_A = 8
NC_C = 8
ROPE_THETA = 500000.0
_LOG = []


def _run(P, ims):
    import time as _t
    t0 = _t.time()
    res = run_bass_kernel_spmd(P.nc, ims, core_ids=list(range(len(ims))))
    _LOG.append(("run", len(ims), P.n_inst, round(_t.time() - t0, 1)))
    return res.results


def _rope_tables():
    pos = np.arange(L, dtype=np.float32)
    inv = (1.0 / (ROPE_THETA ** (np.arange(0, 32, 2, dtype=np.float32) / 32))).astype(np.float32)
    ang = pos[:, None] * inv[None, :]
    return np.cos(ang).astype(np.float32), np.sin(ang).astype(np.float32)


def kernel(**inp):
    x = np.asarray(inp["x"])[0]
    cos, sin = _rope_tables()
    identb = np.eye(128, dtype=np.float32).astype(BF)
    identf = np.eye(128, dtype=np.float32)
    progs = {}

    def prog(key, fn):
        if key not in progs:
            progs[key] = fn()
        return progs[key]

    cc = consts()
    cn = consts_nsa()
    core_c = [dict(core_consts(c), **core_consts_nsa(c)) for c in range(8)]
    memT = np.ascontiguousarray(np.asarray(inp["mem"])[0].T)
    addends = [x]
    lng, lnb = np.asarray(inp["ln_in_g"]), np.asarray(inp["ln_in_b"])
    tokA = L // NC_A
    tokC = L // NC_C
    for i in range(4):
        PA = prog(("A", len(addends), True), lambda: build_A(len(addends), True, tokA))
        w_in = np.ascontiguousarray(np.asarray(inp["w_in"])[i])
        ims = []
        for c in range(NC_A):
            sl = slice(c * tokA, (c + 1) * tokA)
            ims.append({"xin": np.stack([a[sl] for a in addends]), "lng": lng[None], "lnb": lnb[None], "ident": identb, "w": w_in,
                        "cos": cos[sl], "sin": sin[sl]})
        r = _run(PA, ims)
        xres = np.concatenate([q["xres"] for q in r], 0)
        hq = np.concatenate([q["hq"] for q in r], 0)
        u = np.concatenate([q["u"] for q in r], 0)
        del ims, r
        PB = prog(("B",), lambda: build_B())
        nw = nsa_weights(inp, i)
        wk = np.asarray(inp["mem_wk"])[i]
        wv = np.asarray(inp["mem_wv"])[i]
        ims = []
        for c in range(8):
            im = {}
            im.update(cc); im.update(cn); im.update(nw); im.update(core_c[c])
            im.update(prep_B_moba_cross(hq, c)); im.update(prep_B_nsa(hq, c)); im.update(prep_s5(inp, i, c, u))
            im["memT"] = memT; im["wk"] = wk; im["wv"] = wv
            ims.append(im)
        r = _run(PB, ims)
        perm = np.concatenate([core_idx(c) for c in range(8)])
        ycat = np.empty((L, 1536), BF)
        ycat[perm] = np.concatenate([np.concatenate([q[k] for q in r], 0) for k in ("ym", "yn", "yx")], 1)
        ysT = np.concatenate([q["ysT"] for q in r], 0)
        del ims, r, hq, u
        moe = (i % 2 == 1)
        PC = prog(("C", moe), lambda: build_C(moe, tokC))
        ims = []
        for c in range(NC_C):
            sl = slice(c * tokC, (c + 1) * tokC)
            im = {"xres": xres[sl], "ysT": np.ascontiguousarray(ysT[:, sl]), "yT": np.ascontiguousarray(ycat[sl].T),
                  "wglu": np.asarray(inp["ssm_w_glu"])[i], "wo": np.asarray(inp["w_o"])[i],
                  "lng": np.asarray(inp["ln1_g"])[i][None], "lnb": np.asarray(inp["ln1_b"])[i][None], "identb": identb}
            if moe:
                im["identf"] = identf
                im["router"] = np.asarray(inp["moe_router"])[i // 2]
            else:
                im["wg"] = np.asarray(inp["ffn_w_gate"])[i // 2]
                im["wu"] = np.asarray(inp["ffn_w_up"])[i // 2]
                im["wd"] = np.asarray(inp["ffn_w_down"])[i // 2]
            ims.append(im)
        r = _run(PC, ims)
        ax1 = np.concatenate([q["ax1"] for q in r], 0)
        del xres, ycat, ysT
        if not moe:
            f = np.concatenate([np.ascontiguousarray(q["fT"].T) for q in r], 0)
            addends = [ax1, f]
        else:
            x1b = np.concatenate([q["x1b"] for q in r], 0)
            gates = np.concatenate([q["gates"] for q in r], 0)
            del ims, r
            tok_idx, exp_idx = np.nonzero(gates)
            assert tok_idx.shape[0] == 2 * L, "router: expected exactly two experts per token"
            pair = [np.nonzero(exp_idx == e)[0] for e in range(8)]
            nmax = max(len(p) for p in pair)
            N = -(-nmax // 512) * 512
            PD = prog(("D", N), lambda: build_D(N))
            ims = []
            for e in range(8):
                toks = tok_idx[pair[e]]
                xg = np.zeros((N, D), BF)
                xg[:len(toks)] = x1b[toks]
                gw = np.zeros((N,), np.float32)
                gw[:len(toks)] = gates[toks, e]
                ims.append({"xgT": np.ascontiguousarray(xg.T), "gw": np.ascontiguousarray(np.broadcast_to(gw[None], (128, N))),
                            "wg": np.asarray(inp["moe_w_gate"])[i // 2][e], "wu": np.asarray(inp["moe_w_up"])[i // 2][e],
                            "wd": np.asarray(inp["moe_w_down"])[i // 2][e]})
            r = _run(PD, ims)
            Y = [np.zeros((L, D), np.float32), np.zeros((L, D), np.float32)]
            for e in range(8):
                toks = tok_idx[pair[e]]
                slot = pair[e] % 2
                ye = r[e]["yT"].T[:len(toks)]
                for k in range(2):
                    m = slot == k
                    Y[k][toks[m]] = ye[m]
            addends = [ax1, Y[0], Y[1]]
        del ims, r
        lng, lnb = np.asarray(inp["ln2_g"])[i], np.asarray(inp["ln2_b"])[i]
    PF = prog(("A", len(addends), False), lambda: build_A(len(addends), False, tokA))
    ims = []
    for c in range(NC_A):
        sl = slice(c * tokA, (c + 1) * tokA)
        ims.append({"xin": np.stack([a[sl] for a in addends]), "lng": lng[None], "lnb": lnb[None]})
    r = _run(PF, ims)
    out = np.concatenate([q["xres"] for q in r], 0)
    return out[None].astype(np.float32)
```
